# Optimizing a Trainium2 kernel written in Bass

```python
import numpy as np
import jax
import jax.numpy as jnp
from jax import lax

D_MODEL = 1024
BATCH = 2
SEQ = 8192
DEPTH = 2

HEAD_DIM = 64
D_MIX = D_MODEL
GM_GROUPS = 4
GM_WIDTH = GM_GROUPS * HEAD_DIM
GM_CHUNK = 128
NSA_HEADS = 8
NSA_KV_HEADS = 2
NSA_WIDTH = NSA_HEADS * HEAD_DIM
NSA_KV_WIDTH = NSA_KV_HEADS * HEAD_DIM
CMP_BLOCK = 32
CMP_STRIDE = 16
CMP_HIDDEN = 2 * HEAD_DIM
SLC_BLOCK = 64
N_SLC = 16
WINDOW = 512
Q_BLOCK = 128
N_BRANCH = 3
HG_HEADS = 4
HG_WIDTH = HG_HEADS * HEAD_DIM
HG_CHUNK = 64
N_GROUPS = 4
EXPERTS_PER_GROUP = 4
N_EXPERTS = N_GROUPS * EXPERTS_PER_GROUP
TOP_K = 2
D_EXPERT = 512
IN_SPLITS = (GM_WIDTH, GM_WIDTH, NSA_WIDTH) + (NSA_KV_WIDTH,) * 6 + (N_BRANCH * NSA_HEADS,) + (HG_WIDTH,) * 4
IN_COLS = 2 * GM_WIDTH + NSA_WIDTH + 6 * NSA_KV_WIDTH + N_BRANCH * NSA_HEADS + 4 * HG_WIDTH
DEEPNORM_ALPHA = (2.0 * DEPTH) ** 0.25
DEEPNORM_BETA = (8.0 * DEPTH) ** -0.25
LN_EPS = 1e-5
RMS_EPS = 1e-6
NEG_INF = -1e30
FORCE_SELECT = 1e4

kernel_name = 'hybrid_gmlp_nsa_hgrn2_hmoe_deepnorm'


def layer_norm(x, g, b):
    xf = x.astype(jnp.float32)
    mu = jnp.mean(xf, -1, keepdims=True)
    var = jnp.mean(jnp.square(xf - mu), -1, keepdims=True)
    return ((xf - mu) * lax.rsqrt(var + LN_EPS) * g + b).astype(x.dtype)


def head_rms_norm(x, gain):
    xf = x.astype(jnp.float32)
    y = xf * lax.rsqrt(jnp.mean(xf * xf, -1, keepdims=True) + RMS_EPS)
    return (y * gain.reshape(x.shape[-2], x.shape[-1])).astype(x.dtype)


def gmlp_mixer(u, v, v_gain, v_bias, w_s, b_s):
    B, T, G, dh = v.shape
    nc = T // GM_CHUNK
    v = layer_norm(v, v_gain.reshape(G, dh), v_bias.reshape(G, dh))
    causal = jnp.tril(jnp.ones((GM_CHUNK, GM_CHUNK), dtype=bool))
    w = jnp.where(causal, w_s, 0)
    vc = v.reshape(B, nc, GM_CHUNK, G, dh)
    z = jnp.einsum('gts,bcsgd->bctgd', w, vc) + b_s.T[None, None, :, :, None]
    return u * z.reshape(B, T, G, dh)


def compress_blocks(k, pos, w1, w2):
    B, T, H, dh = k.shape
    nc = (T - CMP_BLOCK) // CMP_STRIDE + 1
    idx = np.arange(nc)[:, None] * CMP_STRIDE + np.arange(CMP_BLOCK)[None, :]
    blk = k[:, idx] + pos[None, None, :, None, :]
    blk = blk.transpose(0, 3, 1, 2, 4).reshape(B, H, nc, CMP_BLOCK * dh)
    return jax.nn.gelu(blk @ w1) @ w2


def nsa_mixer(q, k_c, v_c, k_s, v_s, k_w, v_w, gates, cmp_pos, cmp_w1, cmp_w2):
    B, T, H, dh = q.shape
    Hkv = k_c.shape[2]
    G = H // Hkv
    nb = T // Q_BLOCK
    ns = T // SLC_BLOCK
    n_sel = min(N_SLC, ns)
    scale = dh ** -0.5
    kcmp = compress_blocks(k_c, cmp_pos[0], cmp_w1[0], cmp_w2[0])
    vcmp = compress_blocks(v_c, cmp_pos[1], cmp_w1[1], cmp_w2[1])
    ncmp = kcmp.shape[2]
    cmp_end = (np.arange(ncmp) * CMP_STRIDE + CMP_BLOCK - 1).astype(np.int32)
    ii = np.arange(ncmp)[:, None]
    jj = np.arange(ns)[None, :]
    overlap = jnp.asarray(((ii * CMP_STRIDE < (jj + 1) * SLC_BLOCK) &
                           (ii * CMP_STRIDE + CMP_BLOCK > jj * SLC_BLOCK)).astype(np.float32))
    ksb = k_s.transpose(0, 2, 1, 3).reshape(B, Hkv, ns, SLC_BLOCK, dh)
    vsb = v_s.transpose(0, 2, 1, 3).reshape(B, Hkv, ns, SLC_BLOCK, dh)
    kwp = jnp.pad(k_w.transpose(0, 2, 1, 3), ((0, 0), (0, 0), (WINDOW, 0), (0, 0)))
    vwp = jnp.pad(v_w.transpose(0, 2, 1, 3), ((0, 0), (0, 0), (WINDOW, 0), (0, 0)))
    qb = q.reshape(B, nb, Q_BLOCK, Hkv, G, dh).transpose(1, 0, 3, 4, 2, 5)
    gb = gates.reshape(B, nb, Q_BLOCK, Hkv, G, N_BRANCH).transpose(1, 0, 3, 4, 2, 5)
    b_idx = jnp.arange(B)[:, None, None, None]
    h_idx = jnp.arange(Hkv)[None, :, None, None]
    blk_ids = jnp.arange(ns)
    blk_start = blk_ids * SLC_BLOCK

    def block_fn(args):
        c, qc, gc = args
        t = c * Q_BLOCK + jnp.arange(Q_BLOCK)
        s = jnp.einsum('bhgqd,bhnd->bhgqn', qc, kcmp).astype(jnp.float32) * scale
        valid = cmp_end[None, :] <= t[:, None]
        p = jax.nn.softmax(jnp.where(valid, s, NEG_INF), axis=-1) * valid
        o_cmp = jnp.einsum('bhgqn,bhnd->bhgqd', p.astype(vcmp.dtype), vcmp)
        imp = jnp.einsum('bhgqn,nj->bhqj', p, overlap)
        cur = t // SLC_BLOCK
        forced = (blk_ids[None, :] == 0) | (blk_ids[None, :] == cur[:, None]) | (blk_ids[None, :] == cur[:, None] - 1)
        causal_blk = blk_start[None, :] <= t[:, None]
        imp = jnp.where(causal_blk, imp + jnp.where(forced, FORCE_SELECT, 0.0), NEG_INF)
        top_val, top_idx = lax.top_k(imp, n_sel)
        sel_ok = top_val > 0.5 * NEG_INF
        kg = ksb[b_idx, h_idx, top_idx]
        vg = vsb[b_idx, h_idx, top_idx]
        pos = top_idx[..., None] * SLC_BLOCK + jnp.arange(SLC_BLOCK)
        ok = sel_ok[..., None] & (pos <= t[None, None, :, None, None])
        s = jnp.einsum('bhgqd,bhqnld->bhgqnl', qc, kg).astype(jnp.float32) * scale
        s = jnp.where(ok[:, :, None], s, NEG_INF).reshape(B, Hkv, G, Q_BLOCK, n_sel * SLC_BLOCK)
        p = jax.nn.softmax(s, axis=-1).reshape(B, Hkv, G, Q_BLOCK, n_sel, SLC_BLOCK)
        o_slc = jnp.einsum('bhgqnl,bhqnld->bhgqd', p.astype(vg.dtype), vg)
        kwc = lax.dynamic_slice_in_dim(kwp, c * Q_BLOCK, Q_BLOCK + WINDOW, axis=2)
        vwc = lax.dynamic_slice_in_dim(vwp, c * Q_BLOCK, Q_BLOCK + WINDOW, axis=2)
        kpos = c * Q_BLOCK - WINDOW + jnp.arange(Q_BLOCK + WINDOW)
        okw = (kpos[None, :] <= t[:, None]) & (kpos[None, :] > t[:, None] - WINDOW) & (kpos[None, :] >= 0)
        s = jnp.einsum('bhgqd,bhkd->bhgqk', qc, kwc).astype(jnp.float32) * scale
        p = jax.nn.softmax(jnp.where(okw, s, NEG_INF), axis=-1)
        o_win = jnp.einsum('bhgqk,bhkd->bhgqd', p.astype(vwc.dtype), vwc)
        out = gc[..., 0:1] * o_cmp + gc[..., 1:2] * o_slc + gc[..., 2:3] * o_win
        return out.astype(qc.dtype)

    out = lax.map(block_fn, (jnp.arange(nb), qb, gb))
    return out.transpose(1, 0, 4, 2, 3, 5).reshape(B, T, H, dh)


def hgrn2_mixer(q, f_logit, i, lower_bound):
    B, T, H, dk = q.shape
    dv = i.shape[-1]
    dtype = q.dtype
    lb = lower_bound.reshape(H, dk).astype(jnp.float32)
    fl = f_logit.astype(jnp.float32)
    log_f = jnp.logaddexp(jnp.log(lb), jnp.log1p(-lb) + jax.nn.log_sigmoid(fl))
    k = (1.0 - lb) * jax.nn.sigmoid(-fl)
    nc = T // HG_CHUNK

    def chunks(a):
        return a.astype(jnp.float32).reshape(B, nc, HG_CHUNK, H, -1).transpose(1, 0, 3, 2, 4)

    causal = jnp.tril(jnp.ones((HG_CHUNK, HG_CHUNK), dtype=bool))

    def step(S, inp):
        qc, kc, ic, lfc = inp
        b = jnp.cumsum(lfc, axis=2)
        diff = b[:, :, :, None, :] - b[:, :, None, :, :]
        decay = jnp.exp(jnp.where(causal[:, :, None], diff, -jnp.inf))
        attn = jnp.einsum('bhtd,bhsd,bhtsd->bhts', qc, kc, decay)
        o = jnp.einsum('bhts,bhsv->bhtv', attn, ic) + jnp.einsum('bhtd,bhdv->bhtv', qc * jnp.exp(b), S)
        b_last = b[:, :, -1, :]
        S = jnp.exp(b_last)[..., None] * S + jnp.einsum('bhsd,bhsv->bhdv', kc * jnp.exp(b_last[:, :, None, :] - b), ic)
        return S, o

    S0 = jnp.zeros((B, H, dk, dv), jnp.float32)
    _, o = lax.scan(step, S0, (chunks(q), chunks(k), chunks(i), chunks(log_f)))
    return o.transpose(1, 0, 3, 2, 4).reshape(B, T, H, dv).astype(dtype)


def token_mixer(x, w_in, gm_v_gain, gm_v_bias, gm_w_s, gm_b_s, cmp_pos, cmp_w1, cmp_w2,
                nsa_gate_b, lower_bound, out_gain, w_out):
    B, T, _ = x.shape
    h = x @ w_in
    points = np.cumsum(IN_SPLITS)[:-1].tolist()
    (gu, gv, nq, kc, vc, ks, vs, kw, vw, ng, hq, hf, hi, hg) = jnp.split(h, points, axis=-1)

    def heads(a):
        return a.reshape(B, T, -1, HEAD_DIM)

    y_a = gmlp_mixer(heads(jax.nn.gelu(gu)), heads(jax.nn.gelu(gv)), gm_v_gain, gm_v_bias, gm_w_s, gm_b_s)
    gates = jax.nn.sigmoid(ng + nsa_gate_b).reshape(B, T, NSA_HEADS, N_BRANCH)
    y_b = nsa_mixer(heads(nq), heads(kc), heads(vc), heads(ks), heads(vs), heads(kw), heads(vw),
                    gates, cmp_pos, cmp_w1, cmp_w2)
    y_c = hgrn2_mixer(heads(hq), heads(hf), heads(hi), lower_bound)
    y = head_rms_norm(jnp.concatenate([y_a, y_b, y_c], axis=2), out_gain).reshape(B, T, D_MIX)
    n_ab = GM_WIDTH + NSA_WIDTH
    y = jnp.concatenate([y[..., :n_ab], y[..., n_ab:] * jax.nn.silu(hg)], axis=-1)
    return y @ w_out


def hier_moe(x, wg, bg, we, be, w_gate, w_up, w_down):
    B, T, D = x.shape
    xt = x.reshape(-1, D)
    pg = jax.nn.softmax((xt @ wg + bg).astype(jnp.float32), axis=-1)
    g_val, g_idx = lax.top_k(pg, 1)
    le = (xt @ we + be).astype(jnp.float32).reshape(-1, N_GROUPS, EXPERTS_PER_GROUP)
    le_sel = jnp.take_along_axis(le, g_idx[:, :, None], axis=1)[:, 0]
    pe = jax.nn.softmax(le_sel, axis=-1)
    e_val, e_idx = lax.top_k(pe, TOP_K)
    e_val = e_val / jnp.sum(e_val, -1, keepdims=True)
    within = jnp.einsum('nk,nke->ne', e_val, jax.nn.one_hot(e_idx, EXPERTS_PER_GROUP, dtype=jnp.float32))
    gate = (g_val * jax.nn.one_hot(g_idx[:, 0], N_GROUPS, dtype=jnp.float32))[:, :, None] * within[:, None, :]
    gate = gate.reshape(-1, N_EXPERTS).astype(x.dtype)
    out = jnp.zeros_like(xt)
    for e in range(N_EXPERTS):
        hdn = jax.nn.silu(xt @ w_gate[e]) * (xt @ w_up[e])
        out = out + gate[:, e:e + 1] * (hdn @ w_down[e])
    return out.reshape(B, T, D)


def setup_inputs(seed: int = 0) -> dict:
    key = jax.random.key(seed)
    ks = jax.random.split(key, 26)

    def nrm(k, shape, s):
        return jax.random.normal(k, shape, jnp.float32) * s

    L = DEPTH
    return {
        'x': nrm(ks[0], (BATCH, SEQ, D_MODEL), 1.0),
        'w_in': nrm(ks[1], (L, D_MODEL, IN_COLS), D_MODEL ** -0.5),
        'gm_v_gain': 1.0 + nrm(ks[2], (L, GM_WIDTH), 0.02),
        'gm_v_bias': nrm(ks[3], (L, GM_WIDTH), 0.02),
        'gm_w_s': nrm(ks[4], (L, GM_GROUPS, GM_CHUNK, GM_CHUNK), GM_CHUNK ** -0.5),
        'gm_b_s': 1.0 + nrm(ks[5], (L, GM_GROUPS, GM_CHUNK), 0.02),
        'cmp_pos': nrm(ks[6], (L, 2, CMP_BLOCK, HEAD_DIM), 0.1),
        'cmp_w1': nrm(ks[7], (L, 2, CMP_BLOCK * HEAD_DIM, CMP_HIDDEN), (CMP_BLOCK * HEAD_DIM) ** -0.5),
        'cmp_w2': nrm(ks[8], (L, 2, CMP_HIDDEN, HEAD_DIM), CMP_HIDDEN ** -0.5),
        'nsa_gate_b': nrm(ks[9], (L, N_BRANCH * NSA_HEADS), 0.01),
        'hg_lower': nrm(ks[10], (L, HG_WIDTH), 0.1),
        'out_gain': 1.0 + nrm(ks[11], (L, D_MIX), 0.02),
        'w_out': nrm(ks[12], (L, D_MIX, D_MODEL), D_MIX ** -0.5 * DEEPNORM_BETA),
        'ln1_g': 1.0 + nrm(ks[13], (L, D_MODEL), 0.02),
        'ln1_b': nrm(ks[14], (L, D_MODEL), 0.02),
        'router_group_w': nrm(ks[15], (L, D_MODEL, N_GROUPS), D_MODEL ** -0.5),
        'router_group_b': nrm(ks[16], (L, N_GROUPS), 0.01),
        'router_expert_w': nrm(ks[17], (L, D_MODEL, N_EXPERTS), D_MODEL ** -0.5),
        'router_expert_b': nrm(ks[18], (L, N_EXPERTS), 0.01),
        'exp_w_gate': nrm(ks[19], (L, N_EXPERTS, D_MODEL, D_EXPERT), D_MODEL ** -0.5),
        'exp_w_up': nrm(ks[20], (L, N_EXPERTS, D_MODEL, D_EXPERT), D_MODEL ** -0.5),
        'exp_w_down': nrm(ks[21], (L, N_EXPERTS, D_EXPERT, D_MODEL), D_EXPERT ** -0.5 * DEEPNORM_BETA),
        'ln2_g': 1.0 + nrm(ks[22], (L, D_MODEL), 0.02),
        'ln2_b': nrm(ks[23], (L, D_MODEL), 0.02),
    }


def reference(x, w_in, gm_v_gain, gm_v_bias, gm_w_s, gm_b_s, cmp_pos, cmp_w1, cmp_w2, nsa_gate_b,
              hg_lower, out_gain, w_out, ln1_g, ln1_b, router_group_w, router_group_b,
              router_expert_w, router_expert_b, exp_w_gate, exp_w_up, exp_w_down, ln2_g, ln2_b):
    lb_all = jnp.cumsum(jax.nn.softmax(hg_lower.astype(jnp.float32), axis=0), axis=0)
    lb_all = lb_all - lb_all[0]
    for l in range(DEPTH):
        y = token_mixer(x, w_in[l], gm_v_gain[l], gm_v_bias[l], gm_w_s[l], gm_b_s[l], cmp_pos[l],
                        cmp_w1[l], cmp_w2[l], nsa_gate_b[l], lb_all[l], out_gain[l], w_out[l])
        x = layer_norm(DEEPNORM_ALPHA * x + y, ln1_g[l], ln1_b[l])
        y = hier_moe(x, router_group_w[l], router_group_b[l], router_expert_w[l], router_expert_b[l],
                     exp_w_gate[l], exp_w_up[l], exp_w_down[l])
        x = layer_norm(DEEPNORM_ALPHA * x + y, ln2_g[l], ln2_b[l])
    return x
```

```python
import numpy as np
import ml_dtypes
from contextlib import ExitStack
import concourse.bass as bass
import concourse.mybir as mybir
from concourse.bass_utils import run_bass_kernel_spmd

F32 = mybir.dt.float32
BF16 = mybir.dt.bfloat16
AF = mybir.ActivationFunctionType
ALU = mybir.AluOpType
AX = mybir.AxisListType


class T:
    __slots__ = ("name", "t", "last_w", "reads", "dsem", "dcount")

    def __init__(self, name, t=None):
        self.name = name
        self.t = t
        self.last_w = None
        self.reads = {}
        self.dsem = None
        self.dcount = 0

    def __getitem__(self, idx):
        return self.t[idx]


class FW:
    def __init__(self, nc, stack):
        self.nc = nc
        self.stack = stack
        self.eng = {"pe": nc.tensor, "dve": nc.vector, "act": nc.scalar,
                    "pool": nc.gpsimd, "sp": nc.sync}
        self.sems = {}
        self.cnt = {}
        self.waited = {k: {} for k in self.eng}
        for k in self.eng:
            self.sems[k] = stack.enter_context(nc.semaphore("s_" + k))
            self.cnt[k] = 0
        self.n_inst = 0
        self.same_engine_sync = True

    def sb(self, name, shape, dt=F32):
        t = self.stack.enter_context(self.nc.sbuf_tensor("sb_" + name, list(shape), dt))
        return T(name, t)

    def ps(self, name, shape, dt=F32):
        t = self.stack.enter_context(self.nc.psum_tensor("ps_" + name, list(shape), dt))
        return T(name, t)

    def _deps(self, reads, writes):
        deps = {}
        def add(tok):
            if tok is None:
                return
            k, c = tok
            if deps.get(k, 0) < c:
                deps[k] = c
        for t in reads:
            add(t.last_w)
        for t in writes:
            add(t.last_w)
            for k, c in t.reads.items():
                add((k, c))
        return deps

    def _wait(self, e, deps):
        eng = self.eng[e]
        w = self.waited[e]
        for k, c in deps.items():
            if k == e and (e == "pe" or not self.same_engine_sync):
                continue
            if w.get(k, 0) >= c:
                continue
            eng.wait_ge(self.sems[k], c)
            w[k] = c
            self.n_inst += 1

    def op(self, e, fn, reads=(), writes=()):
        deps = self._deps(reads, writes)
        self._wait(e, deps)
        inst = fn(self.eng[e])
        self.cnt[e] += 1
        inst.then_inc(self.sems[e], 1)
        tok = (e, self.cnt[e])
        for t in reads:
            if t.reads.get(e, 0) < tok[1]:
                t.reads[e] = tok[1]
        for t in writes:
            t.last_w = tok
            t.reads = {}
        self.n_inst += 1
        return inst

    def dma(self, q, out, in_, key, reads=(), writes=(), **kw):
        deps = self._deps(reads, writes)
        self._wait(q, deps)
        if key.dsem is None:
            key.dsem = "d_" + key.name + "_%d" % len(self.sems)
            self.sems[key.dsem] = self.stack.enter_context(self.nc.semaphore(key.dsem))
        inst = self.eng[q].dma_start(out=out, in_=in_, **kw)
        key.dcount += 16
        inst.then_inc(self.sems[key.dsem], 16)
        tok = (key.dsem, key.dcount)
        for t in reads:
            if t.reads.get(tok[0], 0) < tok[1]:
                t.reads[tok[0]] = tok[1]
        for t in writes:
            t.last_w = tok
            t.reads = {}
        self.n_inst += 1
        return inst

    def wait_all(self, e, tiles):
        deps = {}
        for t in tiles:
            if t.last_w is not None:
                k, c = t.last_w
                deps[k] = max(deps.get(k, 0), c)
            for k, c in t.reads.items():
                deps[k] = max(deps.get(k, 0), c)
        old = self.same_engine_sync
        self._wait(e, deps)


def new_nc():
    return bass.Bass("TRN2", target_bir_lowering=False)

def din(nc, name, shape, dt=F32):
    return nc.dram_tensor(name, list(shape), dt, kind="ExternalInput").ap()

def dout(nc, name, shape, dt=F32):
    return nc.dram_tensor(name, list(shape), dt, kind="ExternalOutput").ap()

def run(nc, in_maps):
    res = run_bass_kernel_spmd(nc, in_maps, core_ids=list(range(len(in_maps))))
    return res.results

def build_mm(K, N, T):
    nc = new_nc()
    xT = din(nc, "xT", [K, T]); w = din(nc, "w", [K, N]); out = dout(nc, "out", [T, N])
    KC = K // 128
    with ExitStack() as st:
        fw = FW(nc, st)
        xs = fw.sb("xs", [128, KC, T]); ws = fw.sb("ws", [128, KC, N])
        for k in range(KC):
            fw.dma("sp", xs[:, k, :], xT[k*128:(k+1)*128, :], key=xs, writes=[xs])
            fw.dma("pool", ws[:, k, :], w[k*128:(k+1)*128, :], key=ws, writes=[ws])
        pss = [fw.ps("ps%d" % i, [128, 512]) for i in range(6)]
        obs = [fw.sb("ob%d" % i, [128, N]) for i in range(2)]
        cols = [(c0, min(512, N - c0)) for c0 in range(0, N, 512)]
        j = 0
        for t in range(T // 128):
            ob = obs[t % 2]
            for (c0, cn) in cols:
                ps = pss[j % 6]
                for k in range(KC):
                    fw.op("pe", lambda e: e.matmul(ps[:, 0:cn], lhsT=xs[:, k, t*128:(t+1)*128], rhs=ws[:, k, c0:c0+cn],
                                                   start=(k == 0), stop=(k == KC-1)), reads=[xs, ws], writes=[ps])
                if j % 2 == 0:
                    fw.op("act", lambda e: e.copy(out=ob[:, c0:c0+cn], in_=ps[:, 0:cn]), reads=[ps], writes=[ob])
                else:
                    fw.op("dve", lambda e: e.tensor_copy(out=ob[:, c0:c0+cn], in_=ps[:, 0:cn]), reads=[ps], writes=[ob])
                j += 1
            fw.dma("sp", out[t*128:(t+1)*128, :], ob[:], key=ob, reads=[ob])
        fw.wait_all("sp", obs)
    print("mm instr", fw.n_inst)
    return nc

NEGB = -30000.0
NBLK = 32

def nsa_tables(p):
    n = np.arange(128)[:, None]; q = np.arange(128)[None, :]
    tabs = np.zeros((17, 128, 128), np.float32)
    for k in range(9):
        d = 2 * k + p
        tabs[k] = np.where(16 * n + 31 <= 128 * d + q, 0.0, NEGB)
    for u in range(2):
        tabs[9 + u] = np.where(128 * (u - p) + n > q, NEGB, 0.0)
    for u in range(6):
        dl = 128 * (u - 4 - p) + n - q
        tabs[11 + u] = np.where((dl <= 0) & (dl > -512), 0.0, NEGB)
    tabs = np.broadcast_to(tabs[:, :, None, :], (17, 128, 4, 128)).transpose(1, 0, 2, 3).reshape(128, 17, 512)
    ii = np.arange(512)[:, None]; jj = np.arange(128)[None, :]
    ovl = ((ii * 16 < (jj + 1) * 64) & (ii * 16 + 32 > jj * 64)).astype(np.float32)
    ovl = ovl.reshape(4, 128, 128).transpose(1, 0, 2)
    y = np.arange(256)[None, :]; qi = np.arange(128)[:, None]
    rel = (y - 2 * p) - 128; hh = (qi >= 64).astype(np.int64)
    G = np.where(rel > hh, -1e30, np.where((rel == hh) | (rel == hh - 1), 1e4, 0.0)).astype(np.float32)
    x = np.arange(8192)[None, :]; j = np.arange(128)[:, None]
    E = (x // 64 == j).astype(np.float32)
    return dict(tabs=np.ascontiguousarray(tabs).astype(ml_dtypes.bfloat16),
                ovl=np.ascontiguousarray(ovl).astype(ml_dtypes.bfloat16), G=G,
                E=E.astype(ml_dtypes.bfloat16), idb=np.eye(128, dtype=np.float32).astype(ml_dtypes.bfloat16),
                idf=np.eye(128, dtype=np.float32))

def build_nsa():
    nc = new_nc()
    qT_d = din(nc, "qT", [64, NBLK * 512]); ksT_d = din(nc, "ksT", [64, 8192]); kwT_d = din(nc, "kwT", [64, 8192])
    kcT_d = din(nc, "kcT", [64, 8192]); vcT_d = din(nc, "vcT", [64, 8192])
    vs_d = din(nc, "vs", [128, 64, 64]); vw_d = din(nc, "vw", [128, 64, 64])
    w1k_d = din(nc, "w1k", [64, 32 * 128]); w1v_d = din(nc, "w1v", [64, 32 * 128])
    w2k_d = din(nc, "w2k", [128, 64]); w2v_d = din(nc, "w2v", [128, 64])
    posk_d = din(nc, "posk", [64, 32]); posv_d = din(nc, "posv", [64, 32])
    ng_d = din(nc, "ng", [128, NBLK * 12]); gb_d = din(nc, "gb", [128, NBLK * 12])
    tabs_d = din(nc, "tabs", [128, 17, 512], BF16); ovl_d = din(nc, "ovl", [128, 4, 128], BF16)
    G_d = din(nc, "G", [128, 256]); E_d = din(nc, "E", [128, 8192], BF16)
    idb_d = din(nc, "idb", [128, 128], BF16); idf_d = din(nc, "idf", [128, 128])
    out_d = dout(nc, "out", [128, NBLK, 256])
    with ExitStack() as st:
        fw = FW(nc, st)
        op = fw.op
        qT = fw.sb("qT", [64, NBLK * 512], BF16); ksT = fw.sb("ksT", [64, 8192], BF16); kwT = fw.sb("kwT", [64, 8192], BF16)
        vs = fw.sb("vs", [128, 64, 65], BF16); vw = fw.sb("vw", [128, 64, 65], BF16)
        tabs = fw.sb("tabs", [128, 17, 512], BF16); ovl = fw.sb("ovl", [128, 4, 128], BF16)
        G = fw.sb("G", [128, 256]); E = fw.sb("E", [128, 8192], BF16)
        idb = fw.sb("idb", [128, 128], BF16); idf = fw.sb("idf", [128, 128])
        gate = fw.sb("gate", [128, NBLK * 12]); gbt = fw.sb("gbt", [128, NBLK * 12])
        kcmpT = fw.sb("kcmpT", [64, 512], BF16); vcmp = fw.sb("vcmp", [128, 4, 65], BF16)
        ones = fw.sb("ones", [128, 128])
        stg = fw.sb("stg", [128, 4096])
        S = [fw.ps("S0", [128, 512]), fw.ps("S1", [128, 512])]
        O = {b: fw.ps("O" + b, [128, 512]) for b in ("c", "s", "w")}
        M0 = fw.ps("M0", [128, 512]); M1 = fw.ps("M1", [128, 512]); M2 = fw.ps("M2", [128, 512])
        M1b = T("M1b", M1.t[:].bitcast(BF16)) if False else None

        cast_i = [0]
        def load_cast(dst_ap, src_ap, parts, n, dst_t):
            fw.dma("sp", stg[0:parts, 0:n], src_ap, key=stg, writes=[stg])
            e = ("dve", "act", "pool")[cast_i[0] % 3]; cast_i[0] += 1
            if e == "act":
                op(e, lambda en: en.copy(out=dst_ap, in_=stg[0:parts, 0:n]), reads=[stg], writes=[dst_t])
            else:
                op(e, lambda en: en.tensor_copy(out=dst_ap, in_=stg[0:parts, 0:n]), reads=[stg], writes=[dst_t])

        for t_, d_ in ((tabs, tabs_d), (ovl, ovl_d), (G, G_d), (E, E_d), (idb, idb_d), (idf, idf_d), (gate, ng_d), (gbt, gb_d)):
            fw.dma("pool", t_[:], d_, key=t_, writes=[t_])
        op("dve", lambda e: e.memset(ones[:], 1.0), writes=[ones])
        op("dve", lambda e: e.memset(vs[:, :, 64:65], 1.0), writes=[vs])
        op("dve", lambda e: e.memset(vw[:, :, 64:65], 1.0), writes=[vw])
        op("dve", lambda e: e.memset(vcmp[:, :, 64:65], 1.0), writes=[vcmp])
        op("dve", lambda e: e.memset(kcmpT[:], 0.0), writes=[kcmpT])
        op("dve", lambda e: e.memset(vcmp[:, :, 0:64], 0.0), writes=[vcmp])
        op("dve", lambda e: e.tensor_tensor(out=gate[:], in0=gate[:], in1=gbt[:], op=ALU.add), reads=[gate, gbt], writes=[gate])
        op("act", lambda e: e.activation(out=gate[:], in_=gate[:], func=AF.Sigmoid), reads=[gate], writes=[gate])
        for c in range(NBLK * 512 // 4096):
            load_cast(qT[:, c*4096:(c+1)*4096], qT_d[:, c*4096:(c+1)*4096], 64, 4096, qT)
        for c in range(2):
            load_cast(ksT[:, c*4096:(c+1)*4096], ksT_d[:, c*4096:(c+1)*4096], 64, 4096, ksT)
            load_cast(kwT[:, c*4096:(c+1)*4096], kwT_d[:, c*4096:(c+1)*4096], 64, 4096, kwT)
        load_cast(vs[:, :, 0:64], vs_d.rearrange("p k d -> p (k d)"), 128, 4096, vs)
        load_cast(vw[:, :, 0:64], vw_d.rearrange("p k d -> p (k d)"), 128, 4096, vw)

        with ExitStack() as st2:
            fw2stack = fw.stack; fw.stack = st2
            kT = fw.sb("kT", [64, 8192], BF16); w1 = fw.sb("w1", [64, 32 * 128], BF16)
            w2 = fw.sb("w2", [128, 64], BF16); w2f = fw.sb("w2f", [128, 64]); pos = fw.sb("pos", [64, 32], BF16); posf = fw.sb("posf", [64, 32])
            bia = fw.sb("bia", [128, 1]); g1 = fw.sb("g1", [128, 512], BF16)
            fw.stack = fw2stack
            for which in range(2):
                kd, w1d, w2d, pd = ((kcT_d, w1k_d, w2k_d, posk_d), (vcT_d, w1v_d, w2v_d, posv_d))[which]
                for c in range(2):
                    load_cast(kT[:, c*4096:(c+1)*4096], kd[:, c*4096:(c+1)*4096], 64, 4096, kT)
                load_cast(w1[:], w1d, 64, 4096, w1)
                fw.dma("sp", w2f[:], w2d, key=w2f, writes=[w2f])
                op("dve", lambda e: e.tensor_copy(out=w2[:], in_=w2f[:]), reads=[w2f], writes=[w2])
                fw.dma("sp", posf[:], pd, key=posf, writes=[posf])
                op("dve", lambda e: e.tensor_copy(out=pos[:], in_=posf[:]), reads=[posf], writes=[pos])
                for l in range(32):
                    op("pe", lambda e: e.matmul(M0[:, 0:511], lhsT=w1[:, l*128:(l+1)*128], rhs=kT[:, l:l+16*510+1:16],
                                                start=(l == 0), stop=(l == 31)), reads=[w1, kT], writes=[M0])
                for l in range(32):
                    op("pe", lambda e: e.matmul(M1[:, 0:1], lhsT=w1[:, l*128:(l+1)*128], rhs=pos[:, l:l+1],
                                                start=(l == 0), stop=(l == 31)), reads=[w1, pos], writes=[M1])
                op("dve", lambda e: e.tensor_copy(out=bia[:], in_=M1[:, 0:1]), reads=[M1], writes=[bia])
                op("dve", lambda e: e.memset(g1[:], 0.0), writes=[g1])
                op("act", lambda e: e.activation(out=g1[:, 0:511], in_=M0[:, 0:511], func=AF.Gelu, bias=bia[:, 0:1]), reads=[M0, bia], writes=[g1])
                if which == 0:
                    op("pe", lambda e: e.matmul(M2[0:64, 0:512], lhsT=w2[:], rhs=g1[:], start=True, stop=True), reads=[w2, g1], writes=[M2])
                    op("dve", lambda e: e.tensor_copy(out=kcmpT[:], in_=M2[0:64, 0:512]), reads=[M2], writes=[kcmpT])
                else:
                    for m in range(4):
                        op("pe", lambda e: e.matmul(M2[:, m*64:(m+1)*64], lhsT=g1[:, m*128:(m+1)*128], rhs=w2[:], start=True, stop=True),
                           reads=[w2, g1], writes=[M2])
                    op("dve", lambda e: e.tensor_copy(out=vcmp[:, :, 0:64], in_=M2[:, 0:256].rearrange("p (m d) -> p m d", m=4)),
                       reads=[M2], writes=[vcmp])
        eall = fw.sb("eall", [128, 4, 512], BF16); pn = fw.sb("pn", [128, 4, 512]); pg = fw.sb("pg", [128, 4, 128], BF16)
        PT = [fw.sb("PT0", [128, 512], BF16), fw.sb("PT1", [128, 512], BF16)]
        rz = fw.sb("rz", [128, 512]); adj = fw.sb("adj", [128, 128]); adj2 = fw.sb("adj2", [128, 128])
        v8 = fw.sb("v8", [128, 16]); thr = fw.sb("thr", [128, 1]); NM = fw.sb("NM", [128, 128], BF16)
        nmT = fw.sb("nmT", [128, 4, 128], BF16)
        osb = fw.sb("osb", [128, 512]); zz = fw.sb("zz", [128, 4]); wgt = fw.sb("wgt", [128, 4])
        acc = [fw.sb("acc0", [128, 256]), fw.sb("acc1", [128, 256])]
        NMTp = T("NMTp", st.enter_context(nc.psum_tensor("NMTp", [128, 128], BF16))) if False else None
        ucount = [0]

        def unit(qc, kT_t, kslice, extra, Vt, kt, Ot, first, last):
            u = ucount[0]; ucount[0] += 1
            Sp = S[u % 2]; Pt = PT[u % 2]
            op("pe", lambda e: e.matmul(Sp[:, :], lhsT=kT_t[:, kslice], rhs=qc, start=True, stop=(len(extra) == 0)),
               reads=[kT_t, qT], writes=[Sp])
            for xi, (lh, rh, rd) in enumerate(extra):
                op("pe", lambda e: e.matmul(Sp[:, :], lhsT=lh, rhs=rh, start=False, stop=(xi == len(extra) - 1)), reads=rd, writes=[Sp])
            op("act", lambda e: e.activation(out=Pt[:], in_=Sp[:], func=AF.Exp, scale=0.125), reads=[Sp], writes=[Pt])
            op("pe", lambda e: e.matmul(Ot[0:65, :], lhsT=Vt[:, kt, :], rhs=Pt[:], start=first, stop=last), reads=[Vt, Pt], writes=[Ot])

        for i in range(NBLK):
            qc = qT[:, i*512:(i+1)*512]
            ac = acc[i % 2]
            ms = [m for m in range(4) if i - 8 * m >= 0]
            for mi, m in enumerate(ms):
                k = i - 8 * m
                u = ucount[0]; ucount[0] += 1
                Sp = S[u % 2]
                nb = k <= 8
                op("pe", lambda e: e.matmul(Sp[:, :], lhsT=kcmpT[:, m*128:(m+1)*128], rhs=qc, start=True, stop=(not nb)), reads=[kcmpT, qT], writes=[Sp])
                if nb:
                    op("pe", lambda e: e.matmul(Sp[:, :], lhsT=idb[:], rhs=tabs[:, k, :], start=False, stop=True), reads=[idb, tabs], writes=[Sp])
                op("act", lambda e: e.activation(out=eall[:, m, :], in_=Sp[:], func=AF.Exp, scale=0.125), reads=[Sp], writes=[eall])
                op("pe", lambda e: e.matmul(O["c"][0:65, :], lhsT=vcmp[:, m, :], rhs=eall[:, m, :], start=(mi == 0), stop=(mi == len(ms) - 1)),
                   reads=[vcmp, eall], writes=[O["c"]])
            nm_ = len(ms)
            op("dve", lambda e: e.tensor_scalar_max(out=rz[64:65, :], in0=O["c"][64:65, :], scalar1=1e-30), reads=[O["c"]], writes=[rz])
            op("dve", lambda e: e.reciprocal(out=rz[64:65, :], in_=rz[64:65, :]), reads=[rz], writes=[rz])
            op("pe", lambda e: e.matmul(M0[:, :], lhsT=ones[64:65, :], rhs=rz[64:65, :], start=True, stop=True), reads=[ones, rz], writes=[M0])
            for m in ms:
                op("dve", lambda e: e.tensor_tensor(out=pn[:, m, :], in0=eall[:, m, :], in1=M0[:, :], op=ALU.mult), reads=[eall, M0], writes=[pn])
            with nc.allow_low_precision("bf16 out, fp32 internal accumulate"):
                op("dve", lambda e: e.tensor_reduce(out=pg[:, 0:nm_, :], in_=pn[:, 0:nm_, :].rearrange("p m (g q) -> p m q g", g=4),
                                                    axis=AX.X, op=ALU.add), reads=[pn], writes=[pg])
            for mi, m in enumerate(ms):
                op("pe", lambda e: e.matmul(M1[:, 0:128], lhsT=pg[:, m, :], rhs=ovl[:, m, :], start=(mi == 0), stop=(mi == nm_ - 1)),
                   reads=[pg, ovl], writes=[M1])
            op("dve", lambda e: e.tensor_tensor(out=adj[:], in0=M1[:, 0:128], in1=G[:, 128-4*i:256-4*i], op=ALU.add), reads=[M1, G], writes=[adj])
            op("dve", lambda e: e.tensor_scalar_add(out=adj[:, 0:1], in0=adj[:, 0:1], scalar1=1e4), reads=[adj], writes=[adj])
            op("dve", lambda e: e.max(out=v8[:, 0:8], in_=adj[:]), reads=[adj], writes=[v8])
            op("dve", lambda e: e.match_replace(out=adj2[:], in_to_replace=v8[:, 0:8], in_values=adj[:], imm_value=-3e38), reads=[adj, v8], writes=[adj2])
            op("dve", lambda e: e.max(out=v8[:, 8:16], in_=adj2[:]), reads=[adj2], writes=[v8])
            op("dve", lambda e: e.tensor_scalar_max(out=thr[:], in0=v8[:, 15:16], scalar1=-1e29), reads=[v8], writes=[thr])
            op("dve", lambda e: e.tensor_scalar(out=NM[:], in0=adj[:], scalar1=thr[:, 0:1], scalar2=NEGB, op0=ALU.is_lt, op1=ALU.mult),
               reads=[adj, thr], writes=[NM])
            Mb = M2.t[:].bitcast(BF16)
            op("pe", lambda e: e.transpose(out=Mb[:, 0:128], in_=NM[:], identity=idb[:]), reads=[NM, idb], writes=[M2])
            op("dve", lambda e: e.tensor_copy(out=nmT[:], in_=Mb[:, 0:128].unsqueeze(1).to_broadcast([128, 4, 128])), reads=[M2], writes=[nmT])
            nmT2 = nmT[:].rearrange("p g q -> p (g q)")
            nk = 2 * i + 2
            for kt in range(nk):
                extra = [(E[:, kt*128:(kt+1)*128], nmT2, [E, nmT])]
                if kt >= 2 * i:
                    extra.append((idb[:], tabs[:, 9 + kt - 2*i, :], [idb, tabs]))
                unit(qc, ksT, slice(kt*128, (kt+1)*128), extra, vs, kt, O["s"], kt == 0, kt == nk - 1)
            kts = [kt for kt in range(2*i - 4, 2*i + 2) if kt >= 0]
            for kt in kts:
                extra = [(idb[:], tabs[:, 11 + kt - (2*i - 4), :], [idb, tabs])]
                unit(qc, kwT, slice(kt*128, (kt+1)*128), extra, vw, kt, O["w"], kt == kts[0], kt == kts[-1])
            for bi, b in enumerate(("c", "s", "w")):
                op("act", lambda e: e.copy(out=osb[0:65, :], in_=O[b][0:65, :]), reads=[O[b]], writes=[osb])
                for g in range(4):
                    op("pe", lambda e: e.transpose(out=M1[:, 128 + g*65:128 + (g+1)*65], in_=osb[0:65, g*128:(g+1)*128], identity=idf[0:65, 0:65]),
                       reads=[osb, idf], writes=[M1])
                ov = M1[:, 128:128 + 260].rearrange("p (g d) -> p g d", g=4)
                op("dve", lambda e: e.tensor_scalar_max(out=zz[:], in0=ov[:, :, 64], scalar1=1e-30), reads=[M1], writes=[zz])
                op("dve", lambda e: e.reciprocal(out=zz[:], in_=zz[:]), reads=[zz], writes=[zz])
                gv = gate[:, i*12:(i+1)*12].rearrange("p (g b) -> p g b", b=3)
                op("dve", lambda e: e.tensor_tensor(out=wgt[:], in0=zz[:], in1=gv[:, :, bi], op=ALU.mult), reads=[zz, gate], writes=[wgt])
                for g in range(4):
                    if bi == 0:
                        op("dve", lambda e: e.tensor_scalar(out=ac[:, g*64:(g+1)*64], in0=ov[:, g, 0:64], scalar1=wgt[:, g:g+1], scalar2=None, op0=ALU.mult),
                           reads=[M1, wgt], writes=[ac])
                    else:
                        op("dve", lambda e: e.scalar_tensor_tensor(out=ac[:, g*64:(g+1)*64], in0=ov[:, g, 0:64], scalar=wgt[:, g:g+1],
                                                                   in1=ac[:, g*64:(g+1)*64], op0=ALU.mult, op1=ALU.add), reads=[M1, wgt, ac], writes=[ac])
            fw.dma("sp", out_d[:, i, :], ac[:], key=ac, reads=[ac])
        fw.wait_all("sp", acc)
    print("nsa instr", fw.n_inst)
    return nc

def nsa_inputs(h, cmp_pos, cmp_w1, cmp_w2, gate_b):
    sp = np.cumsum((256, 256, 512, 128, 128, 128, 128, 128, 128, 24, 256, 256, 256, 256))
    nq = h[..., sp[1]:sp[2]]; kc = h[..., sp[2]:sp[3]]; vc = h[..., sp[3]:sp[4]]; ks = h[..., sp[4]:sp[5]]
    vs = h[..., sp[5]:sp[6]]; kw = h[..., sp[6]:sp[7]]; vw = h[..., sp[7]:sp[8]]; ng = h[..., sp[8]:sp[9]]
    maps = []
    tb = [nsa_tables(0), nsa_tables(1)]
    for core in range(8):
        b, hk, p = core // 4, (core // 2) % 2, core % 2
        m = dict(tb[p])
        hs = slice(hk*64, (hk+1)*64)
        q = nq[b].reshape(32, 2, 128, 8, 64)[:, p, :, hk*4:(hk+1)*4, :]
        m["qT"] = np.ascontiguousarray(q.transpose(3, 0, 2, 1).reshape(64, 32*512))
        for nm, a in (("ksT", ks), ("kwT", kw), ("kcT", kc), ("vcT", vc)):
            m[nm] = np.ascontiguousarray(a[b][:, hs].T)
        for nm, a in (("vs", vs), ("vw", vw)):
            m[nm] = np.ascontiguousarray(a[b][:, hs].reshape(64, 128, 64).transpose(1, 0, 2))
        for j, nm in enumerate(("k", "v")):
            m["w1" + nm] = np.ascontiguousarray(cmp_w1[j].reshape(32, 64, 128).transpose(1, 0, 2).reshape(64, 32*128))
            m["w2" + nm] = np.ascontiguousarray(cmp_w2[j])
            m["pos" + nm] = np.ascontiguousarray(cmp_pos[j].T)
        g = ng[b].reshape(32, 2, 128, 8, 3)[:, p, :, hk*4:(hk+1)*4, :]
        m["ng"] = np.ascontiguousarray(g.transpose(1, 0, 2, 3).reshape(128, 32*12))
        gb = gate_b.reshape(8, 3)[hk*4:(hk+1)*4].reshape(12)
        m["gb"] = np.ascontiguousarray(np.broadcast_to(np.tile(gb, 32)[None, :], (128, 32*12)))
        maps.append(m)
    return maps

def nsa_gather(res):
    yb = np.zeros((2, 8192, 512), np.float32)
    v = yb.reshape(2, 32, 2, 128, 2, 256)
    for core in range(8):
        b, hk, p = core // 4, (core // 2) % 2, core % 2
        v[b, :, p, :, hk, :] = res[core]["out"].transpose(1, 0, 2)
    return yb


def hgrn_consts():
    s = np.arange(128)[:, None]; t = np.arange(128)[None, :]
    same = (s // 64) == (t // 64)
    mid = (t // 64) * 64 + 31
    LT = (same & (s <= t)).astype(np.float32)
    LR = (same & (s <= mid)).astype(np.float32)
    LU = (same & (s > t)).astype(np.float32)
    ind = np.stack([(np.arange(128) < 64), (np.arange(128) >= 64)], 1).astype(np.float32)
    return dict(LT=LT, LD=LT - LR, LU=LU, ind=ind, cmask=LT.copy(), idb=np.eye(128, dtype=np.float32).astype(ml_dtypes.bfloat16))

def build_hgrn():
    nc = new_nc()
    q_d = din(nc, "q", [128, 64, 64]); f_d = din(nc, "f", [128, 64, 64]); i_d = din(nc, "iv", [128, 64, 64])
    h0_d = din(nc, "h0", [128, 64]); h1_d = din(nc, "h1", [128, 64]); lsel_d = din(nc, "lsel", [128, 64])
    LT_d = din(nc, "LT", [128, 128]); LD_d = din(nc, "LD", [128, 128]); LU_d = din(nc, "LU", [128, 128])
    ind_d = din(nc, "ind", [128, 2]); cm_d = din(nc, "cmask", [128, 128]); idb_d = din(nc, "idb", [128, 128], BF16)
    out_d = dout(nc, "out", [128, 64, 64])
    with ExitStack() as st:
        fw = FW(nc, st); op = fw.op
        q = fw.sb("q", [128, 64, 64]); fl = fw.sb("fl", [128, 64, 64]); iv = fw.sb("iv", [128, 64, 64]); ivb = fw.sb("ivb", [128, 64, 64], BF16)
        kk = fw.sb("kk", [128, 64, 64]); lf = fw.sb("lf", [128, 64, 64]); ob = fw.sb("ob", [128, 64, 64])
        h0 = fw.sb("h0", [128, 64]); h1 = fw.sb("h1", [128, 64]); lsel = fw.sb("lsel", [128, 64]); lb = fw.sb("lb", [128, 64]); oml = fw.sb("oml", [128, 64])
        LT = fw.sb("LT", [128, 128]); LD = fw.sb("LD", [128, 128]); LU = fw.sb("LU", [128, 128]); ind = fw.sb("ind", [128, 2])
        cm = fw.sb("cm", [128, 128]); idb = fw.sb("idb", [128, 128], BF16)
        for t_, d_ in ((q, q_d), (fl, f_d), (iv, i_d), (h0, h0_d), (h1, h1_d), (lsel, lsel_d), (LT, LT_d), (LD, LD_d), (LU, LU_d), (ind, ind_d), (cm, cm_d), (idb, idb_d)):
            fw.dma("sp", t_[:], d_, key=t_, writes=[t_])
        op("dve", lambda e: e.tensor_tensor(out=lb[:], in0=h1[:], in1=h0[:], op=ALU.subtract), reads=[h0, h1], writes=[lb])
        op("act", lambda e: e.activation(out=lb[:], in_=lb[:], func=AF.Sigmoid), reads=[lb], writes=[lb])
        op("dve", lambda e: e.tensor_tensor(out=lb[:], in0=lb[:], in1=lsel[:], op=ALU.mult), reads=[lb, lsel], writes=[lb])
        op("dve", lambda e: e.tensor_scalar(out=oml[:], in0=lb[:], scalar1=-1.0, scalar2=1.0, op0=ALU.mult, op1=ALU.add), reads=[lb], writes=[oml])
        lbb = lb[:].unsqueeze(1).to_broadcast([128, 64, 64]); omb = oml[:].unsqueeze(1).to_broadcast([128, 64, 64])
        op("act", lambda e: e.activation(out=fl[:], in_=fl[:], func=AF.Sigmoid), reads=[fl], writes=[fl])
        op("dve", lambda e: e.tensor_tensor(out=fl[:], in0=fl[:], in1=omb, op=ALU.mult), reads=[fl, oml], writes=[fl])
        op("dve", lambda e: e.tensor_tensor(out=kk[:], in0=fl[:], in1=omb, op=ALU.subtract), reads=[fl, oml], writes=[kk])
        op("dve", lambda e: e.tensor_scalar(out=kk[:], in0=kk[:], scalar1=-1.0, scalar2=None, op0=ALU.mult), reads=[kk], writes=[kk])
        op("dve", lambda e: e.tensor_tensor(out=lf[:], in0=fl[:], in1=lbb, op=ALU.add), reads=[fl, lb], writes=[lf])
        op("act", lambda e: e.activation(out=lf[:], in_=lf[:], func=AF.Ln), reads=[lf], writes=[lf])
        op("pool", lambda e: e.tensor_copy(out=ivb[:], in_=iv[:]), reads=[iv], writes=[ivb])
        CA = fw.ps("CA", [128, 512]); CB = fw.ps("CB", [128, 512]); TAp = fw.ps("TA", [128, 512]); TBp = fw.ps("TB", [128, 512])
        ATp = fw.ps("AT", [128, 512]); Op = fw.ps("O", [128, 512]); Up = fw.ps("U", [128, 512])
        TAb = TAp.t[:].bitcast(BF16); TBb = TBp.t[:].bitcast(BF16)
        e1 = fw.sb("e1", [128, 256]); e2 = fw.sb("e2", [128, 256]); e3 = fw.sb("e3", [128, 256]); e4 = fw.sb("e4", [128, 256])
        eb = fw.sb("eb", [64, 8])
        qt = fw.sb("qt", [128, 256], BF16); kt = fw.sb("kt", [128, 256], BF16); qb = fw.sb("qb", [128, 256], BF16); kd = fw.sb("kd", [128, 256], BF16)
        qkT = fw.sb("qkT", [64, 1024], BF16); qbTA = fw.sb("qbTA", [64, 512], BF16); qbTB = fw.sb("qbTB", [64, 512], BF16)
        att = fw.sb("att", [128, 128], BF16)
        Sf = fw.sb("Sf", [64, 64]); Sb = fw.sb("Sb", [64, 64], BF16)
        op("dve", lambda e: e.memset(Sf[:], 0.0), writes=[Sf]); op("dve", lambda e: e.memset(Sb[:], 0.0), writes=[Sb])
        op("dve", lambda e: e.memset(qbTA[:], 0.0), writes=[qbTA]); op("dve", lambda e: e.memset(qbTB[:], 0.0), writes=[qbTB])
        for g in range(16):
            for j in range(4):
                n = g * 4 + j
                op("pe", lambda e: e.matmul(CA[:, j*64:(j+1)*64], lhsT=LD[:], rhs=lf[:, n, :], start=True, stop=True), reads=[LD, lf], writes=[CA])
                op("pe", lambda e: e.matmul(CA[:, 256+j*64:256+(j+1)*64], lhsT=LT[:], rhs=lf[:, n, :], start=True, stop=True), reads=[LT, lf], writes=[CA])
                op("pe", lambda e: e.matmul(CB[:, j*64:(j+1)*64], lhsT=LU[:], rhs=lf[:, n, :], start=True, stop=True), reads=[LU, lf], writes=[CB])
                op("pe", lambda e: e.matmul(CB[0:64, 256+j*2:256+(j+1)*2], lhsT=lf[:, n, :], rhs=ind[:], start=True, stop=True), reads=[ind, lf], writes=[CB])
            op("act", lambda e: e.activation(out=e1[:], in_=CA[:, 0:256], func=AF.Exp), reads=[CA], writes=[e1])
            op("act", lambda e: e.activation(out=e2[:], in_=CA[:, 0:256], func=AF.Exp, scale=-1.0), reads=[CA], writes=[e2])
            op("act", lambda e: e.activation(out=e3[:], in_=CA[:, 256:512], func=AF.Exp), reads=[CA], writes=[e3])
            op("act", lambda e: e.activation(out=e4[:], in_=CB[:, 0:256], func=AF.Exp), reads=[CB], writes=[e4])
            op("act", lambda e: e.activation(out=eb[:], in_=CB[0:64, 256:264], func=AF.Exp), reads=[CB], writes=[eb])
            qg = q[:, g*4:(g+1)*4, :].rearrange("p a d -> p (a d)"); kg = kk[:, g*4:(g+1)*4, :].rearrange("p a d -> p (a d)")
            op("dve", lambda e: e.tensor_tensor(out=qt[:], in0=qg, in1=e1[:], op=ALU.mult), reads=[q, e1], writes=[qt])
            op("dve", lambda e: e.tensor_tensor(out=kt[:], in0=kg, in1=e2[:], op=ALU.mult), reads=[kk, e2], writes=[kt])
            op("dve", lambda e: e.tensor_tensor(out=qb[:], in0=qg, in1=e3[:], op=ALU.mult), reads=[q, e3], writes=[qb])
            op("dve", lambda e: e.tensor_tensor(out=kd[:], in0=kg, in1=e4[:], op=ALU.mult), reads=[kk, e4], writes=[kd])
            for j in range(4):
                op("pe", lambda e: e.transpose(out=TAb[0:64, j*128:(j+1)*128], in_=qt[:, j*64:(j+1)*64], identity=idb[:]), reads=[qt, idb], writes=[TAp])
                op("pe", lambda e: e.transpose(out=TAb[0:64, 512+j*128:512+(j+1)*128], in_=kt[:, j*64:(j+1)*64], identity=idb[:]), reads=[kt, idb], writes=[TAp])
                op("pe", lambda e: e.transpose(out=TBb[0:64, j*128:(j+1)*128], in_=qb[:, j*64:(j+1)*64], identity=idb[:]), reads=[qb, idb], writes=[TBp])
            op("act", lambda e: e.copy(out=qkT[:], in_=TAb[0:64, 0:1024]), reads=[TAp], writes=[qkT])
            tb3 = TBb[0:64, 0:512].rearrange("p (a t) -> p a t", a=4)
            op("dve", lambda e: e.tensor_copy(out=qbTA[:].rearrange("p (a t) -> p a t", a=4)[:, :, 0:64], in_=tb3[:, :, 0:64]), reads=[TBp], writes=[qbTA])
            op("dve", lambda e: e.tensor_copy(out=qbTB[:].rearrange("p (a t) -> p a t", a=4)[:, :, 64:128], in_=tb3[:, :, 64:128]), reads=[TBp], writes=[qbTB])
            for j in range(4):
                n = g * 4 + j
                op("pe", lambda e: e.matmul(ATp[:, 0:128], lhsT=qkT[:, 512+j*128:512+(j+1)*128], rhs=qkT[:, j*128:(j+1)*128], start=True, stop=True),
                   reads=[qkT], writes=[ATp])
                op("dve", lambda e: e.tensor_tensor(out=att[:], in0=ATp[:, 0:128], in1=cm[:], op=ALU.mult), reads=[ATp, cm], writes=[att])
                op("pe", lambda e: e.matmul(Op[:, 0:64], lhsT=att[:], rhs=ivb[:, n, :], start=True, stop=False), reads=[att, ivb], writes=[Op])
                op("pe", lambda e: e.matmul(Op[:, 0:64], lhsT=qbTA[:, j*128:(j+1)*128], rhs=Sb[:], start=False, stop=False), reads=[qbTA, Sb], writes=[Op])
                op("pe", lambda e: e.matmul(Up[0:64, 0:64], lhsT=kd[0:64, j*64:(j+1)*64], rhs=ivb[0:64, n, :], start=True, stop=True), reads=[kd, ivb], writes=[Up])
                op("dve", lambda e: e.scalar_tensor_tensor(out=Sf[:], in0=Sf[:], scalar=eb[:, 2*j:2*j+1], in1=Up[0:64, 0:64], op0=ALU.mult, op1=ALU.add),
                   reads=[Sf, eb, Up], writes=[Sf])
                op("act", lambda e: e.copy(out=Sb[:], in_=Sf[:]), reads=[Sf], writes=[Sb])
                op("pe", lambda e: e.matmul(Op[:, 0:64], lhsT=qbTB[:, j*128:(j+1)*128], rhs=Sb[:], start=False, stop=True), reads=[qbTB, Sb], writes=[Op])
                op("act", lambda e: e.copy(out=ob[:, n, :], in_=Op[:, 0:64]), reads=[Op], writes=[ob])
                op("pe", lambda e: e.matmul(Up[0:64, 64:128], lhsT=kd[64:128, j*64:(j+1)*64], rhs=ivb[64:128, n, :], start=True, stop=True), reads=[kd, ivb], writes=[Up])
                op("dve", lambda e: e.scalar_tensor_tensor(out=Sf[:], in0=Sf[:], scalar=eb[:, 2*j+1:2*j+2], in1=Up[0:64, 64:128], op0=ALU.mult, op1=ALU.add),
                   reads=[Sf, eb, Up], writes=[Sf])
                op("act", lambda e: e.copy(out=Sb[:], in_=Sf[:]), reads=[Sf], writes=[Sb])
        fw.dma("sp", out_d, ob[:], key=ob, reads=[ob])
        fw.wait_all("sp", [ob])
    print("hgrn instr", fw.n_inst)
    return nc

def hgrn_inputs(h, hg_lower, layer):
    sp = np.cumsum((256, 256, 512, 128, 128, 128, 128, 128, 128, 24, 256, 256, 256, 256))
    hq = h[..., sp[9]:sp[10]]; hf = h[..., sp[10]:sp[11]]; hi = h[..., sp[11]:sp[12]]
    c = hgrn_consts(); maps = []
    for core in range(8):
        b, hd = core // 4, core % 4
        m = dict(c)
        for nm, a in (("q", hq), ("f", hf), ("iv", hi)):
            m[nm] = np.ascontiguousarray(a[b][:, hd*64:(hd+1)*64].reshape(64, 128, 64).transpose(1, 0, 2))
        m["h0"] = np.ascontiguousarray(np.broadcast_to(hg_lower[0][hd*64:(hd+1)*64][None], (128, 64)))
        m["h1"] = np.ascontiguousarray(np.broadcast_to(hg_lower[1][hd*64:(hd+1)*64][None], (128, 64)))
        m["lsel"] = np.full((128, 64), float(layer), np.float32)
        maps.append(m)
    return maps

def hgrn_gather(res):
    yc = np.zeros((2, 8192, 256), np.float32)
    for core in range(8):
        b, hd = core // 4, core % 4
        yc[b][:, hd*64:(hd+1)*64] = res[core]["out"].transpose(1, 0, 2).reshape(8192, 64)
    return yc

ALPHA = (2.0 * 2) ** 0.25
NT = 16

def build_post():
    nc = new_nc()
    gu_d = din(nc, "gu", [2048, 256]); gv_d = din(nc, "gv", [2048, 256]); yb_d = din(nc, "yb", [2048, 512]); yc_d = din(nc, "yc", [2048, 256])
    hg_d = din(nc, "hg", [2048, 256]); vg_d = din(nc, "vgain", [128, 256]); vb_d = din(nc, "vbias", [128, 256])
    ws_d = din(nc, "wsT", [128, 4, 128]); cm_d = din(nc, "cmask", [128, 128]); bs_d = din(nc, "bsT", [128, 4]); og_d = din(nc, "ogain", [128, 1024])
    out_d = dout(nc, "out", [2048, 1024])
    with ExitStack() as st:
        fw = FW(nc, st); op = fw.op
        vg = fw.sb("vg", [128, 256]); vb = fw.sb("vb", [128, 256]); ws = fw.sb("ws", [128, 4, 128]); cm = fw.sb("cm", [128, 128])
        bs = fw.sb("bs", [128, 4]); og = fw.sb("og", [128, 1024])
        for t_, d_ in ((vg, vg_d), (vb, vb_d), (ws, ws_d), (cm, cm_d), (bs, bs_d), (og, og_d)):
            fw.dma("sp", t_[:], d_, key=t_, writes=[t_])
        op("dve", lambda e: e.tensor_tensor(out=ws[:], in0=ws[:], in1=cm[:].unsqueeze(1).to_broadcast([128, 4, 128]), op=ALU.mult), reads=[ws, cm], writes=[ws])
        Zp = [fw.ps("Z0", [128, 512]), fw.ps("Z1", [128, 512])]
        bufs = []
        for i in range(2):
            bufs.append(dict(u=fw.sb("u%d" % i, [128, 256]), v=fw.sb("v%d" % i, [128, 256]), hg=fw.sb("hg%d" % i, [128, 256]),
                             Y=fw.sb("Y%d" % i, [128, 1024]), sq=fw.sb("sq%d" % i, [128, 1024]), st=fw.sb("st%d" % i, [128, 16]), st2=fw.sb("st2%d" % i, [128, 16])))
        for t in range(NT):
            B = bufs[t % 2]; u, v, hg, Y, sq, s1, s2 = B["u"], B["v"], B["hg"], B["Y"], B["sq"], B["st"], B["st2"]
            rows = slice(t*128, (t+1)*128)
            fw.dma("sp", u[:], gu_d[rows, :], key=u, writes=[u]); fw.dma("sp", v[:], gv_d[rows, :], key=v, writes=[v])
            fw.dma("pool", hg[:], hg_d[rows, :], key=hg, writes=[hg])
            fw.dma("pool", Y[:, 256:768], yb_d[rows, :], key=Y, writes=[Y]); fw.dma("pool", Y[:, 768:1024], yc_d[rows, :], key=Y, writes=[Y])
            op("act", lambda e: e.activation(out=u[:], in_=u[:], func=AF.Gelu), reads=[u], writes=[u])
            op("act", lambda e: e.activation(out=v[:], in_=v[:], func=AF.Gelu), reads=[v], writes=[v])
            v3 = v[:].rearrange("p (g d) -> p g d", g=4)
            op("dve", lambda e: e.tensor_reduce(out=s1[:, 0:4], in_=v3, axis=AX.X, op=ALU.add), reads=[v], writes=[s1])
            op("dve", lambda e: e.tensor_scalar(out=s1[:, 0:4], in0=s1[:, 0:4], scalar1=1.0/64, scalar2=None, op0=ALU.mult), reads=[s1], writes=[s1])
            op("dve", lambda e: e.tensor_tensor(out=v3, in0=v3, in1=s1[:, 0:4].unsqueeze(2).to_broadcast([128, 4, 64]), op=ALU.subtract), reads=[v, s1], writes=[v])
            op("dve", lambda e: e.tensor_tensor(out=sq[:, 0:256], in0=v[:], in1=v[:], op=ALU.mult), reads=[v], writes=[sq])
            op("dve", lambda e: e.tensor_reduce(out=s2[:, 0:4], in_=sq[:, 0:256].rearrange("p (g d) -> p g d", g=4), axis=AX.X, op=ALU.add), reads=[sq], writes=[s2])
            op("act", lambda e: e.activation(out=s2[:, 0:4], in_=s2[:, 0:4], func=AF.Sqrt, scale=1.0/64, bias=1e-5), reads=[s2], writes=[s2])
            op("dve", lambda e: e.reciprocal(out=s2[:, 0:4], in_=s2[:, 0:4]), reads=[s2], writes=[s2])
            op("dve", lambda e: e.tensor_tensor(out=v3, in0=v3, in1=s2[:, 0:4].unsqueeze(2).to_broadcast([128, 4, 64]), op=ALU.mult), reads=[v, s2], writes=[v])
            op("dve", lambda e: e.tensor_tensor(out=v[:], in0=v[:], in1=vg[:], op=ALU.mult), reads=[v, vg], writes=[v])
            op("dve", lambda e: e.tensor_tensor(out=v[:], in0=v[:], in1=vb[:], op=ALU.add), reads=[v, vb], writes=[v])
            Z = Zp[t % 2]
            for g in range(4):
                op("pe", lambda e: e.matmul(Z[:, g*64:(g+1)*64], lhsT=ws[:, g, :], rhs=v[:, g*64:(g+1)*64], start=True, stop=True), reads=[ws, v], writes=[Z])
            for g in range(4):
                op("dve", lambda e: e.scalar_tensor_tensor(out=Y[:, g*64:(g+1)*64], in0=Z[:, g*64:(g+1)*64], scalar=bs[:, g:g+1], in1=u[:, g*64:(g+1)*64],
                                                           op0=ALU.add, op1=ALU.mult), reads=[Z, bs, u], writes=[Y])
            op("act", lambda e: e.activation(out=sq[:], in_=Y[:], func=AF.Square), reads=[Y], writes=[sq])
            op("dve", lambda e: e.tensor_reduce(out=s1[:], in_=sq[:].rearrange("p (h d) -> p h d", h=16), axis=AX.X, op=ALU.add), reads=[sq], writes=[s1])
            op("act", lambda e: e.activation(out=s1[:], in_=s1[:], func=AF.Sqrt, scale=1.0/64, bias=1e-6), reads=[s1], writes=[s1])
            op("dve", lambda e: e.reciprocal(out=s1[:], in_=s1[:]), reads=[s1], writes=[s1])
            Y3 = Y[:].rearrange("p (h d) -> p h d", h=16)
            op("dve", lambda e: e.tensor_tensor(out=Y3, in0=Y3, in1=s1[:].unsqueeze(2).to_broadcast([128, 16, 64]), op=ALU.mult), reads=[Y, s1], writes=[Y])
            op("dve", lambda e: e.tensor_tensor(out=Y[:], in0=Y[:], in1=og[:], op=ALU.mult), reads=[Y, og], writes=[Y])
            op("act", lambda e: e.activation(out=hg[:], in_=hg[:], func=AF.Silu), reads=[hg], writes=[hg])
            op("dve", lambda e: e.tensor_tensor(out=Y[:, 768:1024], in0=Y[:, 768:1024], in1=hg[:], op=ALU.mult), reads=[Y, hg], writes=[Y])
            fw.dma("sp", out_d[rows, :], Y[:], key=Y, reads=[Y])
        fw.wait_all("sp", [b["Y"] for b in bufs])
    print("post instr", fw.n_inst)
    return nc

def build_addln():
    nc = new_nc()
    a_d = din(nc, "a", [2048, 1024]); b_d = din(nc, "b", [2048, 1024]); g_d = din(nc, "g", [128, 1024]); be_d = din(nc, "beta", [128, 1024])
    out_d = dout(nc, "out", [2048, 1024])
    with ExitStack() as st:
        fw = FW(nc, st); op = fw.op
        g = fw.sb("g", [128, 1024]); be = fw.sb("be", [128, 1024])
        fw.dma("sp", g[:], g_d, key=g, writes=[g]); fw.dma("sp", be[:], be_d, key=be, writes=[be])
        bufs = [dict(a=fw.sb("a%d" % i, [128, 1024]), b=fw.sb("b%d" % i, [128, 1024]), s=fw.sb("s%d" % i, [128, 12]), mv=fw.sb("mv%d" % i, [128, 2])) for i in range(2)]
        for t in range(NT):
            B = bufs[t % 2]; a, b, s, mv = B["a"], B["b"], B["s"], B["mv"]
            rows = slice(t*128, (t+1)*128)
            fw.dma("sp", a[:], a_d[rows, :], key=a, writes=[a]); fw.dma("pool", b[:], b_d[rows, :], key=b, writes=[b])
            op("dve", lambda e: e.scalar_tensor_tensor(out=a[:], in0=a[:], scalar=ALPHA, in1=b[:], op0=ALU.mult, op1=ALU.add), reads=[a, b], writes=[a])
            op("dve", lambda e: e.bn_stats(out=s[:, 0:6], in_=a[:, 0:512]), reads=[a], writes=[s])
            op("dve", lambda e: e.bn_stats(out=s[:, 6:12], in_=a[:, 512:1024]), reads=[a], writes=[s])
            op("dve", lambda e: e.bn_aggr(out=mv[:], in_=s[:]), reads=[s], writes=[mv])
            op("act", lambda e: e.activation(out=mv[:, 1:2], in_=mv[:, 1:2], func=AF.Sqrt, bias=1e-5), reads=[mv], writes=[mv])
            op("dve", lambda e: e.reciprocal(out=mv[:, 1:2], in_=mv[:, 1:2]), reads=[mv], writes=[mv])
            op("dve", lambda e: e.tensor_scalar(out=a[:], in0=a[:], scalar1=mv[:, 0:1], scalar2=mv[:, 1:2], op0=ALU.subtract, op1=ALU.mult), reads=[a, mv], writes=[a])
            op("dve", lambda e: e.tensor_tensor(out=a[:], in0=a[:], in1=g[:], op=ALU.mult), reads=[a, g], writes=[a])
            op("dve", lambda e: e.tensor_tensor(out=a[:], in0=a[:], in1=be[:], op=ALU.add), reads=[a, be], writes=[a])
            fw.dma("sp", out_d[rows, :], a[:], key=a, reads=[a])
        fw.wait_all("sp", [b["a"] for b in bufs])
    print("addln instr", fw.n_inst)
    return nc

def build_router():
    nc = new_nc()
    x_d = din(nc, "xT", [1024, 2048]); w_d = din(nc, "wr", [1024, 20]); b_d = din(nc, "br", [128, 20]); out_d = dout(nc, "gate", [128, NT, 16])
    BIG = 1e9
    with ExitStack() as st:
        fw = FW(nc, st); op = fw.op
        xs = fw.sb("xs", [128, 8, 2048]); ws = fw.sb("ws", [128, 8, 20]); br = fw.sb("br", [128, 20])
        for k in range(8):
            fw.dma("sp", xs[:, k, :], x_d[k*128:(k+1)*128, :], key=xs, writes=[xs])
        fw.dma("pool", ws[:], w_d.rearrange("(k p) n -> p k n", p=128), key=ws, writes=[ws]); fw.dma("pool", br[:], b_d, key=br, writes=[br])
        P = fw.ps("P", [128, 512])
        for t in range(NT):
            for k in range(8):
                op("pe", lambda e: e.matmul(P[:, t*20:(t+1)*20], lhsT=xs[:, k, t*128:(t+1)*128], rhs=ws[:, k, :], start=(k == 0), stop=(k == 7)), reads=[xs, ws], writes=[P])
        L = fw.sb("L", [128, NT, 20]); gm = fw.sb("gm", [128, NT]); oh = fw.sb("oh", [128, NT, 4]); eg = fw.sb("eg", [128, NT, 4]); zg = fw.sb("zg", [128, NT])
        le = fw.sb("le", [128, NT, 16]); m1 = fw.sb("m1", [128, NT]); k1 = fw.sb("k1", [128, NT, 16]); le2 = fw.sb("le2", [128, NT, 16]); m2 = fw.sb("m2", [128, NT])
        k2 = fw.sb("k2", [128, NT, 16]); w1 = fw.sb("w1", [128, NT]); w2 = fw.sb("w2", [128, NT]); gt = fw.sb("gt", [128, NT, 16])
        op("dve", lambda e: e.tensor_tensor(out=L[:], in0=P[:, 0:NT*20].rearrange("p (t n) -> p t n", n=20), in1=br[:].unsqueeze(1).to_broadcast([128, NT, 20]), op=ALU.add), reads=[P, br], writes=[L])
        lg = L[:, :, 0:4]
        op("dve", lambda e: e.tensor_reduce(out=gm[:], in_=lg, axis=AX.X, op=ALU.max), reads=[L], writes=[gm])
        gmb = gm[:].unsqueeze(2).to_broadcast([128, NT, 4])
        op("dve", lambda e: e.tensor_tensor(out=oh[:], in0=lg, in1=gmb, op=ALU.is_equal), reads=[L, gm], writes=[oh])
        op("dve", lambda e: e.tensor_tensor(out=eg[:], in0=lg, in1=gmb, op=ALU.subtract), reads=[L, gm], writes=[eg])
        op("act", lambda e: e.activation(out=eg[:], in_=eg[:], func=AF.Exp), reads=[eg], writes=[eg])
        op("dve", lambda e: e.tensor_reduce(out=zg[:], in_=eg[:], axis=AX.X, op=ALU.add), reads=[eg], writes=[zg])
        op("dve", lambda e: e.reciprocal(out=zg[:], in_=zg[:]), reads=[zg], writes=[zg])
        op("dve", lambda e: e.tensor_scalar(out=oh[:], in0=oh[:], scalar1=-1.0, scalar2=BIG, op0=ALU.add, op1=ALU.mult), reads=[oh], writes=[oh])
        le4 = le[:].rearrange("p t (g e) -> p t g e", g=4)
        op("dve", lambda e: e.tensor_tensor(out=le4, in0=L[:, :, 4:20].rearrange("p t (g e) -> p t g e", g=4), in1=oh[:].unsqueeze(3).to_broadcast([128, NT, 4, 4]), op=ALU.add),
           reads=[L, oh], writes=[le])
        op("dve", lambda e: e.tensor_reduce(out=m1[:], in_=le[:], axis=AX.X, op=ALU.max), reads=[le], writes=[m1])
        op("dve", lambda e: e.tensor_tensor(out=k1[:], in0=le[:], in1=m1[:].unsqueeze(2).to_broadcast([128, NT, 16]), op=ALU.is_equal), reads=[le, m1], writes=[k1])
        op("dve", lambda e: e.scalar_tensor_tensor(out=le2[:], in0=k1[:], scalar=-BIG, in1=le[:], op0=ALU.mult, op1=ALU.add), reads=[k1, le], writes=[le2])
        op("dve", lambda e: e.tensor_reduce(out=m2[:], in_=le2[:], axis=AX.X, op=ALU.max), reads=[le2], writes=[m2])
        op("dve", lambda e: e.tensor_tensor(out=k2[:], in0=le2[:], in1=m2[:].unsqueeze(2).to_broadcast([128, NT, 16]), op=ALU.is_equal), reads=[le2, m2], writes=[k2])
        op("dve", lambda e: e.tensor_tensor(out=w1[:], in0=m2[:], in1=m1[:], op=ALU.subtract), reads=[m1, m2], writes=[w1])
        op("act", lambda e: e.activation(out=w1[:], in_=w1[:], func=AF.Exp), reads=[w1], writes=[w1])
        op("dve", lambda e: e.tensor_scalar_add(out=w1[:], in0=w1[:], scalar1=1.0), reads=[w1], writes=[w1])
        op("dve", lambda e: e.reciprocal(out=w1[:], in_=w1[:]), reads=[w1], writes=[w1])
        op("dve", lambda e: e.tensor_scalar(out=w2[:], in0=w1[:], scalar1=-1.0, scalar2=1.0, op0=ALU.mult, op1=ALU.add), reads=[w1], writes=[w2])
        op("dve", lambda e: e.tensor_tensor(out=w1[:], in0=w1[:], in1=zg[:], op=ALU.mult), reads=[w1, zg], writes=[w1])
        op("dve", lambda e: e.tensor_tensor(out=w2[:], in0=w2[:], in1=zg[:], op=ALU.mult), reads=[w2, zg], writes=[w2])
        op("dve", lambda e: e.tensor_tensor(out=k1[:], in0=k1[:], in1=w1[:].unsqueeze(2).to_broadcast([128, NT, 16]), op=ALU.mult), reads=[k1, w1], writes=[k1])
        op("dve", lambda e: e.tensor_tensor(out=k2[:], in0=k2[:], in1=w2[:].unsqueeze(2).to_broadcast([128, NT, 16]), op=ALU.mult), reads=[k2, w2], writes=[k2])
        op("dve", lambda e: e.tensor_tensor(out=gt[:], in0=k1[:], in1=k2[:], op=ALU.add), reads=[k1, k2], writes=[gt])
        fw.dma("sp", out_d, gt[:], key=gt, reads=[gt])
        fw.wait_all("sp", [gt])
    print("router instr", fw.n_inst)
    return nc

def build_moe():
    nc = new_nc()
    x_d = din(nc, "xT", [1024, 2048]); gT_d = din(nc, "gateT", [16, 2048]); sel_d = din(nc, "sel", [16, 16 * 128])
    wg_d = din(nc, "wg", [16, 1024, 512]); wu_d = din(nc, "wu", [16, 1024, 512]); wd_d = din(nc, "wd", [16, 512, 1024])
    out_d = dout(nc, "out", [2048, 1024])
    with ExitStack() as st:
        fw = FW(nc, st); op = fw.op
        xb = fw.sb("xb", [128, 8, 2048], BF16); gT = fw.sb("gT", [16, 2048]); sel = fw.sb("sel", [16, 2048])
        acc = fw.sb("acc", [128, NT, 1024])
        stg = [fw.sb("stg%d" % i, [128, 4096]) for i in range(2)]
        W = [dict(g=fw.sb("wg%d" % i, [128, 8, 512], BF16), u=fw.sb("wu%d" % i, [128, 8, 512], BF16), d=fw.sb("wd%d" % i, [128, 4, 1024], BF16)) for i in range(2)]
        hT = [fw.sb("hT%d" % i, [128, 4, 512], BF16) for i in range(2)]
        sg = [fw.sb("sg%d" % i, [128, 512]) for i in range(2)]
        fw.dma("pool", gT[:], gT_d, key=gT, writes=[gT]); fw.dma("pool", sel[:], sel_d, key=sel, writes=[sel])
        ci = [0]
        def load_cast(dst_ap, dst_t, src_ap, n):
            s = stg[ci[0] % 2]; e = ("pool", "dve")[ci[0] % 2]; ci[0] += 1
            fw.dma("sp", s[:, 0:n], src_ap, key=s, writes=[s])
            op(e, lambda en: en.tensor_copy(out=dst_ap, in_=s[:, 0:n]), reads=[s], writes=[dst_t])
        for k in range(8):
            load_cast(xb[:, k, :], xb, x_d[k*128:(k+1)*128, :], 2048)
        Gp = [fw.ps("G0", [128, 512]), fw.ps("G1", [128, 512])]; Up = [fw.ps("U0", [128, 512]), fw.ps("U1", [128, 512])]
        Bp = fw.ps("Bc", [128, 512]); Dp = [fw.ps("D0", [128, 512]), fw.ps("D1", [128, 512])]
        def load_w(e):
            Wb = W[e % 2]
            load_cast(Wb["g"][:].rearrange("p k n -> p (k n)"), Wb["g"], wg_d[e].rearrange("(k p) n -> p k n", p=128), 4096)
            load_cast(Wb["u"][:].rearrange("p k n -> p (k n)"), Wb["u"], wu_d[e].rearrange("(k p) n -> p k n", p=128), 4096)
            load_cast(Wb["d"][:].rearrange("p k n -> p (k n)"), Wb["d"], wd_d[e].rearrange("(k p) n -> p k n", p=128), 4096)
        load_w(0)
        cnt = 0; dc = 0
        for e_ in range(16):
            if e_ + 1 < 16:
                load_w(e_ + 1)
            Wb = W[e_ % 2]
            for tg in range(4):
                ts_ = slice(tg*512, (tg+1)*512)
                op("pe", lambda e: e.matmul(Bp[:, :], lhsT=sel[:, e_*128:(e_+1)*128], rhs=gT[:, ts_], start=True, stop=True), reads=[sel, gT], writes=[Bp])
                h = hT[(e_*4 + tg) % 2]
                for hc in range(4):
                    G = Gp[cnt % 2]; U = Up[cnt % 2]; s_ = sg[cnt % 2]; cnt += 1
                    for k in range(8):
                        op("pe", lambda e: e.matmul(G[:, :], lhsT=Wb["g"][:, k, hc*128:(hc+1)*128], rhs=xb[:, k, ts_], start=(k == 0), stop=(k == 7)), reads=[Wb["g"], xb], writes=[G])
                    for k in range(8):
                        op("pe", lambda e: e.matmul(U[:, :], lhsT=Wb["u"][:, k, hc*128:(hc+1)*128], rhs=xb[:, k, ts_], start=(k == 0), stop=(k == 7)), reads=[Wb["u"], xb], writes=[U])
                    op("act", lambda e: e.activation(out=s_[:], in_=G[:, :], func=AF.Silu), reads=[G], writes=[s_])
                    op("dve", lambda e: e.tensor_tensor(out=s_[:], in0=s_[:], in1=U[:, :], op=ALU.mult), reads=[s_, U], writes=[s_])
                    op("dve", lambda e: e.tensor_tensor(out=h[:, hc, :], in0=s_[:], in1=Bp[:, :], op=ALU.mult), reads=[s_, Bp], writes=[h])
                for tt in range(4):
                    t = tg * 4 + tt
                    for ch in range(2):
                        D = Dp[dc % 2]; dc += 1
                        for hc in range(4):
                            op("pe", lambda e: e.matmul(D[:, :], lhsT=h[:, hc, tt*128:(tt+1)*128], rhs=Wb["d"][:, hc, ch*512:(ch+1)*512], start=(hc == 0), stop=(hc == 3)),
                               reads=[h, Wb["d"]], writes=[D])
                        if e_ == 0:
                            op("act", lambda e: e.copy(out=acc[:, t, ch*512:(ch+1)*512], in_=D[:, :]), reads=[D], writes=[acc])
                        else:
                            op("dve", lambda e: e.tensor_tensor(out=acc[:, t, ch*512:(ch+1)*512], in0=acc[:, t, ch*512:(ch+1)*512], in1=D[:, :], op=ALU.add), reads=[acc, D], writes=[acc])
        fw.dma("sp", out_d.rearrange("(t p) n -> p t n", p=128), acc[:], key=acc, reads=[acc])
        fw.wait_all("sp", [acc])
    print("moe instr", fw.n_inst)
    return nc


_PROGS = {}
def _prog(name, fn, *a):
    if name not in _PROGS:
        _PROGS[name] = fn(*a)
    return _PROGS[name]

def _bc(v, n=128):
    return np.ascontiguousarray(np.broadcast_to(np.asarray(v)[None, :], (n, v.shape[0])))

SPLITS = np.cumsum((256, 256, 512, 128, 128, 128, 128, 128, 128, 24, 256, 256, 256, 256))

def kernel(x, w_in, gm_v_gain, gm_v_bias, gm_w_s, gm_b_s, cmp_pos, cmp_w1, cmp_w2, nsa_gate_b,
           hg_lower, out_gain, w_out, ln1_g, ln1_b, router_group_w, router_group_b,
           router_expert_w, router_expert_b, exp_w_gate, exp_w_up, exp_w_down, ln2_g, ln2_b):
    A = lambda a: np.asarray(a, dtype=np.float32)
    x = A(x); w_in = A(w_in); hg_lower = A(hg_lower)
    cur = x.reshape(8, 2048, 1024)
    cmask = (np.arange(128)[:, None] <= np.arange(128)[None, :]).astype(np.float32)
    sel = np.zeros((16, 16 * 128), np.float32)
    for e in range(16):
        sel[e, e*128:(e+1)*128] = 1
    for l in range(2):
        nc = _prog("mm_in", build_mm, 1024, 2840, 2048)
        res = run(nc, [{"xT": np.ascontiguousarray(cur[c].T), "w": A(w_in[l])} for c in range(8)])
        h8 = np.stack([res[c]["out"] for c in range(8)])
        h = h8.reshape(2, 8192, 2840)
        nc = _prog("nsa", build_nsa)
        yb = nsa_gather(run(nc, nsa_inputs(h, A(cmp_pos[l]), A(cmp_w1[l]), A(cmp_w2[l]), A(nsa_gate_b[l])))).reshape(8, 2048, 512)
        nc = _prog("hgrn", build_hgrn)
        yc = hgrn_gather(run(nc, hgrn_inputs(h, hg_lower, l))).reshape(8, 2048, 256)
        nc = _prog("post", build_post)
        maps = [dict(gu=np.ascontiguousarray(h8[c][:, 0:256]), gv=np.ascontiguousarray(h8[c][:, 256:512]), yb=yb[c], yc=yc[c],
                     hg=np.ascontiguousarray(h8[c][:, SPLITS[12]:SPLITS[13]]), vgain=_bc(A(gm_v_gain[l])), vbias=_bc(A(gm_v_bias[l])),
                     wsT=np.ascontiguousarray(A(gm_w_s[l]).transpose(2, 0, 1)), cmask=cmask,
                     bsT=np.ascontiguousarray(A(gm_b_s[l]).T), ogain=_bc(A(out_gain[l]))) for c in range(8)]
        res = run(nc, maps)
        nc = _prog("mm_out", build_mm, 1024, 1024, 2048)
        res = run(nc, [{"xT": np.ascontiguousarray(res[c]["out"].T), "w": A(w_out[l])} for c in range(8)])
        nc = _prog("addln", build_addln)
        res = run(nc, [dict(a=cur[c], b=res[c]["out"], g=_bc(A(ln1_g[l])), beta=_bc(A(ln1_b[l]))) for c in range(8)])
        x1 = np.stack([res[c]["out"] for c in range(8)])
        xT = [np.ascontiguousarray(x1[c].T) for c in range(8)]
        wr = np.ascontiguousarray(np.concatenate([A(router_group_w[l]), A(router_expert_w[l])], 1))
        br = _bc(np.concatenate([A(router_group_b[l]), A(router_expert_b[l])]))
        nc = _prog("router", build_router)
        res = run(nc, [dict(xT=xT[c], wr=wr, br=br) for c in range(8)])
        gateT = [np.ascontiguousarray(res[c]["gate"].transpose(1, 0, 2).reshape(2048, 16).T) for c in range(8)]
        nc = _prog("moe", build_moe)
        res = run(nc, [dict(xT=xT[c], gateT=gateT[c], sel=sel, wg=A(exp_w_gate[l]), wu=A(exp_w_up[l]), wd=A(exp_w_down[l])) for c in range(8)])
        nc = _prog("addln", build_addln)
        res = run(nc, [dict(a=x1[c], b=res[c]["out"], g=_bc(A(ln2_g[l])), beta=_bc(A(ln2_b[l]))) for c in range(8)])
        cur = np.stack([res[c]["out"] for c in range(8)])
    return cur.reshape(2, 8192, 1024).astype(np.float32)
```

```python
import numpy as np
import ml_dtypes
from contextlib import ExitStack
import concourse.bass as bass
import concourse.mybir as mybir
from concourse.bass_utils import run_bass_kernel_spmd

F32 = mybir.dt.float32
BF16 = mybir.dt.bfloat16
AF = mybir.ActivationFunctionType
ALU = mybir.AluOpType
AX = mybir.AxisListType
ALPHA = (2.0 * 2) ** 0.25
NEGB = -30000.0
SEQ = 8192
class T:
    __slots__ = ("name", "t", "last_w", "reads", "dsem", "dcount")

    def __init__(self, name, t=None):
        self.name = name
        self.t = t
        self.last_w = None
        self.reads = {}
        self.dsem = None
        self.dcount = 0

    def __getitem__(self, idx):
        return self.t[idx]


class FW:
    NPOOL = 90

    def __init__(self, nc, stack):
        self.nc = nc
        self.gstack = stack
        self.stack = stack
        self.eng = {"pe": nc.tensor, "dve": nc.vector, "act": nc.scalar, "pool": nc.gpsimd, "sp": nc.sync}
        self.sems = {}
        self.cnt = {}
        self.waited = {k: {} for k in self.eng}
        for k in self.eng:
            self.sems[k] = stack.enter_context(nc.semaphore("s_" + k))
            self.cnt[k] = 0
        self.free = []
        for i in range(self.NPOOL):
            k = "d%d" % i
            self.sems[k] = stack.enter_context(nc.semaphore("s_" + k))
            self.cnt[k] = 0
            self.free.append(k)
        self.used = []
        self.n_inst = 0
        self.uid = 0

    def sb(self, name, shape, dt=F32):
        self.uid += 1
        t = self.stack.enter_context(self.nc.sbuf_tensor("sb%d_%s" % (self.uid, name), list(shape), dt))
        return T(name, t)

    def ps(self, name, shape, dt=F32):
        self.uid += 1
        t = self.stack.enter_context(self.nc.psum_tensor("ps%d_%s" % (self.uid, name), list(shape), dt))
        return T(name, t)

    def _deps(self, reads, writes):
        deps = {}
        def add(tok):
            if tok is None:
                return
            k, c = tok
            if deps.get(k, 0) < c:
                deps[k] = c
        for t in reads:
            add(t.last_w)
        for t in writes:
            add(t.last_w)
            for k, c in t.reads.items():
                add((k, c))
        return deps

    def _wait(self, e, deps):
        eng = self.eng[e]
        w = self.waited[e]
        for k, c in deps.items():
            if k == e and e == "pe":
                continue
            if w.get(k, 0) >= c:
                continue
            eng.wait_ge(self.sems[k], c)
            w[k] = c
            self.n_inst += 1

    def op(self, e, fn, reads=(), writes=()):
        self._wait(e, self._deps(reads, writes))
        inst = fn(self.eng[e])
        self.cnt[e] += 1
        inst.then_inc(self.sems[e], 1)
        tok = (e, self.cnt[e])
        for t in reads:
            if t.reads.get(e, 0) < tok[1]:
                t.reads[e] = tok[1]
        for t in writes:
            t.last_w = tok
            t.reads = {}
        self.n_inst += 1
        return inst

    def dma(self, q, out, in_, key, reads=(), writes=(), **kw):
        self._wait(q, self._deps(reads, writes))
        if key.dsem is None:
            key.dsem = self.free.pop()
            self.used.append(key.dsem)
        inst = self.eng[q].dma_start(out=out, in_=in_, **kw)
        self.cnt[key.dsem] += 16
        inst.then_inc(self.sems[key.dsem], 16)
        tok = (key.dsem, self.cnt[key.dsem])
        for t in reads:
            if t.reads.get(tok[0], 0) < tok[1]:
                t.reads[tok[0]] = tok[1]
        for t in writes:
            t.last_w = tok
            t.reads = {}
        self.n_inst += 1
        return inst

    def barrier(self, recycle=True):
        sp = self.eng["sp"]
        w = self.waited["sp"]
        for k, c in self.cnt.items():
            if k == "sp" or c == 0 or w.get(k, 0) >= c:
                continue
            sp.wait_ge(self.sems[k], c)
            w[k] = c
            self.n_inst += 1
        self.cnt["sp"] += 1
        sp.nop().then_inc(self.sems["sp"], 1)
        for e in ("pe", "dve", "act", "pool"):
            self.eng[e].wait_ge(self.sems["sp"], self.cnt["sp"])
            self.n_inst += 1
        for e in self.eng:
            for k, c in self.cnt.items():
                self.waited[e][k] = c
        if recycle:
            self.free.extend(self.used)
            self.used = []

STAGE_LIMIT = [None]
STAGE_COUNT = [0]
STAGE_ONLY = [None]

def stage(fw, fn, *a, **kw):
    STAGE_COUNT[0] += 1
    if STAGE_LIMIT[0] is not None and STAGE_COUNT[0] > STAGE_LIMIT[0]:
        return
    if STAGE_ONLY[0] is not None and STAGE_COUNT[0] not in STAGE_ONLY[0]:
        return
    with ExitStack() as st2:
        fw.stack = st2
        fn(fw, *a, **kw)
        fw.barrier()
    fw.stack = fw.gstack


def emit_mm(fw, x_ap, w_ap, out_ap, K, N, NT, idf_ap, hT_ap=None, hT_blocks=()):
    op = fw.op
    KC = K // 128
    ws = fw.sb("ws", [128, KC, N], BF16); idf = fw.sb("idf", [128, 128])
    wst = [fw.sb("wst%d" % i, [128, N]) for i in range(2)]
    fw.dma("pool", idf[:], idf_ap, key=idf, writes=[idf])
    for k in range(KC):
        wt = wst[k % 2]
        fw.dma("pool", wt[:], w_ap[k*128:(k+1)*128, :], key=wt, writes=[wt])
        if k % 2 == 0:
            op("pool", lambda e: e.tensor_copy(out=ws[:, k, :], in_=wt[:]), reads=[wt], writes=[ws])
        else:
            op("dve", lambda e: e.tensor_copy(out=ws[:, k, :], in_=wt[:]), reads=[wt], writes=[ws])
    pss = [fw.ps("ps%d" % i, [128, 512]) for i in range(4)]
    TP = [fw.ps("tp%d" % i, [128, 512]) for i in range(2)]
    HP = [fw.ps("hp%d" % i, [128, 512]) for i in range(2)]
    xt = [fw.sb("xt%d" % i, [128, K]) for i in range(2)]
    xs = [fw.sb("xs%d" % i, [128, KC, 128], BF16) for i in range(2)]
    obs = [fw.sb("ob%d" % i, [128, N]) for i in range(2)]
    hts = [fw.sb("hts%d" % i, [128, 8, 128], BF16) for i in range(2)] if hT_ap is not None else None
    cols = [(c0, min(512, N - c0)) for c0 in range(0, N, 512)]
    j = 0
    for t in range(NT):
        a = xt[t % 2]; s = xs[t % 2]; ob = obs[t % 2]
        fw.dma("sp", a[:], x_ap[t*128:(t+1)*128, :], key=a, writes=[a])
        for k in range(KC):
            P = TP[(k // 4) % 2]
            op("pe", lambda e: e.transpose(out=P[:, (k % 4)*128:(k % 4 + 1)*128], in_=a[:, k*128:(k+1)*128], identity=idf[:]), reads=[a, idf], writes=[P])
            if k % 4 == 3:
                k0 = k - 3
                eng = "act" if (k // 4) % 2 == 0 else "dve"
                if eng == "act":
                    op("act", lambda e: e.copy(out=s[:, k0:k0+4, :], in_=P[:, :].rearrange("p (k t) -> p k t", k=4)), reads=[P], writes=[s])
                else:
                    op("dve", lambda e: e.tensor_copy(out=s[:, k0:k0+4, :], in_=P[:, :].rearrange("p (k t) -> p k t", k=4)), reads=[P], writes=[s])
        for (c0, cn) in cols:
            ps = pss[j % 4]
            for k in range(KC):
                op("pe", lambda e: e.matmul(ps[:, 0:cn], lhsT=s[:, k, :], rhs=ws[:, k, c0:c0+cn], start=(k == 0), stop=(k == KC-1)), reads=[s, ws], writes=[ps])
            if j % 2 == 0:
                op("act", lambda e: e.copy(out=ob[:, c0:c0+cn], in_=ps[:, 0:cn]), reads=[ps], writes=[ob])
            else:
                op("dve", lambda e: e.tensor_copy(out=ob[:, c0:c0+cn], in_=ps[:, 0:cn]), reads=[ps], writes=[ob])
            j += 1
        fw.dma("sp", out_ap[t*128:(t+1)*128, :], ob[:], key=ob, reads=[ob])
        if hT_ap is not None:
            ht = hts[t % 2]
            for bi, cb in enumerate(hT_blocks):
                P = HP[(bi // 4) % 2]
                op("pe", lambda e: e.transpose(out=P[:, (bi % 4)*128:(bi % 4 + 1)*128], in_=ob[:, cb:cb+128], identity=idf[:]), reads=[ob, idf], writes=[P])
                if bi % 4 == 3:
                    b0 = bi - 3
                    op("dve", lambda e: e.tensor_copy(out=ht[:, b0:b0+4, :], in_=P[:, :].rearrange("p (k t) -> p k t", k=4)), reads=[P], writes=[ht])
            fw.dma("pool", hT_ap[:, t*128:(t+1)*128].rearrange("(b p) t -> p b t", p=128), ht[:], key=ht, reads=[ht])


def emit_nsa(fw, nc, A):
    NBLK = 32
    op = fw.op
    qT = fw.sb("qT", [128, NBLK * 512], BF16); qT2 = fw.sb("qT2", [128, NBLK * 512], BF16)
    ksT = fw.sb("ksT", [128, 8192], BF16); kwT = fw.sb("kwT", [64, 8192], BF16)
    qm = [T("qm%d" % i) for i in range(NBLK)]
    vs = fw.sb("vs", [128, 64, 65], BF16); vw = fw.sb("vw", [128, 64, 65], BF16)
    tabs = fw.sb("tabs", [128, 17, 512], BF16); ovl = fw.sb("ovl", [128, 4, 128], BF16)
    G = fw.sb("G", [128, 256])
    idb = fw.sb("idb", [128, 128], BF16); idf = fw.sb("idf", [128, 128])
    gate = fw.sb("gate", [128, NBLK * 12]); gbt = fw.sb("gbt", [128, NBLK * 12])
    kcmpT = fw.sb("kcmpT", [64, 512], BF16); vcmp = fw.sb("vcmp", [128, 4, 65], BF16)
    ones = fw.sb("ones", [128, 128])
    stgs = [fw.sb("stg", [128, 4096]), fw.sb("stg2", [128, 4096])]
    S = [fw.ps("S0", [128, 512]), fw.ps("S1", [128, 512]), fw.ps("S2", [128, 512])]
    NS = 3
    O = {b: fw.ps("O" + b, [128, 512]) for b in ("c", "s", "w")}
    M0 = fw.ps("M0", [128, 512]); M1 = fw.ps("M1", [128, 512]); M2 = M1
    cast_i = [0]

    def load_cast(dst_ap, src_aps, parts, n, dst_t):
        stg = stgs[cast_i[0] % 2]
        for (sl, sap) in src_aps:
            fw.dma(("sp", "pool")[cast_i[0] % 2], sl(stg), sap, key=stg, writes=[stg])
        e = ("dve", "act", "pool")[cast_i[0] % 3]; cast_i[0] += 1
        if e == "act":
            op(e, lambda en: en.copy(out=dst_ap, in_=stg[0:parts, 0:n]), reads=[stg], writes=[dst_t])
        else:
            op(e, lambda en: en.tensor_copy(out=dst_ap, in_=stg[0:parts, 0:n]), reads=[stg], writes=[dst_t])

    def whole(parts, n):
        return lambda s: s[0:parts, 0:n]

    for t_, d_ in ((tabs, A["tabs"]), (ovl, A["ovl"]), (G, A["G"]), (idb, A["idb"]), (idf, A["idf"]), (gbt, A["gb"])):
        fw.dma("pool", t_[:], d_, key=t_, writes=[t_])
    fw.dma("pool", gate[:].rearrange("q (i c) -> q i c", c=12), A["ng"], key=gate, writes=[gate])
    op("dve", lambda e: e.memset(ones[:], 1.0), writes=[ones])
    op("dve", lambda e: e.memset(vs[:, :, 64:65], 1.0), writes=[vs])
    op("dve", lambda e: e.memset(vw[:, :, 64:65], 1.0), writes=[vw])
    op("dve", lambda e: e.memset(vcmp[:, :, 64:65], 1.0), writes=[vcmp])
    op("dve", lambda e: e.memset(kcmpT[:], 0.0), writes=[kcmpT])
    op("dve", lambda e: e.memset(vcmp[:, :, 0:64], 0.0), writes=[vcmp])
    op("dve", lambda e: e.tensor_tensor(out=gate[:], in0=gate[:], in1=gbt[:], op=ALU.add), reads=[gate, gbt], writes=[gate])
    op("act", lambda e: e.activation(out=gate[:], in_=gate[:], func=AF.Sigmoid), reads=[gate], writes=[gate])
    dq = [0]
    def dload(dst_ap, src_ap, dst_t):
        q = ("sp", "pool")[dq[0] % 2]; dq[0] += 1
        fw.dma(q, dst_ap, src_ap, key=dst_t, writes=[dst_t])
    for c in range(NBLK * 512 // 4096):
        for g in range(4):
            dload(qT[0:64, c*4096:(c+1)*4096].rearrange("d (i g q) -> d i g q", i=8, g=4)[:, :, g, :], A["qT"](c, g), qT)
            if c >= 2:
                dload(qT2[0:64, c*4096:(c+1)*4096].rearrange("d (i g q) -> d i g q", i=8, g=4)[:, :, g, :], A["qT"](c, g), qT2)
    for c in range(2):
        dload(ksT[0:64, c*4096:(c+1)*4096], A["ksT"][:, c*4096:(c+1)*4096], ksT)
        dload(ksT[64:128, c*4096:(c+1)*4096], A["E"][:, c*4096:(c+1)*4096], ksT)
        dload(kwT[:, c*4096:(c+1)*4096], A["kwT"][:, c*4096:(c+1)*4096], kwT)
    load_cast(vs[:, :, 0:64], [((lambda s: s[:, 0:4096].rearrange("p (k d) -> p k d", d=64)), A["vs"])], 128, 4096, vs)
    load_cast(vw[:, :, 0:64], [((lambda s: s[:, 0:4096].rearrange("p (k d) -> p k d", d=64)), A["vw"])], 128, 4096, vw)

    with ExitStack() as st3:
        old = fw.stack; fw.stack = st3
        kT = fw.sb("kT", [64, 8192], BF16); w1 = fw.sb("w1", [64, 32 * 128], BF16)
        w2 = fw.sb("w2", [128, 64], BF16); w2f = fw.sb("w2f", [128, 64]); pos = fw.sb("pos", [64, 32], BF16); posf = fw.sb("posf", [64, 32])
        bia = fw.sb("bia", [128, 1]); g1 = fw.sb("g1", [128, 512], BF16)
        fw.stack = old
        for which in range(2):
            kd, w1d, w2d, pd = ((A["kcT"], A["w1k"], A["w2k"], A["posk"]), (A["vcT"], A["w1v"], A["w2v"], A["posv"]))[which]
            for c in range(2):
                dload(kT[:, c*4096:(c+1)*4096], kd[:, c*4096:(c+1)*4096], kT)
            load_cast(w1[:], [(whole(64, 4096), w1d)], 64, 4096, w1)
            fw.dma("sp", w2f[:], w2d, key=w2f, writes=[w2f])
            op("dve", lambda e: e.tensor_copy(out=w2[:], in_=w2f[:]), reads=[w2f], writes=[w2])
            fw.dma("sp", posf[:], pd, key=posf, writes=[posf])
            op("dve", lambda e: e.tensor_copy(out=pos[:], in_=posf[:]), reads=[posf], writes=[pos])
            for l in range(32):
                op("pe", lambda e: e.matmul(M0[:, 0:511], lhsT=w1[:, l*128:(l+1)*128], rhs=kT[:, l:l+16*510+1:16],
                                            start=(l == 0), stop=(l == 31)), reads=[w1, kT], writes=[M0])
            for l in range(32):
                op("pe", lambda e: e.matmul(M1[:, 0:1], lhsT=w1[:, l*128:(l+1)*128], rhs=pos[:, l:l+1],
                                            start=(l == 0), stop=(l == 31)), reads=[w1, pos], writes=[M1])
            op("dve", lambda e: e.tensor_copy(out=bia[:], in_=M1[:, 0:1]), reads=[M1], writes=[bia])
            op("dve", lambda e: e.memset(g1[:], 0.0), writes=[g1])
            op("act", lambda e: e.activation(out=g1[:, 0:511], in_=M0[:, 0:511], func=AF.Gelu, bias=bia[:, 0:1]), reads=[M0, bia], writes=[g1])
            if which == 0:
                op("pe", lambda e: e.matmul(M2[0:64, 0:512], lhsT=w2[:], rhs=g1[:], start=True, stop=True), reads=[w2, g1], writes=[M2])
                op("dve", lambda e: e.tensor_copy(out=kcmpT[:], in_=M2[0:64, 0:512]), reads=[M2], writes=[kcmpT])
            else:
                for m in range(4):
                    op("pe", lambda e: e.matmul(M2[:, m*64:(m+1)*64], lhsT=g1[:, m*128:(m+1)*128], rhs=w2[:], start=True, stop=True),
                       reads=[w2, g1], writes=[M2])
                op("dve", lambda e: e.tensor_copy(out=vcmp[:, :, 0:64], in_=M2[:, 0:256].rearrange("p (m d) -> p m d", m=4)),
                   reads=[M2], writes=[vcmp])
        fw.barrier(recycle=False)
    eall = fw.sb("eall", [128, 4, 512], BF16); pn = fw.sb("pn", [128, 4, 512]); pg = fw.sb("pg", [128, 4, 128], BF16)
    PT = [fw.sb("PT0", [128, 512], BF16), fw.sb("PT1", [128, 512], BF16), fw.sb("PT2", [128, 512], BF16)]
    rz = fw.sb("rz", [128, 512]); adj = fw.sb("adj", [128, 128]); adj2 = fw.sb("adj2", [128, 128])
    v8 = fw.sb("v8", [128, 16]); thr = fw.sb("thr", [128, 1]); NM = fw.sb("NM", [128, 128], BF16)
    nmT = fw.sb("nmT", [128, 4, 128], BF16); NMs = fw.sb("NMs", [128, 128], BF16)
    osb = fw.sb("osb", [128, 512]); zz = fw.sb("zz", [128, 4]); wgt = fw.sb("wgt", [128, 4])
    acc = [fw.sb("acc0", [128, 256]), fw.sb("acc1", [128, 256])]
    ucount = [0]
    pcount = [0]
    nmTs = [nmT, fw.sb("nmT1", [128, 4, 128], BF16)]
    ocs = [fw.sb("ocs0", [65, 512]), fw.sb("ocs1", [65, 512])]

    def unit_a(U):
        qc, extra = U["qc"], U["extra"]
        u = ucount[0]; ucount[0] += 1
        pc = pcount[0]; pcount[0] += 1
        Sp = S[u % NS]; Pt = PT[pc % 3]
        op("pe", lambda e: e.matmul(Sp[:, :], lhsT=U["lh"], rhs=qc, start=True, stop=(len(extra) == 0)),
           reads=U["rd"], writes=[Sp])
        for xi, (lh, rh, rd) in enumerate(extra):
            op("pe", lambda e: e.matmul(Sp[:, :], lhsT=lh, rhs=rh, start=False, stop=(xi == len(extra) - 1)), reads=rd, writes=[Sp])
        op("act", lambda e: e.activation(out=Pt[:], in_=Sp[:], func=AF.Exp, scale=0.125), reads=[Sp], writes=[Pt])
        U["Pt"] = Pt

    def unit_b(U):
        Pt = U["Pt"]; Vt = U["V"]; Ot = U["O"]
        op("pe", lambda e: e.matmul(Ot[0:65, :], lhsT=Vt[:, U["kt"], :], rhs=Pt[:], start=U["first"], stop=U["last"]), reads=[Vt, Pt], writes=[Ot])

    def chain_steps(i):
        qc = qT[0:64, i*512:(i+1)*512]
        nmt = nmTs[i % 2]; oc = ocs[i % 2]
        ms = [m for m in range(4) if i - 8 * m >= 0]
        nm_ = len(ms)
        steps = []

        def cmp_unit(mi, m):
            k = i - 8 * m
            u = ucount[0]; ucount[0] += 1
            Sp = S[u % NS]
            nb = k <= 8
            op("pe", lambda e: e.matmul(Sp[:, :], lhsT=kcmpT[:, m*128:(m+1)*128], rhs=qc, start=True, stop=(not nb)), reads=[kcmpT, qT], writes=[Sp])
            if nb:
                op("pe", lambda e: e.matmul(Sp[:, :], lhsT=idb[:], rhs=tabs[:, k, :], start=False, stop=True), reads=[idb, tabs], writes=[Sp])
            op("act", lambda e: e.activation(out=eall[:, m, :], in_=Sp[:], func=AF.Exp, scale=0.125), reads=[Sp], writes=[eall])
            op("pe", lambda e: e.matmul(O["c"][0:65, :], lhsT=vcmp[:, m, :], rhs=eall[:, m, :], start=(mi == 0), stop=(mi == nm_ - 1)),
               reads=[vcmp, eall], writes=[O["c"]])
        for mi, m in enumerate(ms):
            steps.append(lambda mi=mi, m=m: cmp_unit(mi, m))

        def s1():
            op("dve", lambda e: e.tensor_copy(out=oc[:, :], in_=O["c"][0:65, :]), reads=[O["c"]], writes=[oc])
            op("dve", lambda e: e.tensor_scalar_max(out=rz[64:65, :], in0=oc[64:65, :], scalar1=1e-30), reads=[oc], writes=[rz])
            op("dve", lambda e: e.reciprocal(out=rz[64:65, :], in_=rz[64:65, :]), reads=[rz], writes=[rz])
            op("pe", lambda e: e.matmul(M0[:, :], lhsT=ones[64:65, :], rhs=rz[64:65, :], start=True, stop=True), reads=[ones, rz], writes=[M0])
        steps.append(s1)

        def s2():
            for m in ms:
                op("dve", lambda e: e.tensor_tensor(out=pn[:, m, :], in0=eall[:, m, :], in1=M0[:, :], op=ALU.mult), reads=[eall, M0], writes=[pn])
            with nc.allow_low_precision("bf16 out, fp32 internal accumulate"):
                op("dve", lambda e: e.tensor_reduce(out=pg[:, 0:nm_, :], in_=pn[:, 0:nm_, :].rearrange("p m (g q) -> p m q g", g=4),
                                                    axis=AX.X, op=ALU.add), reads=[pn], writes=[pg])
            for mi, m in enumerate(ms):
                op("pe", lambda e: e.matmul(M0[:, 0:128], lhsT=pg[:, m, :], rhs=ovl[:, m, :], start=(mi == 0), stop=(mi == nm_ - 1)),
                   reads=[pg, ovl], writes=[M0])
        steps.append(s2)

        def s3():
            op("dve", lambda e: e.tensor_tensor(out=adj[:], in0=M0[:, 0:128], in1=G[:, 128-4*i:256-4*i], op=ALU.add), reads=[M0, G], writes=[adj])
            op("dve", lambda e: e.tensor_scalar_add(out=adj[:, 0:1], in0=adj[:, 0:1], scalar1=1e4), reads=[adj], writes=[adj])
            op("dve", lambda e: e.max(out=v8[:, 0:8], in_=adj[:]), reads=[adj], writes=[v8])
            op("dve", lambda e: e.match_replace(out=adj2[:], in_to_replace=v8[:, 0:8], in_values=adj[:], imm_value=-3e38), reads=[adj, v8], writes=[adj2])
            op("dve", lambda e: e.max(out=v8[:, 8:16], in_=adj2[:]), reads=[adj2], writes=[v8])
            op("dve", lambda e: e.tensor_scalar_max(out=thr[:], in0=v8[:, 15:16], scalar1=-1e29), reads=[v8], writes=[thr])
            op("dve", lambda e: e.tensor_scalar(out=NM[:], in0=adj[:], scalar1=thr[:, 0:1], scalar2=NEGB, op0=ALU.is_lt, op1=ALU.mult),
               reads=[adj, thr], writes=[NM])
            op("pool", lambda e: e.tensor_copy(out=NMs[:, 0:64], in_=NM[:, 64:128]), reads=[NM], writes=[NMs])
            op("pool", lambda e: e.tensor_copy(out=NMs[:, 64:128], in_=NM[:, 0:64]), reads=[NM], writes=[NMs])
        steps.append(s3)

        def s4():
            Mb = M2.t[:].bitcast(BF16)
            op("pe", lambda e: e.transpose(out=Mb[:, 0:128], in_=NM[:], identity=idb[:]), reads=[NM, idb], writes=[M2])
            op("pe", lambda e: e.transpose(out=Mb[:, 128:256], in_=NMs[:], identity=idb[:]), reads=[NMs, idb], writes=[M2])
            op("dve", lambda e: e.tensor_copy(out=qT[64:128, i*512:(i+1)*512].rearrange("p (g q) -> p g q", g=4),
                                              in_=Mb[64:128, 128:256].unsqueeze(1).to_broadcast([64, 4, 128])), reads=[M2], writes=[qm[i]])
            if i >= 16:
                op("dve", lambda e: e.tensor_copy(out=qT2[64:128, i*512:(i+1)*512].rearrange("p (g q) -> p g q", g=4),
                                                  in_=Mb[64:128, 0:128].unsqueeze(1).to_broadcast([64, 4, 128])), reads=[M2], writes=[qm[i]])
        steps.append(s4)
        return steps

    def block_units(i):
        qc = qT[0:64, i*512:(i+1)*512]
        units = []
        par = A["p"]
        w_skip = 5 if par == 0 else 0
        w_bias = (0, 4) if par == 0 else (1, 5)
        kts = [kt for kt in range(2*i - 4, 2*i + 2) if kt >= 0 and (kt - (2*i - 4)) != w_skip]
        for kt in kts:
            u_ = kt - (2*i - 4)
            extra = [(idb[:], tabs[:, 11 + u_, :], [idb, tabs])] if u_ in w_bias else []
            units.append(dict(qc=qc, lh=kwT[:, kt*128:(kt+1)*128], rd=[kwT, qT], extra=extra, V=vw, kt=kt, O=O["w"], first=(kt == kts[0]), last=(kt == kts[-1])))
        nk = 2 * i + 1 if par == 0 else 2 * i + 2
        diag = 2 * i + par
        for kt in range(nk):
            extra = []
            if kt == diag:
                extra.append((idb[:], tabs[:, 9 + kt - 2*i, :], [idb, tabs]))
            qt_ = qT if kt < 32 else qT2
            units.append(dict(qc=qt_[:, i*512:(i+1)*512], lh=ksT[:, kt*128:(kt+1)*128], rd=[ksT, qt_, qm[i]], extra=extra, V=vs, kt=kt, O=O["s"],
                              first=(kt == 0), last=(kt == nk - 1)))
        return units

    def combine(i):
        ac = acc[i % 2]
        for bi, b in enumerate(("c", "s", "w")):
            if b == "c":
                src_t = ocs[i % 2]
            else:
                op("act", lambda e: e.copy(out=osb[0:65, :], in_=O[b][0:65, :]), reads=[O[b]], writes=[osb])
                src_t = osb
            for g in range(4):
                op("pe", lambda e: e.transpose(out=M1[:, 128 + g*65:128 + (g+1)*65], in_=src_t[0:65, g*128:(g+1)*128], identity=idf[0:65, 0:65]),
                   reads=[src_t, idf], writes=[M1])
            ov = M1[:, 128:128 + 260].rearrange("p (g d) -> p g d", g=4)
            op("dve", lambda e: e.tensor_scalar_max(out=zz[:], in0=ov[:, :, 64], scalar1=1e-30), reads=[M1], writes=[zz])
            op("dve", lambda e: e.reciprocal(out=zz[:], in_=zz[:]), reads=[zz], writes=[zz])
            gv = gate[:, i*12:(i+1)*12].rearrange("p (g b) -> p g b", b=3)
            op("dve", lambda e: e.tensor_tensor(out=wgt[:], in0=zz[:], in1=gv[:, :, bi], op=ALU.mult), reads=[zz, gate], writes=[wgt])
            for g in range(4):
                if bi == 0:
                    op("dve", lambda e: e.tensor_scalar(out=ac[:, g*64:(g+1)*64], in0=ov[:, g, 0:64], scalar1=wgt[:, g:g+1], scalar2=None, op0=ALU.mult),
                       reads=[M1, wgt], writes=[ac])
                else:
                    op("dve", lambda e: e.scalar_tensor_tensor(out=ac[:, g*64:(g+1)*64], in0=ov[:, g, 0:64], scalar=wgt[:, g:g+1],
                                                               in1=ac[:, g*64:(g+1)*64], op0=ALU.mult, op1=ALU.add), reads=[M1, wgt, ac], writes=[ac])
        fw.dma("sp", A["out"](i), ac[:], key=ac, reads=[ac])

    for stp in chain_steps(0):
        stp()
    for i in range(NBLK):
        units = block_units(i)
        chain = chain_steps(i + 1) if i + 1 < NBLK else []
        n = len(units); ncn = len(chain); ci = 0
        pend = []
        for idx, U in enumerate(units):
            unit_a(U)
            pend.append(U)
            if len(pend) > 2:
                unit_b(pend.pop(0))
            while ci < ncn and ci * n < (idx + 1) * ncn:
                chain[ci](); ci += 1
        for U in pend:
            unit_b(U)
        while ci < ncn:
            chain[ci](); ci += 1
        combine(i)

def emit_hgrn(fw, nc, A):
    op = fw.op
    q = fw.sb("q", [128, 64, 64]); fl = fw.sb("fl", [128, 64, 64]); iv = fw.sb("iv", [128, 64, 64]); ivb = fw.sb("ivb", [128, 64, 64], BF16)
    kk = fw.sb("kk", [128, 64, 64]); lf = fw.sb("lf", [128, 64, 64]); ob = fw.sb("ob", [128, 64, 64])
    h0 = fw.sb("h0", [128, 64]); h1 = fw.sb("h1", [128, 64]); lsel = fw.sb("lsel", [128, 64]); lb = fw.sb("lb", [128, 64]); oml = fw.sb("oml", [128, 64])
    LT = fw.sb("LT", [128, 128]); LD = fw.sb("LD", [128, 128]); LU = fw.sb("LU", [128, 128]); ind = fw.sb("ind", [128, 2])
    cm = fw.sb("cm", [128, 128]); idb = fw.sb("idb", [128, 128], BF16)
    for t_, d_ in ((q, A["q"]), (fl, A["f"]), (iv, A["iv"]), (h0, A["h0"]), (h1, A["h1"]), (lsel, A["lsel"]), (LT, A["LT"]), (LD, A["LD"]),
                   (LU, A["LU"]), (ind, A["ind"]), (cm, A["cmask"]), (idb, A["idb"])):
        fw.dma("sp", t_[:], d_, key=t_, writes=[t_])
    op("dve", lambda e: e.tensor_tensor(out=lb[:], in0=h1[:], in1=h0[:], op=ALU.subtract), reads=[h0, h1], writes=[lb])
    op("act", lambda e: e.activation(out=lb[:], in_=lb[:], func=AF.Sigmoid), reads=[lb], writes=[lb])
    op("dve", lambda e: e.tensor_tensor(out=lb[:], in0=lb[:], in1=lsel[:], op=ALU.mult), reads=[lb, lsel], writes=[lb])
    op("dve", lambda e: e.tensor_scalar(out=oml[:], in0=lb[:], scalar1=-1.0, scalar2=1.0, op0=ALU.mult, op1=ALU.add), reads=[lb], writes=[oml])
    lbb = lb[:].unsqueeze(1).to_broadcast([128, 64, 64]); omb = oml[:].unsqueeze(1).to_broadcast([128, 64, 64])
    op("act", lambda e: e.activation(out=fl[:], in_=fl[:], func=AF.Sigmoid), reads=[fl], writes=[fl])
    op("dve", lambda e: e.tensor_tensor(out=fl[:], in0=fl[:], in1=omb, op=ALU.mult), reads=[fl, oml], writes=[fl])
    op("dve", lambda e: e.tensor_tensor(out=kk[:], in0=fl[:], in1=omb, op=ALU.subtract), reads=[fl, oml], writes=[kk])
    op("dve", lambda e: e.tensor_scalar(out=kk[:], in0=kk[:], scalar1=-1.0, scalar2=None, op0=ALU.mult), reads=[kk], writes=[kk])
    op("dve", lambda e: e.tensor_tensor(out=lf[:], in0=fl[:], in1=lbb, op=ALU.add), reads=[fl, lb], writes=[lf])
    op("act", lambda e: e.activation(out=lf[:], in_=lf[:], func=AF.Ln), reads=[lf], writes=[lf])
    op("pool", lambda e: e.tensor_copy(out=ivb[:], in_=iv[:]), reads=[iv], writes=[ivb])
    CA = fw.ps("CA", [128, 512]); CB = fw.ps("CB", [128, 512]); TAp = fw.ps("TA", [128, 512]); TBp = fw.ps("TB", [128, 512])
    ATp = fw.ps("AT", [128, 512]); Op = fw.ps("O", [128, 512]); Up = fw.ps("U", [128, 512])
    TAb = TAp.t[:].bitcast(BF16); TBb = TBp.t[:].bitcast(BF16)
    e1 = fw.sb("e1", [128, 256]); e2 = fw.sb("e2", [128, 256]); e3 = fw.sb("e3", [128, 256]); e4 = fw.sb("e4", [128, 256])
    eb = fw.sb("eb", [64, 8])
    qt = fw.sb("qt", [128, 256], BF16); kt = fw.sb("kt", [128, 256], BF16); qb = fw.sb("qb", [128, 256], BF16); kd = fw.sb("kd", [128, 256], BF16)
    qkT = fw.sb("qkT", [64, 1024], BF16); qbTA = fw.sb("qbTA", [64, 512], BF16); qbTB = fw.sb("qbTB", [64, 512], BF16)
    att = fw.sb("att", [128, 128], BF16)
    Sf = fw.sb("Sf", [64, 64]); Sb = fw.sb("Sb", [64, 64], BF16)
    op("dve", lambda e: e.memset(Sf[:], 0.0), writes=[Sf]); op("dve", lambda e: e.memset(Sb[:], 0.0), writes=[Sb])
    op("dve", lambda e: e.memset(qbTA[:], 0.0), writes=[qbTA]); op("dve", lambda e: e.memset(qbTB[:], 0.0), writes=[qbTB])
    for g in range(16):
        for j in range(4):
            n = g * 4 + j
            op("pe", lambda e: e.matmul(CA[:, j*64:(j+1)*64], lhsT=LD[:], rhs=lf[:, n, :], start=True, stop=True), reads=[LD, lf], writes=[CA])
            op("pe", lambda e: e.matmul(CA[:, 256+j*64:256+(j+1)*64], lhsT=LT[:], rhs=lf[:, n, :], start=True, stop=True), reads=[LT, lf], writes=[CA])
            op("pe", lambda e: e.matmul(CB[:, j*64:(j+1)*64], lhsT=LU[:], rhs=lf[:, n, :], start=True, stop=True), reads=[LU, lf], writes=[CB])
            op("pe", lambda e: e.matmul(CB[0:64, 256+j*2:256+(j+1)*2], lhsT=lf[:, n, :], rhs=ind[:], start=True, stop=True), reads=[ind, lf], writes=[CB])
        op("act", lambda e: e.activation(out=e1[:], in_=CA[:, 0:256], func=AF.Exp), reads=[CA], writes=[e1])
        op("act", lambda e: e.activation(out=e2[:], in_=CA[:, 0:256], func=AF.Exp, scale=-1.0), reads=[CA], writes=[e2])
        op("act", lambda e: e.activation(out=e3[:], in_=CA[:, 256:512], func=AF.Exp), reads=[CA], writes=[e3])
        op("act", lambda e: e.activation(out=e4[:], in_=CB[:, 0:256], func=AF.Exp), reads=[CB], writes=[e4])
        op("act", lambda e: e.activation(out=eb[:], in_=CB[0:64, 256:264], func=AF.Exp), reads=[CB], writes=[eb])
        qg = q[:, g*4:(g+1)*4, :].rearrange("p a d -> p (a d)"); kg = kk[:, g*4:(g+1)*4, :].rearrange("p a d -> p (a d)")
        op("dve", lambda e: e.tensor_tensor(out=qt[:], in0=qg, in1=e1[:], op=ALU.mult), reads=[q, e1], writes=[qt])
        op("dve", lambda e: e.tensor_tensor(out=kt[:], in0=kg, in1=e2[:], op=ALU.mult), reads=[kk, e2], writes=[kt])
        op("dve", lambda e: e.tensor_tensor(out=qb[:], in0=qg, in1=e3[:], op=ALU.mult), reads=[q, e3], writes=[qb])
        op("dve", lambda e: e.tensor_tensor(out=kd[:], in0=kg, in1=e4[:], op=ALU.mult), reads=[kk, e4], writes=[kd])
        for j in range(4):
            op("pe", lambda e: e.transpose(out=TAb[0:64, j*128:(j+1)*128], in_=qt[:, j*64:(j+1)*64], identity=idb[:]), reads=[qt, idb], writes=[TAp])
            op("pe", lambda e: e.transpose(out=TAb[0:64, 512+j*128:512+(j+1)*128], in_=kt[:, j*64:(j+1)*64], identity=idb[:]), reads=[kt, idb], writes=[TAp])
            op("pe", lambda e: e.transpose(out=TBb[0:64, j*128:(j+1)*128], in_=qb[:, j*64:(j+1)*64], identity=idb[:]), reads=[qb, idb], writes=[TBp])
        op("act", lambda e: e.copy(out=qkT[:], in_=TAb[0:64, 0:1024]), reads=[TAp], writes=[qkT])
        tb3 = TBb[0:64, 0:512].rearrange("p (a t) -> p a t", a=4)
        op("dve", lambda e: e.tensor_copy(out=qbTA[:].rearrange("p (a t) -> p a t", a=4)[:, :, 0:64], in_=tb3[:, :, 0:64]), reads=[TBp], writes=[qbTA])
        op("dve", lambda e: e.tensor_copy(out=qbTB[:].rearrange("p (a t) -> p a t", a=4)[:, :, 64:128], in_=tb3[:, :, 64:128]), reads=[TBp], writes=[qbTB])
        for j in range(4):
            n = g * 4 + j
            op("pe", lambda e: e.matmul(ATp[:, 0:128], lhsT=qkT[:, 512+j*128:512+(j+1)*128], rhs=qkT[:, j*128:(j+1)*128], start=True, stop=True),
               reads=[qkT], writes=[ATp])
            op("dve", lambda e: e.tensor_tensor(out=att[:], in0=ATp[:, 0:128], in1=cm[:], op=ALU.mult), reads=[ATp, cm], writes=[att])
            op("pe", lambda e: e.matmul(Op[:, 0:64], lhsT=att[:], rhs=ivb[:, n, :], start=True, stop=False), reads=[att, ivb], writes=[Op])
            op("pe", lambda e: e.matmul(Op[:, 0:64], lhsT=qbTA[:, j*128:(j+1)*128], rhs=Sb[:], start=False, stop=False), reads=[qbTA, Sb], writes=[Op])
            op("pe", lambda e: e.matmul(Up[0:64, 0:64], lhsT=kd[0:64, j*64:(j+1)*64], rhs=ivb[0:64, n, :], start=True, stop=True), reads=[kd, ivb], writes=[Up])
            op("dve", lambda e: e.scalar_tensor_tensor(out=Sf[:], in0=Sf[:], scalar=eb[:, 2*j:2*j+1], in1=Up[0:64, 0:64], op0=ALU.mult, op1=ALU.add),
               reads=[Sf, eb, Up], writes=[Sf])
            op("act", lambda e: e.copy(out=Sb[:], in_=Sf[:]), reads=[Sf], writes=[Sb])
            op("pe", lambda e: e.matmul(Op[:, 0:64], lhsT=qbTB[:, j*128:(j+1)*128], rhs=Sb[:], start=False, stop=True), reads=[qbTB, Sb], writes=[Op])
            op("act", lambda e: e.copy(out=ob[:, n, :], in_=Op[:, 0:64]), reads=[Op], writes=[ob])
            op("pe", lambda e: e.matmul(Up[0:64, 64:128], lhsT=kd[64:128, j*64:(j+1)*64], rhs=ivb[64:128, n, :], start=True, stop=True), reads=[kd, ivb], writes=[Up])
            op("dve", lambda e: e.scalar_tensor_tensor(out=Sf[:], in0=Sf[:], scalar=eb[:, 2*j+1:2*j+2], in1=Up[0:64, 64:128], op0=ALU.mult, op1=ALU.add),
               reads=[Sf, eb, Up], writes=[Sf])
            op("act", lambda e: e.copy(out=Sb[:], in_=Sf[:]), reads=[Sf], writes=[Sb])
    fw.dma("sp", A["out"], ob[:], key=ob, reads=[ob])


def emit_post(fw, nc, A, NT):
    op = fw.op
    vg = fw.sb("vg", [128, 256]); vb = fw.sb("vb", [128, 256]); ws = fw.sb("ws", [128, 4, 128]); cm = fw.sb("cm", [128, 128])
    bs = fw.sb("bs", [128, 4]); og = fw.sb("og", [128, 1024])
    for t_, d_ in ((vg, A["vgain"]), (vb, A["vbias"]), (ws, A["wsT"]), (cm, A["cmask"]), (bs, A["bsT"]), (og, A["ogain"])):
        fw.dma("sp", t_[:], d_, key=t_, writes=[t_])
    op("dve", lambda e: e.tensor_tensor(out=ws[:], in0=ws[:], in1=cm[:].unsqueeze(1).to_broadcast([128, 4, 128]), op=ALU.mult), reads=[ws, cm], writes=[ws])
    Zp = [fw.ps("Z0", [128, 512]), fw.ps("Z1", [128, 512])]
    bufs = []
    for i in range(2):
        bufs.append(dict(u=fw.sb("u%d" % i, [128, 256]), v=fw.sb("v%d" % i, [128, 256]), hg=fw.sb("hg%d" % i, [128, 256]),
                         Y=fw.sb("Y%d" % i, [128, 1024]), sq=fw.sb("sq%d" % i, [128, 1024]), st=fw.sb("st%d" % i, [128, 16]), st2=fw.sb("st2%d" % i, [128, 16])))
    for t in range(NT):
        B = bufs[t % 2]; u, v, hg, Y, sq, s1, s2 = B["u"], B["v"], B["hg"], B["Y"], B["sq"], B["st"], B["st2"]
        rows = slice(t*128, (t+1)*128)
        fw.dma("sp", u[:], A["h"][rows, 0:256], key=u, writes=[u]); fw.dma("sp", v[:], A["h"][rows, 256:512], key=v, writes=[v])
        fw.dma("pool", hg[:], A["h"][rows, 2584:2840], key=hg, writes=[hg])
        fw.dma("pool", Y[:, 256:768], A["yb"][rows, :], key=Y, writes=[Y]); fw.dma("pool", Y[:, 768:1024], A["yc"][rows, :], key=Y, writes=[Y])
        op("act", lambda e: e.activation(out=u[:], in_=u[:], func=AF.Gelu), reads=[u], writes=[u])
        op("act", lambda e: e.activation(out=v[:], in_=v[:], func=AF.Gelu), reads=[v], writes=[v])
        v3 = v[:].rearrange("p (g d) -> p g d", g=4)
        op("dve", lambda e: e.tensor_reduce(out=s1[:, 0:4], in_=v3, axis=AX.X, op=ALU.add), reads=[v], writes=[s1])
        op("dve", lambda e: e.tensor_scalar(out=s1[:, 0:4], in0=s1[:, 0:4], scalar1=1.0/64, scalar2=None, op0=ALU.mult), reads=[s1], writes=[s1])
        op("dve", lambda e: e.tensor_tensor(out=v3, in0=v3, in1=s1[:, 0:4].unsqueeze(2).to_broadcast([128, 4, 64]), op=ALU.subtract), reads=[v, s1], writes=[v])
        op("dve", lambda e: e.tensor_tensor(out=sq[:, 0:256], in0=v[:], in1=v[:], op=ALU.mult), reads=[v], writes=[sq])
        op("dve", lambda e: e.tensor_reduce(out=s2[:, 0:4], in_=sq[:, 0:256].rearrange("p (g d) -> p g d", g=4), axis=AX.X, op=ALU.add), reads=[sq], writes=[s2])
        op("act", lambda e: e.activation(out=s2[:, 0:4], in_=s2[:, 0:4], func=AF.Sqrt, scale=1.0/64, bias=1e-5), reads=[s2], writes=[s2])
        op("dve", lambda e: e.reciprocal(out=s2[:, 0:4], in_=s2[:, 0:4]), reads=[s2], writes=[s2])
        op("dve", lambda e: e.tensor_tensor(out=v3, in0=v3, in1=s2[:, 0:4].unsqueeze(2).to_broadcast([128, 4, 64]), op=ALU.mult), reads=[v, s2], writes=[v])
        op("dve", lambda e: e.tensor_tensor(out=v[:], in0=v[:], in1=vg[:], op=ALU.mult), reads=[v, vg], writes=[v])
        op("dve", lambda e: e.tensor_tensor(out=v[:], in0=v[:], in1=vb[:], op=ALU.add), reads=[v, vb], writes=[v])
        Z = Zp[t % 2]
        for g in range(4):
            op("pe", lambda e: e.matmul(Z[:, g*64:(g+1)*64], lhsT=ws[:, g, :], rhs=v[:, g*64:(g+1)*64], start=True, stop=True), reads=[ws, v], writes=[Z])
        for g in range(4):
            op("dve", lambda e: e.scalar_tensor_tensor(out=Y[:, g*64:(g+1)*64], in0=Z[:, g*64:(g+1)*64], scalar=bs[:, g:g+1], in1=u[:, g*64:(g+1)*64],
                                                       op0=ALU.add, op1=ALU.mult), reads=[Z, bs, u], writes=[Y])
        op("act", lambda e: e.activation(out=sq[:], in_=Y[:], func=AF.Square), reads=[Y], writes=[sq])
        op("dve", lambda e: e.tensor_reduce(out=s1[:], in_=sq[:].rearrange("p (h d) -> p h d", h=16), axis=AX.X, op=ALU.add), reads=[sq], writes=[s1])
        op("act", lambda e: e.activation(out=s1[:], in_=s1[:], func=AF.Sqrt, scale=1.0/64, bias=1e-6), reads=[s1], writes=[s1])
        op("dve", lambda e: e.reciprocal(out=s1[:], in_=s1[:]), reads=[s1], writes=[s1])
        Y3 = Y[:].rearrange("p (h d) -> p h d", h=16)
        op("dve", lambda e: e.tensor_tensor(out=Y3, in0=Y3, in1=s1[:].unsqueeze(2).to_broadcast([128, 16, 64]), op=ALU.mult), reads=[Y, s1], writes=[Y])
        op("dve", lambda e: e.tensor_tensor(out=Y[:], in0=Y[:], in1=og[:], op=ALU.mult), reads=[Y, og], writes=[Y])
        op("act", lambda e: e.activation(out=hg[:], in_=hg[:], func=AF.Silu), reads=[hg], writes=[hg])
        op("dve", lambda e: e.tensor_tensor(out=Y[:, 768:1024], in0=Y[:, 768:1024], in1=hg[:], op=ALU.mult), reads=[Y, hg], writes=[Y])
        fw.dma("sp", A["out"][rows, :], Y[:], key=Y, reads=[Y])


def emit_addln(fw, nc, A, NT):
    op = fw.op
    g = fw.sb("g", [128, 1024]); be = fw.sb("be", [128, 1024])
    fw.dma("sp", g[:], A["g"], key=g, writes=[g]); fw.dma("sp", be[:], A["beta"], key=be, writes=[be])
    bufs = [dict(a=fw.sb("a%d" % i, [128, 1024]), b=fw.sb("b%d" % i, [128, 1024]), s=fw.sb("s%d" % i, [128, 12]), mv=fw.sb("mv%d" % i, [128, 2])) for i in range(2)]
    for t in range(NT):
        B = bufs[t % 2]; a, b, s, mv = B["a"], B["b"], B["s"], B["mv"]
        rows = slice(t*128, (t+1)*128)
        fw.dma("sp", a[:], A["a"][rows, :], key=a, writes=[a]); fw.dma("pool", b[:], A["b"][rows, :], key=b, writes=[b])
        op("dve", lambda e: e.scalar_tensor_tensor(out=a[:], in0=a[:], scalar=ALPHA, in1=b[:], op0=ALU.mult, op1=ALU.add), reads=[a, b], writes=[a])
        op("dve", lambda e: e.bn_stats(out=s[:, 0:6], in_=a[:, 0:512]), reads=[a], writes=[s])
        op("dve", lambda e: e.bn_stats(out=s[:, 6:12], in_=a[:, 512:1024]), reads=[a], writes=[s])
        op("dve", lambda e: e.bn_aggr(out=mv[:], in_=s[:]), reads=[s], writes=[mv])
        op("act", lambda e: e.activation(out=mv[:, 1:2], in_=mv[:, 1:2], func=AF.Sqrt, bias=1e-5), reads=[mv], writes=[mv])
        op("dve", lambda e: e.reciprocal(out=mv[:, 1:2], in_=mv[:, 1:2]), reads=[mv], writes=[mv])
        op("dve", lambda e: e.tensor_scalar(out=s[:, 0:1], in0=mv[:, 0:1], scalar1=mv[:, 1:2], scalar2=-1.0, op0=ALU.mult, op1=ALU.mult), reads=[mv], writes=[s])
        op("act", lambda e: e.activation(out=b[:], in_=a[:], func=AF.Identity, scale=mv[:, 1:2], bias=s[:, 0:1]), reads=[a, mv, s], writes=[b])
        op("pool", lambda e: e.tensor_tensor(out=b[:], in0=b[:], in1=g[:], op=ALU.mult), reads=[b, g], writes=[b])
        op("dve", lambda e: e.tensor_tensor(out=b[:], in0=b[:], in1=be[:], op=ALU.add), reads=[b, be], writes=[b])
        fw.dma("sp", A["out"][rows, :], b[:], key=b, reads=[b])


MOE_DEBUG = [0]

def emit_moe(fw, nc, A):
    NT = 16
    BIG = 1e9
    op = fw.op
    xb = fw.sb("xb", [128, 8, 2048], BF16); gT = fw.sb("gT", [16, 2048]); sel = fw.sb("sel", [16, 2048])
    acc = fw.sb("acc", [128, NT, 1024])
    idf = fw.sb("idf", [128, 128]); wr = fw.sb("wr", [128, 8, 20]); br = fw.sb("br", [128, 20])
    fw.dma("pool", sel[:], A["sel"], key=sel, writes=[sel]); fw.dma("pool", idf[:], A["idf"], key=idf, writes=[idf])
    fw.dma("pool", wr[:], A["wr"].rearrange("(k p) n -> p k n", p=128), key=wr, writes=[wr]); fw.dma("pool", br[:], A["br"], key=br, writes=[br])
    Gp = [fw.ps("G0", [128, 512]), fw.ps("G1", [128, 512])]; Up = [fw.ps("U0", [128, 512]), fw.ps("U1", [128, 512])]
    Bp = fw.ps("Bc", [128, 512]); Dp = [fw.ps("D0", [128, 512]), fw.ps("D1", [128, 512])]; Rp = fw.ps("R", [128, 512])
    if MOE_DEBUG[0] == 2:
        return
    with ExitStack() as st3:
        old = fw.stack; fw.stack = st3
        xt = [fw.sb("xt%d" % i, [128, 1024]) for i in range(2)]
        xf = [fw.sb("xf%d" % i, [128, 8, 128]) for i in range(2)]
        L = fw.sb("L", [128, NT, 20]); gm = fw.sb("gm", [128, NT]); oh = fw.sb("oh", [128, NT, 4]); eg = fw.sb("eg", [128, NT, 4]); zg = fw.sb("zg", [128, NT])
        le = fw.sb("le", [128, NT, 16]); m1 = fw.sb("m1", [128, NT]); k1 = fw.sb("k1", [128, NT, 16]); le2 = fw.sb("le2", [128, NT, 16]); m2 = fw.sb("m2", [128, NT])
        k2 = fw.sb("k2", [128, NT, 16]); w1 = fw.sb("w1", [128, NT]); w2 = fw.sb("w2", [128, NT]); gt = fw.sb("gt", [128, NT, 16]); gpad = fw.sb("gpad", [128, NT, 128])
        fw.stack = old
        for t in range(NT):
            a = xt[t % 2]; f = xf[t % 2]
            fw.dma("sp", a[:], A["x"][t*128:(t+1)*128, :], key=a, writes=[a])
            for k in range(8):
                P = Gp[(k // 4) % 2]
                op("pe", lambda e: e.transpose(out=P[:, (k % 4)*128:(k % 4 + 1)*128], in_=a[:, k*128:(k+1)*128], identity=idf[:]), reads=[a, idf], writes=[P])
                if k % 4 == 3:
                    k0 = k - 3
                    op("act", lambda e: e.copy(out=f[:, k0:k0+4, :], in_=P[:, :].rearrange("p (k t) -> p k t", k=4)), reads=[P], writes=[f])
                    op("dve", lambda e: e.tensor_copy(out=xb[:, k0:k0+4, t*128:(t+1)*128], in_=f[:, k0:k0+4, :]), reads=[f], writes=[xb])
            if MOE_DEBUG[0] == 3:
                continue
            for k in range(8):
                op("pe", lambda e: e.matmul(Rp[:, t*20:(t+1)*20], lhsT=f[:, k, :], rhs=wr[:, k, :], start=(k == 0), stop=(k == 7)), reads=[f, wr], writes=[Rp])
        if MOE_DEBUG[0] in (3, 4):
            fw.barrier(recycle=False)
            return
        op("dve", lambda e: e.tensor_tensor(out=L[:], in0=Rp[:, 0:NT*20].rearrange("p (t n) -> p t n", n=20), in1=br[:].unsqueeze(1).to_broadcast([128, NT, 20]), op=ALU.add), reads=[Rp, br], writes=[L])
        lg = L[:, :, 0:4]
        op("dve", lambda e: e.tensor_reduce(out=gm[:], in_=lg, axis=AX.X, op=ALU.max), reads=[L], writes=[gm])
        gmb = gm[:].unsqueeze(2).to_broadcast([128, NT, 4])
        op("dve", lambda e: e.tensor_tensor(out=oh[:], in0=lg, in1=gmb, op=ALU.is_equal), reads=[L, gm], writes=[oh])
        op("dve", lambda e: e.tensor_tensor(out=eg[:], in0=lg, in1=gmb, op=ALU.subtract), reads=[L, gm], writes=[eg])
        op("act", lambda e: e.activation(out=eg[:], in_=eg[:], func=AF.Exp), reads=[eg], writes=[eg])
        op("dve", lambda e: e.tensor_reduce(out=zg[:], in_=eg[:], axis=AX.X, op=ALU.add), reads=[eg], writes=[zg])
        op("dve", lambda e: e.reciprocal(out=zg[:], in_=zg[:]), reads=[zg], writes=[zg])
        op("dve", lambda e: e.tensor_scalar(out=oh[:], in0=oh[:], scalar1=-1.0, scalar2=BIG, op0=ALU.add, op1=ALU.mult), reads=[oh], writes=[oh])
        le4 = le[:].rearrange("p t (g e) -> p t g e", g=4)
        op("dve", lambda e: e.tensor_tensor(out=le4, in0=L[:, :, 4:20].rearrange("p t (g e) -> p t g e", g=4), in1=oh[:].unsqueeze(3).to_broadcast([128, NT, 4, 4]), op=ALU.add),
           reads=[L, oh], writes=[le])
        op("dve", lambda e: e.tensor_reduce(out=m1[:], in_=le[:], axis=AX.X, op=ALU.max), reads=[le], writes=[m1])
        op("dve", lambda e: e.tensor_tensor(out=k1[:], in0=le[:], in1=m1[:].unsqueeze(2).to_broadcast([128, NT, 16]), op=ALU.is_equal), reads=[le, m1], writes=[k1])
        op("dve", lambda e: e.scalar_tensor_tensor(out=le2[:], in0=k1[:], scalar=-BIG, in1=le[:], op0=ALU.mult, op1=ALU.add), reads=[k1, le], writes=[le2])
        op("dve", lambda e: e.tensor_reduce(out=m2[:], in_=le2[:], axis=AX.X, op=ALU.max), reads=[le2], writes=[m2])
        op("dve", lambda e: e.tensor_tensor(out=k2[:], in0=le2[:], in1=m2[:].unsqueeze(2).to_broadcast([128, NT, 16]), op=ALU.is_equal), reads=[le2, m2], writes=[k2])
        op("dve", lambda e: e.tensor_tensor(out=w1[:], in0=m2[:], in1=m1[:], op=ALU.subtract), reads=[m1, m2], writes=[w1])
        op("act", lambda e: e.activation(out=w1[:], in_=w1[:], func=AF.Exp), reads=[w1], writes=[w1])
        op("dve", lambda e: e.tensor_scalar_add(out=w1[:], in0=w1[:], scalar1=1.0), reads=[w1], writes=[w1])
        op("dve", lambda e: e.reciprocal(out=w1[:], in_=w1[:]), reads=[w1], writes=[w1])
        op("dve", lambda e: e.tensor_scalar(out=w2[:], in0=w1[:], scalar1=-1.0, scalar2=1.0, op0=ALU.mult, op1=ALU.add), reads=[w1], writes=[w2])
        op("dve", lambda e: e.tensor_tensor(out=w1[:], in0=w1[:], in1=zg[:], op=ALU.mult), reads=[w1, zg], writes=[w1])
        op("dve", lambda e: e.tensor_tensor(out=w2[:], in0=w2[:], in1=zg[:], op=ALU.mult), reads=[w2, zg], writes=[w2])
        op("dve", lambda e: e.tensor_tensor(out=k1[:], in0=k1[:], in1=w1[:].unsqueeze(2).to_broadcast([128, NT, 16]), op=ALU.mult), reads=[k1, w1], writes=[k1])
        op("dve", lambda e: e.tensor_tensor(out=k2[:], in0=k2[:], in1=w2[:].unsqueeze(2).to_broadcast([128, NT, 16]), op=ALU.mult), reads=[k2, w2], writes=[k2])
        op("dve", lambda e: e.tensor_tensor(out=gt[:], in0=k1[:], in1=k2[:], op=ALU.add), reads=[k1, k2], writes=[gt])
        if MOE_DEBUG[0] == 5:
            fw.barrier(recycle=False)
            return
        op("pool", lambda e: e.memset(gpad[:], 0.0), writes=[gpad])
        op("dve", lambda e: e.tensor_copy(out=gpad[:, :, 0:16], in_=gt[:]), reads=[gt], writes=[gpad])
        for t in range(NT):
            op("pe", lambda e: e.transpose(out=Bp[:, (t % 4)*128:(t % 4 + 1)*128], in_=gpad[:, t, :], identity=idf[:]), reads=[gpad, idf], writes=[Bp])
            if t % 4 == 3:
                t0 = t - 3
                op("dve", lambda e: e.tensor_copy(out=gT[:, t0*128:(t0+4)*128], in_=Bp[0:16, :]), reads=[Bp], writes=[gT])
        fw.barrier(recycle=False)
    if MOE_DEBUG[0] == 1:
        return
    stg = [fw.sb("stg%d" % i, [128, 2048]) for i in range(2)]
    W = [dict(g=fw.sb("wg%d" % i, [128, 8, 512], BF16), u=fw.sb("wu%d" % i, [128, 8, 512], BF16), d=fw.sb("wd%d" % i, [128, 4, 1024], BF16)) for i in range(2)]
    hT = [fw.sb("hT%d" % i, [128, 4, 512], BF16) for i in range(2)]
    sg = [fw.sb("sg%d" % i, [128, 512]) for i in range(2)]
    ci = [0]
    def load_cast(dst_ap, dst_t, src_ap, n):
        s = stg[ci[0] % 2]; e = ("pool", "dve")[ci[0] % 2]; ci[0] += 1
        fw.dma("sp", s[:, 0:n], src_ap, key=s, writes=[s])
        op(e, lambda en: en.tensor_copy(out=dst_ap, in_=s[:, 0:n]), reads=[s], writes=[dst_t])
    def load_w(e):
        Wb = W[e % 2]
        for half in range(2):
            load_cast(Wb["g"][:, half*4:(half+1)*4, :], Wb["g"], A["wg"][e, half*512:(half+1)*512, :].rearrange("(k p) n -> p k n", p=128), 2048)
        for half in range(2):
            load_cast(Wb["u"][:, half*4:(half+1)*4, :], Wb["u"], A["wu"][e, half*512:(half+1)*512, :].rearrange("(k p) n -> p k n", p=128), 2048)
        for half in range(2):
            load_cast(Wb["d"][:, half*2:(half+1)*2, :], Wb["d"], A["wd"][e, half*256:(half+1)*256, :].rearrange("(k p) n -> p k n", p=128), 2048)
    load_w(0)
    cnt = 0; dc = 0
    for e_ in range(16):
        if e_ + 1 < 16:
            load_w(e_ + 1)
        Wb = W[e_ % 2]
        for tg in range(4):
            ts_ = slice(tg*512, (tg+1)*512)
            op("pe", lambda e: e.matmul(Bp[:, :], lhsT=sel[:, e_*128:(e_+1)*128], rhs=gT[:, ts_], start=True, stop=True), reads=[sel, gT], writes=[Bp])
            h = hT[(e_*4 + tg) % 2]
            for hc in range(4):
                G = Gp[cnt % 2]; U = Up[cnt % 2]; s_ = sg[cnt % 2]; cnt += 1
                for k in range(8):
                    op("pe", lambda e: e.matmul(G[:, :], lhsT=Wb["g"][:, k, hc*128:(hc+1)*128], rhs=xb[:, k, ts_], start=(k == 0), stop=(k == 7)), reads=[Wb["g"], xb], writes=[G])
                for k in range(8):
                    op("pe", lambda e: e.matmul(U[:, :], lhsT=Wb["u"][:, k, hc*128:(hc+1)*128], rhs=xb[:, k, ts_], start=(k == 0), stop=(k == 7)), reads=[Wb["u"], xb], writes=[U])
                op("act", lambda e: e.activation(out=s_[:], in_=G[:, :], func=AF.Silu), reads=[G], writes=[s_])
                op("dve", lambda e: e.tensor_tensor(out=s_[:], in0=s_[:], in1=U[:, :], op=ALU.mult), reads=[s_, U], writes=[s_])
                op("dve", lambda e: e.tensor_tensor(out=h[:, hc, :], in0=s_[:], in1=Bp[:, :], op=ALU.mult), reads=[s_, Bp], writes=[h])
            for tt in range(4):
                t = tg * 4 + tt
                for ch in range(2):
                    D = Dp[dc % 2]; dc += 1
                    for hc in range(4):
                        op("pe", lambda e: e.matmul(D[:, :], lhsT=h[:, hc, tt*128:(tt+1)*128], rhs=Wb["d"][:, hc, ch*512:(ch+1)*512], start=(hc == 0), stop=(hc == 3)),
                           reads=[h, Wb["d"]], writes=[D])
                    if e_ == 0:
                        op("act", lambda e: e.copy(out=acc[:, t, ch*512:(ch+1)*512], in_=D[:, :]), reads=[D], writes=[acc])
                    else:
                        op("dve", lambda e: e.tensor_tensor(out=acc[:, t, ch*512:(ch+1)*512], in0=acc[:, t, ch*512:(ch+1)*512], in1=D[:, :], op=ALU.add), reads=[acc, D], writes=[acc])
    fw.dma("sp", A["out"].rearrange("(t p) n -> p t n", p=128), acc[:], key=acc, reads=[acc])


def emit_select(fw, nc, A):
    op = fw.op
    ind = fw.sb("ind", [128, 4])
    fw.dma("sp", ind[:], A["ind"], key=ind, writes=[ind])
    pairs = A["pairs"]
    CT = sum(c for _, _, c in pairs)
    xin = [fw.sb("xin%d" % i, [128, CT]) for i in range(3)]
    acc = [fw.sb("sacc%d" % i, [128, CT]) for i in range(2)]
    n = 0
    for t in range(16):
        a = acc[t % 2]
        for q in range(4):
            xi = xin[n % 3]; n += 1
            r0 = q * 2048 + t * 128
            c0 = 0
            for pi, (src, dst, C) in enumerate(pairs):
                fw.dma(("sp", "pool")[pi % 2], xi[:, c0:c0+C], src[r0:r0+128, :], key=xi, writes=[xi])
                c0 += C
            eng = "dve" if q % 2 == 0 else "pool"
            if q == 0:
                op("dve", lambda e: e.tensor_scalar(out=a[:], in0=xi[:], scalar1=ind[:, 0:1], scalar2=None, op0=ALU.mult), reads=[xi, ind], writes=[a])
            else:
                op("dve", lambda e: e.scalar_tensor_tensor(out=a[:], in0=xi[:], scalar=ind[:, q:q+1], in1=a[:], op0=ALU.mult, op1=ALU.add), reads=[xi, ind, a], writes=[a])
        c0 = 0
        for pi, (src, dst, C) in enumerate(pairs):
            fw.dma("sp", dst[t*128:(t+1)*128, :], a[:, c0:c0+C], key=a, reads=[a])
            c0 += C

def nsa_tables(p):
    n = np.arange(128)[:, None]; q = np.arange(128)[None, :]
    tabs = np.zeros((17, 128, 128), np.float32)
    for k in range(9):
        d = 2 * k + p
        tabs[k] = np.where(16 * n + 31 <= 128 * d + q, 0.0, NEGB)
    for u in range(2):
        tabs[9 + u] = np.where(128 * (u - p) + n > q, NEGB, 0.0)
    for u in range(6):
        dl = 128 * (u - 4 - p) + n - q
        tabs[11 + u] = np.where((dl <= 0) & (dl > -512), 0.0, NEGB)
    tabs = np.broadcast_to(tabs[:, :, None, :], (17, 128, 4, 128)).transpose(1, 0, 2, 3).reshape(128, 17, 512)
    y = np.arange(256)[None, :]; qi = np.arange(128)[:, None]
    rel = (y - 2 * p) - 128; hh = (qi >= 64).astype(np.int64)
    G = np.where(rel > hh, -1e30, np.where((rel == hh) | (rel == hh - 1), 1e4, 0.0)).astype(np.float32)
    return np.ascontiguousarray(tabs).astype(ml_dtypes.bfloat16), G


def const_inputs():
    c = {}
    for p in range(2):
        c["tabs%d" % p], c["G%d" % p] = nsa_tables(p)
    ii = np.arange(512)[:, None]; jj = np.arange(128)[None, :]
    ovl = ((ii * 16 < (jj + 1) * 64) & (ii * 16 + 32 > jj * 64)).astype(np.float32)
    c["ovl"] = np.ascontiguousarray(ovl.reshape(4, 128, 128).transpose(1, 0, 2)).astype(ml_dtypes.bfloat16)
    x = np.arange(8192)[None, :]; j = np.arange(128)[:, None]
    j64 = np.arange(64)[:, None]
    c["E"] = ((x // 64) % 64 == j64).astype(np.float32).astype(ml_dtypes.bfloat16)
    c["idb"] = np.eye(128, dtype=np.float32).astype(ml_dtypes.bfloat16)
    c["idf"] = np.eye(128, dtype=np.float32)
    s = np.arange(128)[:, None]; t = np.arange(128)[None, :]
    same = (s // 64) == (t // 64)
    mid = (t // 64) * 64 + 31
    LT = (same & (s <= t)).astype(np.float32)
    LR = (same & (s <= mid)).astype(np.float32)
    c["LT"] = LT; c["LD"] = LT - LR; c["LU"] = (same & (s > t)).astype(np.float32)
    c["ind"] = np.stack([(np.arange(128) < 64), (np.arange(128) >= 64)], 1).astype(np.float32)
    c["cmaskh"] = LT.copy()
    c["cmask"] = (s <= t).astype(np.float32)
    sel = np.zeros((16, 16 * 128), np.float32)
    for e in range(16):
        sel[e, e*128:(e+1)*128] = 1
    c["sel"] = sel
    return c


CONST_SPECS = [("tabs0", [128, 17, 512], BF16), ("tabs1", [128, 17, 512], BF16), ("G0", [128, 256], F32), ("G1", [128, 256], F32),
               ("ovl", [128, 4, 128], BF16), ("E", [64, 8192], BF16), ("idb", [128, 128], BF16), ("idf", [128, 128], F32),
               ("LT", [128, 128], F32), ("LD", [128, 128], F32), ("LU", [128, 128], F32), ("ind", [128, 2], F32),
               ("cmaskh", [128, 128], F32), ("cmask", [128, 128], F32), ("sel", [16, 2048], F32)]
LAYER_SPECS = [("w_in", [1024, 2840]), ("w1k", [64, 4096]), ("w1v", [64, 4096]), ("w2k", [128, 64]), ("w2v", [128, 64]), ("posk", [64, 32]), ("posv", [64, 32]),
               ("gb_0", [128, 384]), ("gb_1", [128, 384]), ("lsel", [128, 64]), ("vgain", [128, 256]), ("vbias", [128, 256]), ("wsT", [128, 4, 128]),
               ("bsT", [128, 4]), ("ogain", [128, 1024]), ("w_out", [1024, 1024]), ("ln1g", [128, 1024]), ("ln1b", [128, 1024]), ("wr", [1024, 20]),
               ("br", [128, 20]), ("wg", [16, 1024, 512]), ("wu", [16, 1024, 512]), ("wd", [16, 512, 1024]), ("ln2g", [128, 1024]), ("ln2b", [128, 1024])]
HT_BLOCKS = (512, 640, 768, 896, 1024, 1152, 1280, 1536)
DEBUG = False
NCORES = 2
DEBUG_NAMES = ()


def build_fused():
    nc = bass.Bass("TRN2", target_bir_lowering=False)
    I = {}
    def din(name, shape, dt=F32):
        I[name] = nc.dram_tensor(name, list(shape), dt, kind="ExternalInput").ap()
    din("x", [SEQ, 1024])
    for nm, sh, dt in CONST_SPECS:
        din(nm, sh, dt)
    for hd in range(4):
        din("hgl0_%d" % hd, [128, 64]); din("hgl1_%d" % hd, [128, 64])
    for l in range(2):
        for nm, sh in LAYER_SPECS:
            din("%s%d" % (nm, l), sh)
    din("qind", [128, 4])
    out = nc.dram_tensor("out", [2048, 1024], F32, kind="ExternalOutput").ap()
    def scr(name, shape):
        return nc.dram_tensor(name, list(shape), F32, kind="Internal").ap()
    h = scr("s_h", [SEQ, 2840]); hT = nc.dram_tensor("s_hT", [1024, SEQ], BF16, kind="Internal").ap(); yb = scr("s_yb", [SEQ, 512]); yc = scr("s_yc", [SEQ, 256])
    y = scr("s_y", [SEQ, 1024]); mix = scr("s_mix", [SEQ, 1024]); x1 = scr("s_x1", [SEQ, 1024]); moe = scr("s_moe", [SEQ, 1024]); x2 = scr("s_x2", [SEQ, 1024])
    dbg = {}
    STAGE_COUNT[0] = 0
    with ExitStack() as st:
        fw = FW(nc, st)
        for l in range(2):
            P = lambda nm: I["%s%d" % (nm, l)]
            src = I["x"] if l == 0 else x2
            dst = x2
            for qt in range(4):
                r = slice(qt*2048, (qt+1)*2048)
                stage(fw, emit_mm, src[r, :], P("w_in"), h[r, :], 1024, 2840, 16, I["idf"], hT_ap=hT[:, r], hT_blocks=HT_BLOCKS)
            for hk in range(2):
                for p in range(2):
                    A = dict(tabs=I["tabs%d" % p], G=I["G%d" % p], ovl=I["ovl"], E=I["E"], idb=I["idb"], idf=I["idf"], gb=P("gb_%d" % hk),
                             w1k=P("w1k"), w1v=P("w1v"), w2k=P("w2k"), w2v=P("w2v"), posk=P("posk"), posv=P("posv"),
                             ksT=hT[768 + hk*64:768 + (hk+1)*64, :], kwT=hT[896 + hk*64:896 + (hk+1)*64, :],
                             kcT=hT[512 + hk*64:512 + (hk+1)*64, :], vcT=hT[640 + hk*64:640 + (hk+1)*64, :],
                             vs=h[:, 1408 + hk*64:1408 + (hk+1)*64].rearrange("(k p) d -> p k d", p=128),
                             vw=h[:, 1664 + hk*64:1664 + (hk+1)*64].rearrange("(k p) d -> p k d", p=128),
                             ng=h[:, 1792 + hk*12:1792 + (hk+1)*12].rearrange("(i t q) c -> q i t c", t=2, q=128)[:, :, p, :])
                    A["p"] = p
                    A["qT"] = (lambda c, g, hk=hk, p=p: hT[hk*256 + g*64:hk*256 + (g+1)*64, :].rearrange("d (i t q) -> d i t q", t=2, q=128)[:, c*8:(c+1)*8, p, :])
                    A["out"] = (lambda i, hk=hk, p=p: yb[(2*i+p)*128:(2*i+p+1)*128, hk*256:(hk+1)*256])
                    stage(fw, emit_nsa, nc, A)
            for hd in range(4):
                tm = lambda c0: h[:, c0 + hd*64:c0 + (hd+1)*64].rearrange("(k p) d -> p k d", p=128)
                A = dict(q=tm(1816), f=tm(2072), iv=tm(2328), h0=I["hgl0_%d" % hd], h1=I["hgl1_%d" % hd], lsel=P("lsel"), LT=I["LT"], LD=I["LD"], LU=I["LU"],
                         ind=I["ind"], cmask=I["cmaskh"], idb=I["idb"], out=yc[:, hd*64:(hd+1)*64].rearrange("(k p) d -> p k d", p=128))
                stage(fw, emit_hgrn, nc, A)
            if l == 1:
                hq = scr("s_hq", [2048, 2840]); ybq = scr("s_ybq", [2048, 512]); ycq = scr("s_ycq", [2048, 256]); xq = scr("s_xq", [2048, 1024])
                pairs = [(h[:, 0:512], hq[:, 0:512], 512), (h[:, 2584:2840], hq[:, 2584:2840], 256), (yb, ybq, 512), (yc, ycq, 256), (x2, xq, 1024)]
                stage(fw, emit_select, nc, dict(ind=I["qind"], pairs=pairs))
                r = slice(0, 2048)
                A = dict(h=hq, yb=ybq, yc=ycq, out=y[r, :], vgain=P("vgain"), vbias=P("vbias"), wsT=P("wsT"), cmask=I["cmask"], bsT=P("bsT"), ogain=P("ogain"))
                stage(fw, emit_post, nc, A, 16)
                stage(fw, emit_mm, y[r, :], P("w_out"), mix[r, :], 1024, 1024, 16, I["idf"])
                stage(fw, emit_addln, nc, dict(a=xq, b=mix[r, :], g=P("ln1g"), beta=P("ln1b"), out=x1[r, :]), 16)
                A = dict(x=x1[r, :], out=moe[r, :], sel=I["sel"], idf=I["idf"], wr=P("wr"), br=P("br"), wg=P("wg"), wu=P("wu"), wd=P("wd"))
                stage(fw, emit_moe, nc, A)
                stage(fw, emit_addln, nc, dict(a=x1[r, :], b=moe[r, :], g=P("ln2g"), beta=P("ln2b"), out=out), 16)
                continue
            for qt in range(4):
                r = slice(qt*2048, (qt+1)*2048)
                A = dict(h=h[r, :], yb=yb[r, :], yc=yc[r, :], out=y[r, :], vgain=P("vgain"), vbias=P("vbias"), wsT=P("wsT"), cmask=I["cmask"], bsT=P("bsT"), ogain=P("ogain"))
                stage(fw, emit_post, nc, A, 16)
            for qt in range(4):
                r = slice(qt*2048, (qt+1)*2048)
                stage(fw, emit_mm, y[r, :], P("w_out"), mix[r, :], 1024, 1024, 16, I["idf"])
            for qt in range(4):
                r = slice(qt*2048, (qt+1)*2048)
                stage(fw, emit_addln, nc, dict(a=src[r, :], b=mix[r, :], g=P("ln1g"), beta=P("ln1b"), out=x1[r, :]), 16)
            for qt in range(4):
                r = slice(qt*2048, (qt+1)*2048)
                A = dict(x=x1[r, :], out=moe[r, :], sel=I["sel"], idf=I["idf"], wr=P("wr"), br=P("br"), wg=P("wg"), wu=P("wu"), wd=P("wd"))
                stage(fw, emit_moe, nc, A)
            for qt in range(4):
                r = slice(qt*2048, (qt+1)*2048)
                stage(fw, emit_addln, nc, dict(a=x1[r, :], b=moe[r, :], g=P("ln2g"), beta=P("ln2b"), out=dst[r, :]), 16)
            if DEBUG and l == 0:
                for nm, ap in (("h", h), ("yb", yb), ("yc", yc), ("y", y), ("x1", x1), ("moe", moe)):
                    if nm not in DEBUG_NAMES:
                        continue
                    d = nc.dram_tensor("dbg_" + nm, list(ap.shape), F32, kind="ExternalOutput").ap()
                    tt = T("dbg" + nm)
                    nrow = ap.shape[0]
                    for c in range(8):
                        fw.dma("sp", d[c*nrow//8:(c+1)*nrow//8, :], ap[c*nrow//8:(c+1)*nrow//8, :], key=tt)
                fw.barrier()
        print("fused instr", fw.n_inst)
    return nc


def _bc(v, n=128):
    v = np.asarray(v)
    return np.ascontiguousarray(np.broadcast_to(v[None, :], (n, v.shape[0])))


_NC = {}

def kernel(x, w_in, gm_v_gain, gm_v_bias, gm_w_s, gm_b_s, cmp_pos, cmp_w1, cmp_w2, nsa_gate_b,
           hg_lower, out_gain, w_out, ln1_g, ln1_b, router_group_w, router_group_b,
           router_expert_w, router_expert_b, exp_w_gate, exp_w_up, exp_w_down, ln2_g, ln2_b):
    A = lambda a: np.ascontiguousarray(np.asarray(a, dtype=np.float32))
    x = A(x); hg_lower = A(hg_lower)
    m = dict(const_inputs())
    for hd in range(4):
        m["hgl0_%d" % hd] = _bc(hg_lower[0][hd*64:(hd+1)*64]); m["hgl1_%d" % hd] = _bc(hg_lower[1][hd*64:(hd+1)*64])
    for l in range(2):
        L = lambda nm, v: m.__setitem__("%s%d" % (nm, l), v)
        L("w_in", A(w_in[l]))
        for j, nm in enumerate(("k", "v")):
            L("w1" + nm, np.ascontiguousarray(A(cmp_w1[l][j]).reshape(32, 64, 128).transpose(1, 0, 2).reshape(64, 32*128)))
            L("w2" + nm, A(cmp_w2[l][j])); L("pos" + nm, np.ascontiguousarray(A(cmp_pos[l][j]).T))
        gb = A(nsa_gate_b[l]).reshape(8, 3)
        for hk in range(2):
            L("gb_%d" % hk, _bc(np.tile(gb[hk*4:(hk+1)*4].reshape(12), 32)))
        L("lsel", np.full((128, 64), float(l), np.float32))
        L("vgain", _bc(A(gm_v_gain[l]))); L("vbias", _bc(A(gm_v_bias[l])))
        L("wsT", np.ascontiguousarray(A(gm_w_s[l]).transpose(2, 0, 1))); L("bsT", np.ascontiguousarray(A(gm_b_s[l]).T))
        L("ogain", _bc(A(out_gain[l]))); L("w_out", A(w_out[l]))
        L("ln1g", _bc(A(ln1_g[l]))); L("ln1b", _bc(A(ln1_b[l]))); L("ln2g", _bc(A(ln2_g[l]))); L("ln2b", _bc(A(ln2_b[l])))
        L("wr", np.ascontiguousarray(np.concatenate([A(router_group_w[l]), A(router_expert_w[l])], 1)))
        L("br", _bc(np.concatenate([A(router_group_b[l]), A(router_expert_b[l])])))
        L("wg", A(exp_w_gate[l])); L("wu", A(exp_w_up[l])); L("wd", A(exp_w_down[l]))
    if "nc" not in _NC:
        _NC["nc"] = build_fused()
    in_maps = []
    for c in range(8):
        mm = dict(m); mm["x"] = x[c // 4]
        ind = np.zeros((128, 4), np.float32); ind[:, c % 4] = 1.0
        mm["qind"] = ind
        in_maps.append(mm)
    res = run_bass_kernel_spmd(_NC["nc"], in_maps, core_ids=list(range(8)))
    _NC["res"] = res.results
    out = np.zeros((2, SEQ, 1024), np.float32)
    for c in range(8):
        out[c // 4, (c % 4)*2048:(c % 4 + 1)*2048] = res.results[c]["out"]
    return out
```

```python
import numpy as np
import ml_dtypes
from contextlib import ExitStack
import concourse.bass as bass
import concourse.mybir as mybir
from concourse.bass_utils import run_bass_kernel_spmd

F32 = mybir.dt.float32
BF16 = mybir.dt.bfloat16
AF = mybir.ActivationFunctionType
ALU = mybir.AluOpType
AX = mybir.AxisListType
ALPHA = (2.0 * 2) ** 0.25
NEGB = -30000.0
SEQ = 8192
class T:
    __slots__ = ("name", "t", "last_w", "reads", "dsem", "dcount")

    def __init__(self, name, t=None):
        self.name = name
        self.t = t
        self.last_w = None
        self.reads = {}
        self.dsem = None
        self.dcount = 0

    def __getitem__(self, idx):
        return self.t[idx]


class FW:
    NPOOL = 90

    def __init__(self, nc, stack):
        self.nc = nc
        self.gstack = stack
        self.stack = stack
        self.eng = {"pe": nc.tensor, "dve": nc.vector, "act": nc.scalar, "pool": nc.gpsimd, "sp": nc.sync}
        self.sems = {}
        self.cnt = {}
        self.waited = {k: {} for k in self.eng}
        for k in self.eng:
            self.sems[k] = stack.enter_context(nc.semaphore("s_" + k))
            self.cnt[k] = 0
        self.free = []
        for i in range(self.NPOOL):
            k = "d%d" % i
            self.sems[k] = stack.enter_context(nc.semaphore("s_" + k))
            self.cnt[k] = 0
            self.free.append(k)
        self.used = []
        self.n_inst = 0
        self.uid = 0

    def sb(self, name, shape, dt=F32):
        self.uid += 1
        t = self.stack.enter_context(self.nc.sbuf_tensor("sb%d_%s" % (self.uid, name), list(shape), dt))
        return T(name, t)

    def ps(self, name, shape, dt=F32):
        self.uid += 1
        t = self.stack.enter_context(self.nc.psum_tensor("ps%d_%s" % (self.uid, name), list(shape), dt))
        return T(name, t)

    def _deps(self, reads, writes):
        deps = {}
        def add(tok):
            if tok is None:
                return
            k, c = tok
            if deps.get(k, 0) < c:
                deps[k] = c
        for t in reads:
            add(t.last_w)
        for t in writes:
            add(t.last_w)
            for k, c in t.reads.items():
                add((k, c))
        return deps

    def _wait(self, e, deps):
        eng = self.eng[e]
        w = self.waited[e]
        for k, c in deps.items():
            if k == e and e == "pe":
                continue
            if w.get(k, 0) >= c:
                continue
            eng.wait_ge(self.sems[k], c)
            w[k] = c
            self.n_inst += 1

    def op(self, e, fn, reads=(), writes=()):
        self._wait(e, self._deps(reads, writes))
        inst = fn(self.eng[e])
        self.cnt[e] += 1
        inst.then_inc(self.sems[e], 1)
        tok = (e, self.cnt[e])
        for t in reads:
            if t.reads.get(e, 0) < tok[1]:
                t.reads[e] = tok[1]
        for t in writes:
            t.last_w = tok
            t.reads = {}
        self.n_inst += 1
        return inst

    def dma(self, q, out, in_, key, reads=(), writes=(), **kw):
        self._wait(q, self._deps(reads, writes))
        if key.dsem is None:
            key.dsem = {}
        if q not in key.dsem:
            key.dsem[q] = self.free.pop()
            self.used.append(key.dsem[q])
        sk = key.dsem[q]
        inst = self.eng[q].dma_start(out=out, in_=in_, **kw)
        self.cnt[sk] += 16
        inst.then_inc(self.sems[sk], 16)
        tok = (sk, self.cnt[sk])
        for t in reads:
            if t.reads.get(tok[0], 0) < tok[1]:
                t.reads[tok[0]] = tok[1]
        for t in writes:
            t.last_w = tok
            t.reads = {}
        self.n_inst += 1
        return inst

    def barrier(self, recycle=True):
        sp = self.eng["sp"]
        w = self.waited["sp"]
        for k, c in self.cnt.items():
            if k == "sp" or c == 0 or w.get(k, 0) >= c:
                continue
            sp.wait_ge(self.sems[k], c)
            w[k] = c
            self.n_inst += 1
        self.cnt["sp"] += 1
        sp.nop().then_inc(self.sems["sp"], 1)
        for e in ("pe", "dve", "act", "pool"):
            self.eng[e].wait_ge(self.sems["sp"], self.cnt["sp"])
            self.n_inst += 1
        for e in self.eng:
            for k, c in self.cnt.items():
                self.waited[e][k] = c
        if recycle:
            self.free.extend(self.used)
            self.used = []

STAGE_LIMIT = [None]
STAGE_COUNT = [0]
STAGE_ONLY = [None]

def stage(fw, fn, *a, **kw):
    STAGE_COUNT[0] += 1
    if STAGE_LIMIT[0] is not None and STAGE_COUNT[0] > STAGE_LIMIT[0]:
        return
    if STAGE_ONLY[0] is not None and STAGE_COUNT[0] not in STAGE_ONLY[0]:
        return
    with ExitStack() as st2:
        fw.stack = st2
        fn(fw, *a, **kw)
        fw.barrier()
    fw.stack = fw.gstack


def emit_mm(fw, x_ap, w_ap, out_ap, K, N, NT, idf_ap, hT_ap=None, hT_blocks=()):
    op = fw.op
    KC = K // 128
    ws = fw.sb("ws", [128, KC, N], BF16); idf = fw.sb("idf", [128, 128])
    wst = [fw.sb("wst%d" % i, [128, N]) for i in range(2)]
    fw.dma("pool", idf[:], idf_ap, key=idf, writes=[idf])
    for k in range(KC):
        wt = wst[k % 2]
        fw.dma("pool", wt[:], w_ap[k*128:(k+1)*128, :], key=wt, writes=[wt])
        if k % 2 == 0:
            op("pool", lambda e: e.tensor_copy(out=ws[:, k, :], in_=wt[:]), reads=[wt], writes=[ws])
        else:
            op("dve", lambda e: e.tensor_copy(out=ws[:, k, :], in_=wt[:]), reads=[wt], writes=[ws])
    pss = [fw.ps("ps%d" % i, [128, 512]) for i in range(4)]
    TP = [fw.ps("tp%d" % i, [128, 512]) for i in range(2)]
    HP = [fw.ps("hp%d" % i, [128, 512]) for i in range(2)]
    xt = [fw.sb("xt%d" % i, [128, K]) for i in range(2)]
    xs = [fw.sb("xs%d" % i, [128, KC, 128], BF16) for i in range(2)]
    obs = [fw.sb("ob%d" % i, [128, N]) for i in range(2)]
    hts = [fw.sb("hts%d" % i, [128, 8, 128], BF16) for i in range(2)] if hT_ap is not None else None
    cols = [(c0, min(512, N - c0)) for c0 in range(0, N, 512)]
    j = 0
    for t in range(NT):
        a = xt[t % 2]; s = xs[t % 2]; ob = obs[t % 2]
        fw.dma("sp", a[:], x_ap[t*128:(t+1)*128, :], key=a, writes=[a])
        for k in range(KC):
            P = TP[(k // 4) % 2]
            op("pe", lambda e: e.transpose(out=P[:, (k % 4)*128:(k % 4 + 1)*128], in_=a[:, k*128:(k+1)*128], identity=idf[:]), reads=[a, idf], writes=[P])
            if k % 4 == 3:
                k0 = k - 3
                eng = "act" if (k // 4) % 2 == 0 else "dve"
                if eng == "act":
                    op("act", lambda e: e.copy(out=s[:, k0:k0+4, :], in_=P[:, :].rearrange("p (k t) -> p k t", k=4)), reads=[P], writes=[s])
                else:
                    op("dve", lambda e: e.tensor_copy(out=s[:, k0:k0+4, :], in_=P[:, :].rearrange("p (k t) -> p k t", k=4)), reads=[P], writes=[s])
        for (c0, cn) in cols:
            ps = pss[j % 4]
            for k in range(KC):
                op("pe", lambda e: e.matmul(ps[:, 0:cn], lhsT=s[:, k, :], rhs=ws[:, k, c0:c0+cn], start=(k == 0), stop=(k == KC-1)), reads=[s, ws], writes=[ps])
            if j % 2 == 0:
                op("act", lambda e: e.copy(out=ob[:, c0:c0+cn], in_=ps[:, 0:cn]), reads=[ps], writes=[ob])
            else:
                op("dve", lambda e: e.tensor_copy(out=ob[:, c0:c0+cn], in_=ps[:, 0:cn]), reads=[ps], writes=[ob])
            j += 1
        fw.dma("sp", out_ap[t*128:(t+1)*128, :], ob[:], key=ob, reads=[ob])
        if hT_ap is not None:
            ht = hts[t % 2]
            for bi, cb in enumerate(hT_blocks):
                P = HP[(bi // 4) % 2]
                op("pe", lambda e: e.transpose(out=P[:, (bi % 4)*128:(bi % 4 + 1)*128], in_=ob[:, cb:cb+128], identity=idf[:]), reads=[ob, idf], writes=[P])
                if bi % 4 == 3:
                    b0 = bi - 3
                    op("dve", lambda e: e.tensor_copy(out=ht[:, b0:b0+4, :], in_=P[:, :].rearrange("p (k t) -> p k t", k=4)), reads=[P], writes=[ht])
            fw.dma("pool", hT_ap[:, t*128:(t+1)*128].rearrange("(b p) t -> p b t", p=128), ht[:], key=ht, reads=[ht])


def emit_nsa(fw, nc, A):
    NBLK = 32
    op = fw.op
    qT = fw.sb("qT", [128, NBLK * 512], BF16); qT2 = fw.sb("qT2", [128, NBLK * 512], BF16)
    ksT = fw.sb("ksT", [128, 8192], BF16); kwT = fw.sb("kwT", [64, 8192], BF16)
    qm = [T("qm%d" % i) for i in range(NBLK)]
    vs = fw.sb("vs", [128, 64, 65], BF16); vw = fw.sb("vw", [128, 64, 65], BF16)
    tabs = fw.sb("tabs", [128, 17, 512], BF16); ovl = fw.sb("ovl", [128, 4, 128], BF16)
    G = fw.sb("G", [128, 256])
    idb = fw.sb("idb", [128, 128], BF16); idf = fw.sb("idf", [128, 128])
    gate = fw.sb("gate", [128, NBLK * 12]); gbt = fw.sb("gbt", [128, NBLK * 12])
    kcmpT = fw.sb("kcmpT", [64, 512], BF16); vcmp = fw.sb("vcmp", [128, 4, 65], BF16)
    ones = fw.sb("ones", [128, 128])
    stgs = [fw.sb("stg", [128, 4096]), fw.sb("stg2", [128, 4096])]
    S = [fw.ps("S0", [128, 512]), fw.ps("S1", [128, 512]), fw.ps("S2", [128, 512])]
    NS = 3
    O = {b: fw.ps("O" + b, [128, 512]) for b in ("c", "s", "w")}
    M0 = fw.ps("M0", [128, 512]); M1 = fw.ps("M1", [128, 512]); M2 = M1
    cast_i = [0]

    def load_cast(dst_ap, src_aps, parts, n, dst_t):
        stg = stgs[cast_i[0] % 2]
        for (sl, sap) in src_aps:
            fw.dma(("sp", "pool")[cast_i[0] % 2], sl(stg), sap, key=stg, writes=[stg])
        e = ("dve", "act", "pool")[cast_i[0] % 3]; cast_i[0] += 1
        if e == "act":
            op(e, lambda en: en.copy(out=dst_ap, in_=stg[0:parts, 0:n]), reads=[stg], writes=[dst_t])
        else:
            op(e, lambda en: en.tensor_copy(out=dst_ap, in_=stg[0:parts, 0:n]), reads=[stg], writes=[dst_t])

    def whole(parts, n):
        return lambda s: s[0:parts, 0:n]

    for t_, d_ in ((tabs, A["tabs"]), (ovl, A["ovl"]), (G, A["G"]), (idb, A["idb"]), (idf, A["idf"]), (gbt, A["gb"])):
        fw.dma("pool", t_[:], d_, key=t_, writes=[t_])
    fw.dma("pool", gate[:].rearrange("q (i c) -> q i c", c=12), A["ng"], key=gate, writes=[gate])
    op("dve", lambda e: e.memset(ones[:], 1.0), writes=[ones])
    op("dve", lambda e: e.memset(vs[:, :, 64:65], 1.0), writes=[vs])
    op("dve", lambda e: e.memset(vw[:, :, 64:65], 1.0), writes=[vw])
    op("dve", lambda e: e.memset(vcmp[:, :, 64:65], 1.0), writes=[vcmp])
    op("dve", lambda e: e.memset(kcmpT[:], 0.0), writes=[kcmpT])
    op("dve", lambda e: e.memset(vcmp[:, :, 0:64], 0.0), writes=[vcmp])
    op("dve", lambda e: e.tensor_tensor(out=gate[:], in0=gate[:], in1=gbt[:], op=ALU.add), reads=[gate, gbt], writes=[gate])
    op("act", lambda e: e.activation(out=gate[:], in_=gate[:], func=AF.Sigmoid), reads=[gate], writes=[gate])
    dq = [0]
    def dload(dst_ap, src_ap, dst_t):
        q = ("sp", "pool")[dq[0] % 2]; dq[0] += 1
        fw.dma(q, dst_ap, src_ap, key=dst_t, writes=[dst_t])
    for c in range(NBLK * 512 // 4096):
        for g in range(4):
            dload(qT[0:64, c*4096:(c+1)*4096].rearrange("d (i g q) -> d i g q", i=8, g=4)[:, :, g, :], A["qT"](c, g), qT)
            if c >= 2:
                dload(qT2[0:64, c*4096:(c+1)*4096].rearrange("d (i g q) -> d i g q", i=8, g=4)[:, :, g, :], A["qT"](c, g), qT2)
    for c in range(2):
        dload(ksT[0:64, c*4096:(c+1)*4096], A["ksT"][:, c*4096:(c+1)*4096], ksT)
        dload(ksT[64:128, c*4096:(c+1)*4096], A["E"][:, c*4096:(c+1)*4096], ksT)
        dload(kwT[:, c*4096:(c+1)*4096], A["kwT"][:, c*4096:(c+1)*4096], kwT)
    load_cast(vs[:, :, 0:64], [((lambda s: s[:, 0:4096].rearrange("p (k d) -> p k d", d=64)), A["vs"])], 128, 4096, vs)
    load_cast(vw[:, :, 0:64], [((lambda s: s[:, 0:4096].rearrange("p (k d) -> p k d", d=64)), A["vw"])], 128, 4096, vw)

    with ExitStack() as st3:
        old = fw.stack; fw.stack = st3
        kT = fw.sb("kT", [64, 8192], BF16); w1 = fw.sb("w1", [64, 32 * 128], BF16)
        w2 = fw.sb("w2", [128, 64], BF16); w2f = fw.sb("w2f", [128, 64]); pos = fw.sb("pos", [64, 32], BF16); posf = fw.sb("posf", [64, 32])
        bia = fw.sb("bia", [128, 1]); g1 = fw.sb("g1", [128, 512], BF16)
        fw.stack = old
        for which in range(2):
            kd, w1d, w2d, pd = ((A["kcT"], A["w1k"], A["w2k"], A["posk"]), (A["vcT"], A["w1v"], A["w2v"], A["posv"]))[which]
            for c in range(2):
                dload(kT[:, c*4096:(c+1)*4096], kd[:, c*4096:(c+1)*4096], kT)
            load_cast(w1[:], [(whole(64, 4096), w1d)], 64, 4096, w1)
            fw.dma("sp", w2f[:], w2d, key=w2f, writes=[w2f])
            op("dve", lambda e: e.tensor_copy(out=w2[:], in_=w2f[:]), reads=[w2f], writes=[w2])
            fw.dma("sp", posf[:], pd, key=posf, writes=[posf])
            op("dve", lambda e: e.tensor_copy(out=pos[:], in_=posf[:]), reads=[posf], writes=[pos])
            for l in range(32):
                op("pe", lambda e: e.matmul(M0[:, 0:511], lhsT=w1[:, l*128:(l+1)*128], rhs=kT[:, l:l+16*510+1:16],
                                            start=(l == 0), stop=(l == 31)), reads=[w1, kT], writes=[M0])
            for l in range(32):
                op("pe", lambda e: e.matmul(M1[:, 0:1], lhsT=w1[:, l*128:(l+1)*128], rhs=pos[:, l:l+1],
                                            start=(l == 0), stop=(l == 31)), reads=[w1, pos], writes=[M1])
            op("dve", lambda e: e.tensor_copy(out=bia[:], in_=M1[:, 0:1]), reads=[M1], writes=[bia])
            op("dve", lambda e: e.memset(g1[:], 0.0), writes=[g1])
            op("act", lambda e: e.activation(out=g1[:, 0:511], in_=M0[:, 0:511], func=AF.Gelu, bias=bia[:, 0:1]), reads=[M0, bia], writes=[g1])
            if which == 0:
                op("pe", lambda e: e.matmul(M2[0:64, 0:512], lhsT=w2[:], rhs=g1[:], start=True, stop=True), reads=[w2, g1], writes=[M2])
                op("dve", lambda e: e.tensor_copy(out=kcmpT[:], in_=M2[0:64, 0:512]), reads=[M2], writes=[kcmpT])
            else:
                for m in range(4):
                    op("pe", lambda e: e.matmul(M2[:, m*64:(m+1)*64], lhsT=g1[:, m*128:(m+1)*128], rhs=w2[:], start=True, stop=True),
                       reads=[w2, g1], writes=[M2])
                op("dve", lambda e: e.tensor_copy(out=vcmp[:, :, 0:64], in_=M2[:, 0:256].rearrange("p (m d) -> p m d", m=4)),
                   reads=[M2], writes=[vcmp])
        fw.barrier(recycle=False)
    eall = fw.sb("eall", [128, 4, 512], BF16); pn = fw.sb("pn", [128, 4, 512]); pg = fw.sb("pg", [128, 4, 128], BF16)
    PT = [fw.sb("PT0", [128, 512], BF16), fw.sb("PT1", [128, 512], BF16), fw.sb("PT2", [128, 512], BF16)]
    rz = fw.sb("rz", [128, 512]); adj = fw.sb("adj", [128, 128]); adj2 = fw.sb("adj2", [128, 128])
    v8 = fw.sb("v8", [128, 16]); thr = fw.sb("thr", [128, 1]); NM = fw.sb("NM", [128, 128], BF16)
    nmT = fw.sb("nmT", [128, 4, 128], BF16); NMs = fw.sb("NMs", [128, 128], BF16)
    osb = fw.sb("osb", [128, 512]); zz = fw.sb("zz", [128, 4]); wgt = fw.sb("wgt", [128, 4])
    acc = [fw.sb("acc0", [128, 256]), fw.sb("acc1", [128, 256])]
    ucount = [0]
    pcount = [0]
    nmTs = [nmT, fw.sb("nmT1", [128, 4, 128], BF16)]
    ocs = [fw.sb("ocs0", [65, 512]), fw.sb("ocs1", [65, 512])]

    def unit_a(U):
        qc, extra = U["qc"], U["extra"]
        u = ucount[0]; ucount[0] += 1
        pc = pcount[0]; pcount[0] += 1
        Sp = S[u % NS]; Pt = PT[pc % 3]
        op("pe", lambda e: e.matmul(Sp[:, :], lhsT=U["lh"], rhs=qc, start=True, stop=(len(extra) == 0)),
           reads=U["rd"], writes=[Sp])
        for xi, (lh, rh, rd) in enumerate(extra):
            op("pe", lambda e: e.matmul(Sp[:, :], lhsT=lh, rhs=rh, start=False, stop=(xi == len(extra) - 1)), reads=rd, writes=[Sp])
        op("act", lambda e: e.activation(out=Pt[:], in_=Sp[:], func=AF.Exp, scale=0.125), reads=[Sp], writes=[Pt])
        U["Pt"] = Pt

    def unit_b(U):
        Pt = U["Pt"]; Vt = U["V"]; Ot = U["O"]
        op("pe", lambda e: e.matmul(Ot[0:65, :], lhsT=Vt[:, U["kt"], :], rhs=Pt[:], start=U["first"], stop=U["last"]), reads=[Vt, Pt], writes=[Ot])

    def chain_steps(i):
        qc = qT[0:64, i*512:(i+1)*512]
        nmt = nmTs[i % 2]; oc = ocs[i % 2]
        ms = [m for m in range(4) if i - 8 * m >= 0]
        nm_ = len(ms)
        steps = []

        def cmp_unit(mi, m):
            k = i - 8 * m
            u = ucount[0]; ucount[0] += 1
            Sp = S[u % NS]
            nb = k <= 8
            op("pe", lambda e: e.matmul(Sp[:, :], lhsT=kcmpT[:, m*128:(m+1)*128], rhs=qc, start=True, stop=(not nb)), reads=[kcmpT, qT], writes=[Sp])
            if nb:
                op("pe", lambda e: e.matmul(Sp[:, :], lhsT=idb[:], rhs=tabs[:, k, :], start=False, stop=True), reads=[idb, tabs], writes=[Sp])
            op("act", lambda e: e.activation(out=eall[:, m, :], in_=Sp[:], func=AF.Exp, scale=0.125), reads=[Sp], writes=[eall])
            op("pe", lambda e: e.matmul(O["c"][0:65, :], lhsT=vcmp[:, m, :], rhs=eall[:, m, :], start=(mi == 0), stop=(mi == nm_ - 1)),
               reads=[vcmp, eall], writes=[O["c"]])
        for mi, m in enumerate(ms):
            steps.append(lambda mi=mi, m=m: cmp_unit(mi, m))

        def s1():
            op("dve", lambda e: e.tensor_copy(out=oc[:, :], in_=O["c"][0:65, :]), reads=[O["c"]], writes=[oc])
            op("dve", lambda e: e.tensor_scalar_max(out=rz[64:65, :], in0=oc[64:65, :], scalar1=1e-30), reads=[oc], writes=[rz])
            op("dve", lambda e: e.reciprocal(out=rz[64:65, :], in_=rz[64:65, :]), reads=[rz], writes=[rz])
            op("pe", lambda e: e.matmul(M0[:, :], lhsT=ones[64:65, :], rhs=rz[64:65, :], start=True, stop=True), reads=[ones, rz], writes=[M0])
        steps.append(s1)

        def s2():
            for m in ms:
                op("dve", lambda e: e.tensor_tensor(out=pn[:, m, :], in0=eall[:, m, :], in1=M0[:, :], op=ALU.mult), reads=[eall, M0], writes=[pn])
            with nc.allow_low_precision("bf16 out, fp32 internal accumulate"):
                op("dve", lambda e: e.tensor_reduce(out=pg[:, 0:nm_, :], in_=pn[:, 0:nm_, :].rearrange("p m (g q) -> p m q g", g=4),
                                                    axis=AX.X, op=ALU.add), reads=[pn], writes=[pg])
            for mi, m in enumerate(ms):
                op("pe", lambda e: e.matmul(M0[:, 0:128], lhsT=pg[:, m, :], rhs=ovl[:, m, :], start=(mi == 0), stop=(mi == nm_ - 1)),
                   reads=[pg, ovl], writes=[M0])
        steps.append(s2)

        def s3():
            op("dve", lambda e: e.tensor_tensor(out=adj[:], in0=M0[:, 0:128], in1=G[:, 128-4*i:256-4*i], op=ALU.add), reads=[M0, G], writes=[adj])
            op("dve", lambda e: e.tensor_scalar_add(out=adj[:, 0:1], in0=adj[:, 0:1], scalar1=1e4), reads=[adj], writes=[adj])
            op("dve", lambda e: e.max(out=v8[:, 0:8], in_=adj[:]), reads=[adj], writes=[v8])
            op("dve", lambda e: e.match_replace(out=adj2[:], in_to_replace=v8[:, 0:8], in_values=adj[:], imm_value=-3e38), reads=[adj, v8], writes=[adj2])
            op("dve", lambda e: e.max(out=v8[:, 8:16], in_=adj2[:]), reads=[adj2], writes=[v8])
            op("dve", lambda e: e.tensor_scalar_max(out=thr[:], in0=v8[:, 15:16], scalar1=-1e29), reads=[v8], writes=[thr])
            op("dve", lambda e: e.tensor_scalar(out=NM[:], in0=adj[:], scalar1=thr[:, 0:1], scalar2=NEGB, op0=ALU.is_lt, op1=ALU.mult),
               reads=[adj, thr], writes=[NM])
            op("pool", lambda e: e.tensor_copy(out=NMs[:, 0:64], in_=NM[:, 64:128]), reads=[NM], writes=[NMs])
            op("pool", lambda e: e.tensor_copy(out=NMs[:, 64:128], in_=NM[:, 0:64]), reads=[NM], writes=[NMs])
        steps.append(s3)

        def s4():
            Mb = M2.t[:].bitcast(BF16)
            op("pe", lambda e: e.transpose(out=Mb[:, 0:128], in_=NM[:], identity=idb[:]), reads=[NM, idb], writes=[M2])
            op("pe", lambda e: e.transpose(out=Mb[:, 128:256], in_=NMs[:], identity=idb[:]), reads=[NMs, idb], writes=[M2])
            op("dve", lambda e: e.tensor_copy(out=qT[64:128, i*512:(i+1)*512].rearrange("p (g q) -> p g q", g=4),
                                              in_=Mb[64:128, 128:256].unsqueeze(1).to_broadcast([64, 4, 128])), reads=[M2], writes=[qm[i]])
            if i >= 16:
                op("dve", lambda e: e.tensor_copy(out=qT2[64:128, i*512:(i+1)*512].rearrange("p (g q) -> p g q", g=4),
                                                  in_=Mb[64:128, 0:128].unsqueeze(1).to_broadcast([64, 4, 128])), reads=[M2], writes=[qm[i]])
        steps.append(s4)
        return steps

    def block_units(i):
        qc = qT[0:64, i*512:(i+1)*512]
        units = []
        par = A["p"]
        w_skip = 5 if par == 0 else 0
        w_bias = (0, 4) if par == 0 else (1, 5)
        kts = [kt for kt in range(2*i - 4, 2*i + 2) if kt >= 0 and (kt - (2*i - 4)) != w_skip]
        for kt in kts:
            u_ = kt - (2*i - 4)
            extra = [(idb[:], tabs[:, 11 + u_, :], [idb, tabs])] if u_ in w_bias else []
            units.append(dict(qc=qc, lh=kwT[:, kt*128:(kt+1)*128], rd=[kwT, qT], extra=extra, V=vw, kt=kt, O=O["w"], first=(kt == kts[0]), last=(kt == kts[-1])))
        nk = 2 * i + 1 if par == 0 else 2 * i + 2
        diag = 2 * i + par
        for kt in range(nk):
            extra = []
            if kt == diag:
                extra.append((idb[:], tabs[:, 9 + kt - 2*i, :], [idb, tabs]))
            qt_ = qT if kt < 32 else qT2
            units.append(dict(qc=qt_[:, i*512:(i+1)*512], lh=ksT[:, kt*128:(kt+1)*128], rd=[ksT, qt_, qm[i]], extra=extra, V=vs, kt=kt, O=O["s"],
                              first=(kt == 0), last=(kt == nk - 1)))
        return units

    def combine(i):
        ac = acc[i % 2]
        for bi, b in enumerate(("c", "s", "w")):
            if b == "c":
                src_t = ocs[i % 2]
            else:
                op("act", lambda e: e.copy(out=osb[0:65, :], in_=O[b][0:65, :]), reads=[O[b]], writes=[osb])
                src_t = osb
            for g in range(4):
                op("pe", lambda e: e.transpose(out=M1[:, 128 + g*65:128 + (g+1)*65], in_=src_t[0:65, g*128:(g+1)*128], identity=idf[0:65, 0:65]),
                   reads=[src_t, idf], writes=[M1])
            ov = M1[:, 128:128 + 260].rearrange("p (g d) -> p g d", g=4)
            op("dve", lambda e: e.tensor_scalar_max(out=zz[:], in0=ov[:, :, 64], scalar1=1e-30), reads=[M1], writes=[zz])
            op("dve", lambda e: e.reciprocal(out=zz[:], in_=zz[:]), reads=[zz], writes=[zz])
            gv = gate[:, i*12:(i+1)*12].rearrange("p (g b) -> p g b", b=3)
            op("dve", lambda e: e.tensor_tensor(out=wgt[:], in0=zz[:], in1=gv[:, :, bi], op=ALU.mult), reads=[zz, gate], writes=[wgt])
            for g in range(4):
                if bi == 0:
                    op("dve", lambda e: e.tensor_scalar(out=ac[:, g*64:(g+1)*64], in0=ov[:, g, 0:64], scalar1=wgt[:, g:g+1], scalar2=None, op0=ALU.mult),
                       reads=[M1, wgt], writes=[ac])
                else:
                    op("dve", lambda e: e.scalar_tensor_tensor(out=ac[:, g*64:(g+1)*64], in0=ov[:, g, 0:64], scalar=wgt[:, g:g+1],
                                                               in1=ac[:, g*64:(g+1)*64], op0=ALU.mult, op1=ALU.add), reads=[M1, wgt, ac], writes=[ac])
        fw.dma("sp", A["out"](i), ac[:], key=ac, reads=[ac])

    for stp in chain_steps(0):
        stp()
    for i in range(NBLK):
        units = block_units(i)
        chain = chain_steps(i + 1) if i + 1 < NBLK else []
        n = len(units); ncn = len(chain); ci = 0
        pend = []
        for idx, U in enumerate(units):
            unit_a(U)
            pend.append(U)
            if len(pend) > 2:
                unit_b(pend.pop(0))
            while ci < ncn and ci * n < (idx + 1) * ncn:
                chain[ci](); ci += 1
        for U in pend:
            unit_b(U)
        while ci < ncn:
            chain[ci](); ci += 1
        combine(i)

def emit_hgrn(fw, nc, A):
    op = fw.op
    q = fw.sb("q", [128, 64, 64]); fl = fw.sb("fl", [128, 64, 64]); iv = fw.sb("iv", [128, 64, 64]); ivb = fw.sb("ivb", [128, 64, 64], BF16)
    kk = fw.sb("kk", [128, 64, 64]); lf = fw.sb("lf", [128, 64, 64]); ob = fw.sb("ob", [128, 64, 64])
    h0 = fw.sb("h0", [128, 64]); h1 = fw.sb("h1", [128, 64]); lsel = fw.sb("lsel", [128, 64]); lb = fw.sb("lb", [128, 64]); oml = fw.sb("oml", [128, 64])
    LT = fw.sb("LT", [128, 128]); LD = fw.sb("LD", [128, 128]); LU = fw.sb("LU", [128, 128]); ind = fw.sb("ind", [128, 2])
    cm = fw.sb("cm", [128, 128]); idb = fw.sb("idb", [128, 128], BF16)
    for t_, d_ in ((q, A["q"]), (fl, A["f"]), (iv, A["iv"]), (h0, A["h0"]), (h1, A["h1"]), (lsel, A["lsel"]), (LT, A["LT"]), (LD, A["LD"]),
                   (LU, A["LU"]), (ind, A["ind"]), (cm, A["cmask"]), (idb, A["idb"])):
        fw.dma("sp", t_[:], d_, key=t_, writes=[t_])
    op("dve", lambda e: e.tensor_tensor(out=lb[:], in0=h1[:], in1=h0[:], op=ALU.subtract), reads=[h0, h1], writes=[lb])
    op("act", lambda e: e.activation(out=lb[:], in_=lb[:], func=AF.Sigmoid), reads=[lb], writes=[lb])
    op("dve", lambda e: e.tensor_tensor(out=lb[:], in0=lb[:], in1=lsel[:], op=ALU.mult), reads=[lb, lsel], writes=[lb])
    op("dve", lambda e: e.tensor_scalar(out=oml[:], in0=lb[:], scalar1=-1.0, scalar2=1.0, op0=ALU.mult, op1=ALU.add), reads=[lb], writes=[oml])
    lbb = lb[:].unsqueeze(1).to_broadcast([128, 64, 64]); omb = oml[:].unsqueeze(1).to_broadcast([128, 64, 64])
    op("act", lambda e: e.activation(out=fl[:], in_=fl[:], func=AF.Sigmoid), reads=[fl], writes=[fl])
    op("dve", lambda e: e.tensor_tensor(out=fl[:], in0=fl[:], in1=omb, op=ALU.mult), reads=[fl, oml], writes=[fl])
    op("dve", lambda e: e.tensor_tensor(out=kk[:], in0=fl[:], in1=omb, op=ALU.subtract), reads=[fl, oml], writes=[kk])
    op("dve", lambda e: e.tensor_scalar(out=kk[:], in0=kk[:], scalar1=-1.0, scalar2=None, op0=ALU.mult), reads=[kk], writes=[kk])
    op("dve", lambda e: e.tensor_tensor(out=lf[:], in0=fl[:], in1=lbb, op=ALU.add), reads=[fl, lb], writes=[lf])
    op("act", lambda e: e.activation(out=lf[:], in_=lf[:], func=AF.Ln), reads=[lf], writes=[lf])
    op("pool", lambda e: e.tensor_copy(out=ivb[:], in_=iv[:]), reads=[iv], writes=[ivb])
    CA = fw.ps("CA", [128, 512]); CB = fw.ps("CB", [128, 512]); TAp = fw.ps("TA", [128, 512]); TBp = fw.ps("TB", [128, 512])
    ATp = fw.ps("AT", [128, 512]); Op = fw.ps("O", [128, 512]); Up = fw.ps("U", [128, 512])
    TAb = TAp.t[:].bitcast(BF16); TBb = TBp.t[:].bitcast(BF16)
    e1 = fw.sb("e1", [128, 256]); e2 = fw.sb("e2", [128, 256]); e3 = fw.sb("e3", [128, 256]); e4 = fw.sb("e4", [128, 256])
    eb = fw.sb("eb", [64, 8])
    qt = fw.sb("qt", [128, 256], BF16); kt = fw.sb("kt", [128, 256], BF16); qb = fw.sb("qb", [128, 256], BF16); kd = fw.sb("kd", [128, 256], BF16)
    qkT = fw.sb("qkT", [64, 1024], BF16); qbTA = fw.sb("qbTA", [64, 512], BF16); qbTB = fw.sb("qbTB", [64, 512], BF16)
    att = fw.sb("att", [128, 128], BF16)
    Sf = fw.sb("Sf", [64, 64]); Sb = fw.sb("Sb", [64, 64], BF16)
    op("dve", lambda e: e.memset(Sf[:], 0.0), writes=[Sf]); op("dve", lambda e: e.memset(Sb[:], 0.0), writes=[Sb])
    op("dve", lambda e: e.memset(qbTA[:], 0.0), writes=[qbTA]); op("dve", lambda e: e.memset(qbTB[:], 0.0), writes=[qbTB])
    for g in range(16):
        for j in range(4):
            n = g * 4 + j
            op("pe", lambda e: e.matmul(CA[:, j*64:(j+1)*64], lhsT=LD[:], rhs=lf[:, n, :], start=True, stop=True), reads=[LD, lf], writes=[CA])
            op("pe", lambda e: e.matmul(CA[:, 256+j*64:256+(j+1)*64], lhsT=LT[:], rhs=lf[:, n, :], start=True, stop=True), reads=[LT, lf], writes=[CA])
            op("pe", lambda e: e.matmul(CB[:, j*64:(j+1)*64], lhsT=LU[:], rhs=lf[:, n, :], start=True, stop=True), reads=[LU, lf], writes=[CB])
            op("pe", lambda e: e.matmul(CB[0:64, 256+j*2:256+(j+1)*2], lhsT=lf[:, n, :], rhs=ind[:], start=True, stop=True), reads=[ind, lf], writes=[CB])
        op("act", lambda e: e.activation(out=e1[:], in_=CA[:, 0:256], func=AF.Exp), reads=[CA], writes=[e1])
        op("act", lambda e: e.activation(out=e2[:], in_=CA[:, 0:256], func=AF.Exp, scale=-1.0), reads=[CA], writes=[e2])
        op("act", lambda e: e.activation(out=e3[:], in_=CA[:, 256:512], func=AF.Exp), reads=[CA], writes=[e3])
        op("act", lambda e: e.activation(out=e4[:], in_=CB[:, 0:256], func=AF.Exp), reads=[CB], writes=[e4])
        op("act", lambda e: e.activation(out=eb[:], in_=CB[0:64, 256:264], func=AF.Exp), reads=[CB], writes=[eb])
        qg = q[:, g*4:(g+1)*4, :].rearrange("p a d -> p (a d)"); kg = kk[:, g*4:(g+1)*4, :].rearrange("p a d -> p (a d)")
        op("dve", lambda e: e.tensor_tensor(out=qt[:], in0=qg, in1=e1[:], op=ALU.mult), reads=[q, e1], writes=[qt])
        op("dve", lambda e: e.tensor_tensor(out=kt[:], in0=kg, in1=e2[:], op=ALU.mult), reads=[kk, e2], writes=[kt])
        op("dve", lambda e: e.tensor_tensor(out=qb[:], in0=qg, in1=e3[:], op=ALU.mult), reads=[q, e3], writes=[qb])
        op("dve", lambda e: e.tensor_tensor(out=kd[:], in0=kg, in1=e4[:], op=ALU.mult), reads=[kk, e4], writes=[kd])
        for j in range(4):
            op("pe", lambda e: e.transpose(out=TAb[0:64, j*128:(j+1)*128], in_=qt[:, j*64:(j+1)*64], identity=idb[:]), reads=[qt, idb], writes=[TAp])
            op("pe", lambda e: e.transpose(out=TAb[0:64, 512+j*128:512+(j+1)*128], in_=kt[:, j*64:(j+1)*64], identity=idb[:]), reads=[kt, idb], writes=[TAp])
            op("pe", lambda e: e.transpose(out=TBb[0:64, j*128:(j+1)*128], in_=qb[:, j*64:(j+1)*64], identity=idb[:]), reads=[qb, idb], writes=[TBp])
        op("act", lambda e: e.copy(out=qkT[:], in_=TAb[0:64, 0:1024]), reads=[TAp], writes=[qkT])
        tb3 = TBb[0:64, 0:512].rearrange("p (a t) -> p a t", a=4)
        op("dve", lambda e: e.tensor_copy(out=qbTA[:].rearrange("p (a t) -> p a t", a=4)[:, :, 0:64], in_=tb3[:, :, 0:64]), reads=[TBp], writes=[qbTA])
        op("dve", lambda e: e.tensor_copy(out=qbTB[:].rearrange("p (a t) -> p a t", a=4)[:, :, 64:128], in_=tb3[:, :, 64:128]), reads=[TBp], writes=[qbTB])
        for j in range(4):
            n = g * 4 + j
            op("pe", lambda e: e.matmul(ATp[:, 0:128], lhsT=qkT[:, 512+j*128:512+(j+1)*128], rhs=qkT[:, j*128:(j+1)*128], start=True, stop=True),
               reads=[qkT], writes=[ATp])
            op("dve", lambda e: e.tensor_tensor(out=att[:], in0=ATp[:, 0:128], in1=cm[:], op=ALU.mult), reads=[ATp, cm], writes=[att])
            op("pe", lambda e: e.matmul(Op[:, 0:64], lhsT=att[:], rhs=ivb[:, n, :], start=True, stop=False), reads=[att, ivb], writes=[Op])
            op("pe", lambda e: e.matmul(Op[:, 0:64], lhsT=qbTA[:, j*128:(j+1)*128], rhs=Sb[:], start=False, stop=False), reads=[qbTA, Sb], writes=[Op])
            op("pe", lambda e: e.matmul(Up[0:64, 0:64], lhsT=kd[0:64, j*64:(j+1)*64], rhs=ivb[0:64, n, :], start=True, stop=True), reads=[kd, ivb], writes=[Up])
            op("dve", lambda e: e.scalar_tensor_tensor(out=Sf[:], in0=Sf[:], scalar=eb[:, 2*j:2*j+1], in1=Up[0:64, 0:64], op0=ALU.mult, op1=ALU.add),
               reads=[Sf, eb, Up], writes=[Sf])
            op("act", lambda e: e.copy(out=Sb[:], in_=Sf[:]), reads=[Sf], writes=[Sb])
            op("pe", lambda e: e.matmul(Op[:, 0:64], lhsT=qbTB[:, j*128:(j+1)*128], rhs=Sb[:], start=False, stop=True), reads=[qbTB, Sb], writes=[Op])
            op("act", lambda e: e.copy(out=ob[:, n, :], in_=Op[:, 0:64]), reads=[Op], writes=[ob])
            op("pe", lambda e: e.matmul(Up[0:64, 64:128], lhsT=kd[64:128, j*64:(j+1)*64], rhs=ivb[64:128, n, :], start=True, stop=True), reads=[kd, ivb], writes=[Up])
            op("dve", lambda e: e.scalar_tensor_tensor(out=Sf[:], in0=Sf[:], scalar=eb[:, 2*j+1:2*j+2], in1=Up[0:64, 64:128], op0=ALU.mult, op1=ALU.add),
               reads=[Sf, eb, Up], writes=[Sf])
            op("act", lambda e: e.copy(out=Sb[:], in_=Sf[:]), reads=[Sf], writes=[Sb])
    fw.dma("sp", A["out"], ob[:], key=ob, reads=[ob])


def emit_post(fw, nc, A, NT):
    op = fw.op
    vg = fw.sb("vg", [128, 256]); vb = fw.sb("vb", [128, 256]); ws = fw.sb("ws", [128, 4, 128]); cm = fw.sb("cm", [128, 128])
    bs = fw.sb("bs", [128, 4]); og = fw.sb("og", [128, 1024])
    for t_, d_ in ((vg, A["vgain"]), (vb, A["vbias"]), (ws, A["wsT"]), (cm, A["cmask"]), (bs, A["bsT"]), (og, A["ogain"])):
        fw.dma("sp", t_[:], d_, key=t_, writes=[t_])
    op("dve", lambda e: e.tensor_tensor(out=ws[:], in0=ws[:], in1=cm[:].unsqueeze(1).to_broadcast([128, 4, 128]), op=ALU.mult), reads=[ws, cm], writes=[ws])
    Zp = [fw.ps("Z0", [128, 512]), fw.ps("Z1", [128, 512])]
    bufs = []
    for i in range(2):
        bufs.append(dict(u=fw.sb("u%d" % i, [128, 256]), v=fw.sb("v%d" % i, [128, 256]), hg=fw.sb("hg%d" % i, [128, 256]),
                         Y=fw.sb("Y%d" % i, [128, 1024]), sq=fw.sb("sq%d" % i, [128, 1024]), st=fw.sb("st%d" % i, [128, 16]), st2=fw.sb("st2%d" % i, [128, 16])))
    for t in range(NT):
        B = bufs[t % 2]; u, v, hg, Y, sq, s1, s2 = B["u"], B["v"], B["hg"], B["Y"], B["sq"], B["st"], B["st2"]
        rows = slice(t*128, (t+1)*128)
        fw.dma("sp", u[:], A["h"][rows, 0:256], key=u, writes=[u]); fw.dma("sp", v[:], A["h"][rows, 256:512], key=v, writes=[v])
        fw.dma("pool", hg[:], A["h"][rows, 2584:2840], key=hg, writes=[hg])
        fw.dma("pool", Y[:, 256:768], A["yb"][rows, :], key=Y, writes=[Y]); fw.dma("pool", Y[:, 768:1024], A["yc"][rows, :], key=Y, writes=[Y])
        op("act", lambda e: e.activation(out=u[:], in_=u[:], func=AF.Gelu), reads=[u], writes=[u])
        op("act", lambda e: e.activation(out=v[:], in_=v[:], func=AF.Gelu), reads=[v], writes=[v])
        v3 = v[:].rearrange("p (g d) -> p g d", g=4)
        op("dve", lambda e: e.tensor_reduce(out=s1[:, 0:4], in_=v3, axis=AX.X, op=ALU.add), reads=[v], writes=[s1])
        op("dve", lambda e: e.tensor_scalar(out=s1[:, 0:4], in0=s1[:, 0:4], scalar1=1.0/64, scalar2=None, op0=ALU.mult), reads=[s1], writes=[s1])
        op("dve", lambda e: e.tensor_tensor(out=v3, in0=v3, in1=s1[:, 0:4].unsqueeze(2).to_broadcast([128, 4, 64]), op=ALU.subtract), reads=[v, s1], writes=[v])
        op("dve", lambda e: e.tensor_tensor(out=sq[:, 0:256], in0=v[:], in1=v[:], op=ALU.mult), reads=[v], writes=[sq])
        op("dve", lambda e: e.tensor_reduce(out=s2[:, 0:4], in_=sq[:, 0:256].rearrange("p (g d) -> p g d", g=4), axis=AX.X, op=ALU.add), reads=[sq], writes=[s2])
        op("act", lambda e: e.activation(out=s2[:, 0:4], in_=s2[:, 0:4], func=AF.Sqrt, scale=1.0/64, bias=1e-5), reads=[s2], writes=[s2])
        op("dve", lambda e: e.reciprocal(out=s2[:, 0:4], in_=s2[:, 0:4]), reads=[s2], writes=[s2])
        op("dve", lambda e: e.tensor_tensor(out=v3, in0=v3, in1=s2[:, 0:4].unsqueeze(2).to_broadcast([128, 4, 64]), op=ALU.mult), reads=[v, s2], writes=[v])
        op("dve", lambda e: e.tensor_tensor(out=v[:], in0=v[:], in1=vg[:], op=ALU.mult), reads=[v, vg], writes=[v])
        op("dve", lambda e: e.tensor_tensor(out=v[:], in0=v[:], in1=vb[:], op=ALU.add), reads=[v, vb], writes=[v])
        Z = Zp[t % 2]
        for g in range(4):
            op("pe", lambda e: e.matmul(Z[:, g*64:(g+1)*64], lhsT=ws[:, g, :], rhs=v[:, g*64:(g+1)*64], start=True, stop=True), reads=[ws, v], writes=[Z])
        for g in range(4):
            op("dve", lambda e: e.scalar_tensor_tensor(out=Y[:, g*64:(g+1)*64], in0=Z[:, g*64:(g+1)*64], scalar=bs[:, g:g+1], in1=u[:, g*64:(g+1)*64],
                                                       op0=ALU.add, op1=ALU.mult), reads=[Z, bs, u], writes=[Y])
        op("act", lambda e: e.activation(out=sq[:], in_=Y[:], func=AF.Square), reads=[Y], writes=[sq])
        op("dve", lambda e: e.tensor_reduce(out=s1[:], in_=sq[:].rearrange("p (h d) -> p h d", h=16), axis=AX.X, op=ALU.add), reads=[sq], writes=[s1])
        op("act", lambda e: e.activation(out=s1[:], in_=s1[:], func=AF.Sqrt, scale=1.0/64, bias=1e-6), reads=[s1], writes=[s1])
        op("dve", lambda e: e.reciprocal(out=s1[:], in_=s1[:]), reads=[s1], writes=[s1])
        Y3 = Y[:].rearrange("p (h d) -> p h d", h=16)
        op("dve", lambda e: e.tensor_tensor(out=Y3, in0=Y3, in1=s1[:].unsqueeze(2).to_broadcast([128, 16, 64]), op=ALU.mult), reads=[Y, s1], writes=[Y])
        op("dve", lambda e: e.tensor_tensor(out=Y[:], in0=Y[:], in1=og[:], op=ALU.mult), reads=[Y, og], writes=[Y])
        op("act", lambda e: e.activation(out=hg[:], in_=hg[:], func=AF.Silu), reads=[hg], writes=[hg])
        op("dve", lambda e: e.tensor_tensor(out=Y[:, 768:1024], in0=Y[:, 768:1024], in1=hg[:], op=ALU.mult), reads=[Y, hg], writes=[Y])
        fw.dma("sp", A["out"][rows, :], Y[:], key=Y, reads=[Y])


def emit_addln(fw, nc, A, NT):
    op = fw.op
    g = fw.sb("g", [128, 1024]); be = fw.sb("be", [128, 1024])
    fw.dma("sp", g[:], A["g"], key=g, writes=[g]); fw.dma("sp", be[:], A["beta"], key=be, writes=[be])
    bufs = [dict(a=fw.sb("a%d" % i, [128, 1024]), b=fw.sb("b%d" % i, [128, 1024]), s=fw.sb("s%d" % i, [128, 12]), mv=fw.sb("mv%d" % i, [128, 2])) for i in range(2)]
    for t in range(NT):
        B = bufs[t % 2]; a, b, s, mv = B["a"], B["b"], B["s"], B["mv"]
        rows = slice(t*128, (t+1)*128)
        fw.dma("sp", a[:], A["a"][rows, :], key=a, writes=[a]); fw.dma("pool", b[:], A["b"][rows, :], key=b, writes=[b])
        op("dve", lambda e: e.scalar_tensor_tensor(out=a[:], in0=a[:], scalar=ALPHA, in1=b[:], op0=ALU.mult, op1=ALU.add), reads=[a, b], writes=[a])
        op("dve", lambda e: e.bn_stats(out=s[:, 0:6], in_=a[:, 0:512]), reads=[a], writes=[s])
        op("dve", lambda e: e.bn_stats(out=s[:, 6:12], in_=a[:, 512:1024]), reads=[a], writes=[s])
        op("dve", lambda e: e.bn_aggr(out=mv[:], in_=s[:]), reads=[s], writes=[mv])
        op("act", lambda e: e.activation(out=mv[:, 1:2], in_=mv[:, 1:2], func=AF.Sqrt, bias=1e-5), reads=[mv], writes=[mv])
        op("dve", lambda e: e.reciprocal(out=mv[:, 1:2], in_=mv[:, 1:2]), reads=[mv], writes=[mv])
        op("dve", lambda e: e.tensor_scalar(out=s[:, 0:1], in0=mv[:, 0:1], scalar1=mv[:, 1:2], scalar2=-1.0, op0=ALU.mult, op1=ALU.mult), reads=[mv], writes=[s])
        op("act", lambda e: e.activation(out=b[:], in_=a[:], func=AF.Identity, scale=mv[:, 1:2], bias=s[:, 0:1]), reads=[a, mv, s], writes=[b])
        op("pool", lambda e: e.tensor_tensor(out=b[:], in0=b[:], in1=g[:], op=ALU.mult), reads=[b, g], writes=[b])
        op("dve", lambda e: e.tensor_tensor(out=b[:], in0=b[:], in1=be[:], op=ALU.add), reads=[b, be], writes=[b])
        fw.dma("sp", A["out"][rows, :], b[:], key=b, reads=[b])


MOE_DEBUG = [0]

def emit_moe(fw, nc, A):
    NT = 16
    BIG = 1e9
    op = fw.op
    xb = fw.sb("xb", [128, 8, 2048], BF16); gT = fw.sb("gT", [16, 2048]); sel = fw.sb("sel", [16, 2048])
    acc = fw.sb("acc", [128, NT, 1024])
    idf = fw.sb("idf", [128, 128]); wr = fw.sb("wr", [128, 8, 20]); br = fw.sb("br", [128, 20])
    fw.dma("pool", sel[:], A["sel"], key=sel, writes=[sel]); fw.dma("pool", idf[:], A["idf"], key=idf, writes=[idf])
    fw.dma("pool", wr[:], A["wr"].rearrange("(k p) n -> p k n", p=128), key=wr, writes=[wr]); fw.dma("pool", br[:], A["br"], key=br, writes=[br])
    Gp = [fw.ps("G0", [128, 512]), fw.ps("G1", [128, 512])]; Up = [fw.ps("U0", [128, 512]), fw.ps("U1", [128, 512])]
    Bp = fw.ps("Bc", [128, 512]); Dp = [fw.ps("D0", [128, 512]), fw.ps("D1", [128, 512])]; Rp = fw.ps("R", [128, 512])
    if MOE_DEBUG[0] == 2:
        return
    with ExitStack() as st3:
        old = fw.stack; fw.stack = st3
        xt = [fw.sb("xt%d" % i, [128, 1024]) for i in range(2)]
        xf = [fw.sb("xf%d" % i, [128, 8, 128]) for i in range(2)]
        L = fw.sb("L", [128, NT, 20]); gm = fw.sb("gm", [128, NT]); oh = fw.sb("oh", [128, NT, 4]); eg = fw.sb("eg", [128, NT, 4]); zg = fw.sb("zg", [128, NT])
        le = fw.sb("le", [128, NT, 16]); m1 = fw.sb("m1", [128, NT]); k1 = fw.sb("k1", [128, NT, 16]); le2 = fw.sb("le2", [128, NT, 16]); m2 = fw.sb("m2", [128, NT])
        k2 = fw.sb("k2", [128, NT, 16]); w1 = fw.sb("w1", [128, NT]); w2 = fw.sb("w2", [128, NT]); gt = fw.sb("gt", [128, NT, 16]); gpad = fw.sb("gpad", [128, NT, 128])
        fw.stack = old
        for t in range(NT):
            a = xt[t % 2]; f = xf[t % 2]
            fw.dma("sp", a[:], A["x"][t*128:(t+1)*128, :], key=a, writes=[a])
            for k in range(8):
                P = Gp[(k // 4) % 2]
                op("pe", lambda e: e.transpose(out=P[:, (k % 4)*128:(k % 4 + 1)*128], in_=a[:, k*128:(k+1)*128], identity=idf[:]), reads=[a, idf], writes=[P])
                if k % 4 == 3:
                    k0 = k - 3
                    op("act", lambda e: e.copy(out=f[:, k0:k0+4, :], in_=P[:, :].rearrange("p (k t) -> p k t", k=4)), reads=[P], writes=[f])
                    op("dve", lambda e: e.tensor_copy(out=xb[:, k0:k0+4, t*128:(t+1)*128], in_=f[:, k0:k0+4, :]), reads=[f], writes=[xb])
            if MOE_DEBUG[0] == 3:
                continue
            for k in range(8):
                op("pe", lambda e: e.matmul(Rp[:, t*20:(t+1)*20], lhsT=f[:, k, :], rhs=wr[:, k, :], start=(k == 0), stop=(k == 7)), reads=[f, wr], writes=[Rp])
        if MOE_DEBUG[0] in (3, 4):
            fw.barrier(recycle=False)
            return
        op("dve", lambda e: e.tensor_tensor(out=L[:], in0=Rp[:, 0:NT*20].rearrange("p (t n) -> p t n", n=20), in1=br[:].unsqueeze(1).to_broadcast([128, NT, 20]), op=ALU.add), reads=[Rp, br], writes=[L])
        lg = L[:, :, 0:4]
        op("dve", lambda e: e.tensor_reduce(out=gm[:], in_=lg, axis=AX.X, op=ALU.max), reads=[L], writes=[gm])
        gmb = gm[:].unsqueeze(2).to_broadcast([128, NT, 4])
        op("dve", lambda e: e.tensor_tensor(out=oh[:], in0=lg, in1=gmb, op=ALU.is_equal), reads=[L, gm], writes=[oh])
        op("dve", lambda e: e.tensor_tensor(out=eg[:], in0=lg, in1=gmb, op=ALU.subtract), reads=[L, gm], writes=[eg])
        op("act", lambda e: e.activation(out=eg[:], in_=eg[:], func=AF.Exp), reads=[eg], writes=[eg])
        op("dve", lambda e: e.tensor_reduce(out=zg[:], in_=eg[:], axis=AX.X, op=ALU.add), reads=[eg], writes=[zg])
        op("dve", lambda e: e.reciprocal(out=zg[:], in_=zg[:]), reads=[zg], writes=[zg])
        op("dve", lambda e: e.tensor_scalar(out=oh[:], in0=oh[:], scalar1=-1.0, scalar2=BIG, op0=ALU.add, op1=ALU.mult), reads=[oh], writes=[oh])
        le4 = le[:].rearrange("p t (g e) -> p t g e", g=4)
        op("dve", lambda e: e.tensor_tensor(out=le4, in0=L[:, :, 4:20].rearrange("p t (g e) -> p t g e", g=4), in1=oh[:].unsqueeze(3).to_broadcast([128, NT, 4, 4]), op=ALU.add),
           reads=[L, oh], writes=[le])
        op("dve", lambda e: e.tensor_reduce(out=m1[:], in_=le[:], axis=AX.X, op=ALU.max), reads=[le], writes=[m1])
        op("dve", lambda e: e.tensor_tensor(out=k1[:], in0=le[:], in1=m1[:].unsqueeze(2).to_broadcast([128, NT, 16]), op=ALU.is_equal), reads=[le, m1], writes=[k1])
        op("dve", lambda e: e.scalar_tensor_tensor(out=le2[:], in0=k1[:], scalar=-BIG, in1=le[:], op0=ALU.mult, op1=ALU.add), reads=[k1, le], writes=[le2])
        op("dve", lambda e: e.tensor_reduce(out=m2[:], in_=le2[:], axis=AX.X, op=ALU.max), reads=[le2], writes=[m2])
        op("dve", lambda e: e.tensor_tensor(out=k2[:], in0=le2[:], in1=m2[:].unsqueeze(2).to_broadcast([128, NT, 16]), op=ALU.is_equal), reads=[le2, m2], writes=[k2])
        op("dve", lambda e: e.tensor_tensor(out=w1[:], in0=m2[:], in1=m1[:], op=ALU.subtract), reads=[m1, m2], writes=[w1])
        op("act", lambda e: e.activation(out=w1[:], in_=w1[:], func=AF.Exp), reads=[w1], writes=[w1])
        op("dve", lambda e: e.tensor_scalar_add(out=w1[:], in0=w1[:], scalar1=1.0), reads=[w1], writes=[w1])
        op("dve", lambda e: e.reciprocal(out=w1[:], in_=w1[:]), reads=[w1], writes=[w1])
        op("dve", lambda e: e.tensor_scalar(out=w2[:], in0=w1[:], scalar1=-1.0, scalar2=1.0, op0=ALU.mult, op1=ALU.add), reads=[w1], writes=[w2])
        op("dve", lambda e: e.tensor_tensor(out=w1[:], in0=w1[:], in1=zg[:], op=ALU.mult), reads=[w1, zg], writes=[w1])
        op("dve", lambda e: e.tensor_tensor(out=w2[:], in0=w2[:], in1=zg[:], op=ALU.mult), reads=[w2, zg], writes=[w2])
        op("dve", lambda e: e.tensor_tensor(out=k1[:], in0=k1[:], in1=w1[:].unsqueeze(2).to_broadcast([128, NT, 16]), op=ALU.mult), reads=[k1, w1], writes=[k1])
        op("dve", lambda e: e.tensor_tensor(out=k2[:], in0=k2[:], in1=w2[:].unsqueeze(2).to_broadcast([128, NT, 16]), op=ALU.mult), reads=[k2, w2], writes=[k2])
        op("dve", lambda e: e.tensor_tensor(out=gt[:], in0=k1[:], in1=k2[:], op=ALU.add), reads=[k1, k2], writes=[gt])
        if MOE_DEBUG[0] == 5:
            fw.barrier(recycle=False)
            return
        op("pool", lambda e: e.memset(gpad[:], 0.0), writes=[gpad])
        op("dve", lambda e: e.tensor_copy(out=gpad[:, :, 0:16], in_=gt[:]), reads=[gt], writes=[gpad])
        for t in range(NT):
            op("pe", lambda e: e.transpose(out=Bp[:, (t % 4)*128:(t % 4 + 1)*128], in_=gpad[:, t, :], identity=idf[:]), reads=[gpad, idf], writes=[Bp])
            if t % 4 == 3:
                t0 = t - 3
                op("dve", lambda e: e.tensor_copy(out=gT[:, t0*128:(t0+4)*128], in_=Bp[0:16, :]), reads=[Bp], writes=[gT])
        fw.barrier(recycle=False)
    if MOE_DEBUG[0] == 1:
        return
    stg = [fw.sb("stg%d" % i, [128, 2048]) for i in range(2)]
    W = [dict(g=fw.sb("wg%d" % i, [128, 8, 512], BF16), u=fw.sb("wu%d" % i, [128, 8, 512], BF16), d=fw.sb("wd%d" % i, [128, 4, 1024], BF16)) for i in range(2)]
    hT = [fw.sb("hT%d" % i, [128, 4, 512], BF16) for i in range(2)]
    sg = [fw.sb("sg%d" % i, [128, 512]) for i in range(2)]
    ci = [0]
    def load_cast(dst_ap, dst_t, src_ap, n):
        s = stg[ci[0] % 2]; e = ("pool", "dve")[ci[0] % 2]; ci[0] += 1
        fw.dma("sp", s[:, 0:n], src_ap, key=s, writes=[s])
        op(e, lambda en: en.tensor_copy(out=dst_ap, in_=s[:, 0:n]), reads=[s], writes=[dst_t])
    def load_w(e):
        Wb = W[e % 2]
        for half in range(2):
            load_cast(Wb["g"][:, half*4:(half+1)*4, :], Wb["g"], A["wg"][e, half*512:(half+1)*512, :].rearrange("(k p) n -> p k n", p=128), 2048)
        for half in range(2):
            load_cast(Wb["u"][:, half*4:(half+1)*4, :], Wb["u"], A["wu"][e, half*512:(half+1)*512, :].rearrange("(k p) n -> p k n", p=128), 2048)
        for half in range(2):
            load_cast(Wb["d"][:, half*2:(half+1)*2, :], Wb["d"], A["wd"][e, half*256:(half+1)*256, :].rearrange("(k p) n -> p k n", p=128), 2048)
    load_w(0)
    cnt = 0; dc = 0
    for e_ in range(16):
        if e_ + 1 < 16:
            load_w(e_ + 1)
        Wb = W[e_ % 2]
        for tg in range(4):
            ts_ = slice(tg*512, (tg+1)*512)
            op("pe", lambda e: e.matmul(Bp[:, :], lhsT=sel[:, e_*128:(e_+1)*128], rhs=gT[:, ts_], start=True, stop=True), reads=[sel, gT], writes=[Bp])
            h = hT[(e_*4 + tg) % 2]
            for hc in range(4):
                G = Gp[cnt % 2]; U = Up[cnt % 2]; s_ = sg[cnt % 2]; cnt += 1
                for k in range(8):
                    op("pe", lambda e: e.matmul(G[:, :], lhsT=Wb["g"][:, k, hc*128:(hc+1)*128], rhs=xb[:, k, ts_], start=(k == 0), stop=(k == 7)), reads=[Wb["g"], xb], writes=[G])
                for k in range(8):
                    op("pe", lambda e: e.matmul(U[:, :], lhsT=Wb["u"][:, k, hc*128:(hc+1)*128], rhs=xb[:, k, ts_], start=(k == 0), stop=(k == 7)), reads=[Wb["u"], xb], writes=[U])
                op("act", lambda e: e.activation(out=s_[:], in_=G[:, :], func=AF.Silu), reads=[G], writes=[s_])
                op("dve", lambda e: e.tensor_tensor(out=s_[:], in0=s_[:], in1=U[:, :], op=ALU.mult), reads=[s_, U], writes=[s_])
                op("dve", lambda e: e.tensor_tensor(out=h[:, hc, :], in0=s_[:], in1=Bp[:, :], op=ALU.mult), reads=[s_, Bp], writes=[h])
            for tt in range(4):
                t = tg * 4 + tt
                for ch in range(2):
                    D = Dp[dc % 2]; dc += 1
                    for hc in range(4):
                        op("pe", lambda e: e.matmul(D[:, :], lhsT=h[:, hc, tt*128:(tt+1)*128], rhs=Wb["d"][:, hc, ch*512:(ch+1)*512], start=(hc == 0), stop=(hc == 3)),
                           reads=[h, Wb["d"]], writes=[D])
                    if e_ == 0:
                        op("act", lambda e: e.copy(out=acc[:, t, ch*512:(ch+1)*512], in_=D[:, :]), reads=[D], writes=[acc])
                    else:
                        op("dve", lambda e: e.tensor_tensor(out=acc[:, t, ch*512:(ch+1)*512], in0=acc[:, t, ch*512:(ch+1)*512], in1=D[:, :], op=ALU.add), reads=[acc, D], writes=[acc])
    fw.dma("sp", A["out"].rearrange("(t p) n -> p t n", p=128), acc[:], key=acc, reads=[acc])


def emit_select(fw, nc, A):
    op = fw.op
    ind = fw.sb("ind", [128, 4])
    fw.dma("sp", ind[:], A["ind"], key=ind, writes=[ind])
    pairs = A["pairs"]
    CT = sum(c for _, _, c in pairs)
    xin = [fw.sb("xin%d" % i, [128, CT]) for i in range(3)]
    acc = [fw.sb("sacc%d" % i, [128, CT]) for i in range(2)]
    n = 0
    for t in range(16):
        a = acc[t % 2]
        for q in range(4):
            xi = xin[n % 3]; n += 1
            r0 = q * 2048 + t * 128
            c0 = 0
            for pi, (src, dst, C) in enumerate(pairs):
                fw.dma(("sp", "pool")[pi % 2], xi[:, c0:c0+C], src[r0:r0+128, :], key=xi, writes=[xi])
                c0 += C
            eng = "dve" if q % 2 == 0 else "pool"
            if q == 0:
                op("dve", lambda e: e.tensor_scalar(out=a[:], in0=xi[:], scalar1=ind[:, 0:1], scalar2=None, op0=ALU.mult), reads=[xi, ind], writes=[a])
            else:
                op("dve", lambda e: e.scalar_tensor_tensor(out=a[:], in0=xi[:], scalar=ind[:, q:q+1], in1=a[:], op0=ALU.mult, op1=ALU.add), reads=[xi, ind, a], writes=[a])
        c0 = 0
        for pi, (src, dst, C) in enumerate(pairs):
            fw.dma("sp", dst[t*128:(t+1)*128, :], a[:, c0:c0+C], key=a, reads=[a])
            c0 += C

def nsa_tables(p):
    n = np.arange(128)[:, None]; q = np.arange(128)[None, :]
    tabs = np.zeros((17, 128, 128), np.float32)
    for k in range(9):
        d = 2 * k + p
        tabs[k] = np.where(16 * n + 31 <= 128 * d + q, 0.0, NEGB)
    for u in range(2):
        tabs[9 + u] = np.where(128 * (u - p) + n > q, NEGB, 0.0)
    for u in range(6):
        dl = 128 * (u - 4 - p) + n - q
        tabs[11 + u] = np.where((dl <= 0) & (dl > -512), 0.0, NEGB)
    tabs = np.broadcast_to(tabs[:, :, None, :], (17, 128, 4, 128)).transpose(1, 0, 2, 3).reshape(128, 17, 512)
    y = np.arange(256)[None, :]; qi = np.arange(128)[:, None]
    rel = (y - 2 * p) - 128; hh = (qi >= 64).astype(np.int64)
    G = np.where(rel > hh, -1e30, np.where((rel == hh) | (rel == hh - 1), 1e4, 0.0)).astype(np.float32)
    return np.ascontiguousarray(tabs).astype(ml_dtypes.bfloat16), G


def const_inputs():
    c = {}
    for p in range(2):
        c["tabs%d" % p], c["G%d" % p] = nsa_tables(p)
    ii = np.arange(512)[:, None]; jj = np.arange(128)[None, :]
    ovl = ((ii * 16 < (jj + 1) * 64) & (ii * 16 + 32 > jj * 64)).astype(np.float32)
    c["ovl"] = np.ascontiguousarray(ovl.reshape(4, 128, 128).transpose(1, 0, 2)).astype(ml_dtypes.bfloat16)
    x = np.arange(8192)[None, :]; j = np.arange(128)[:, None]
    j64 = np.arange(64)[:, None]
    c["E"] = ((x // 64) % 64 == j64).astype(np.float32).astype(ml_dtypes.bfloat16)
    c["idb"] = np.eye(128, dtype=np.float32).astype(ml_dtypes.bfloat16)
    c["idf"] = np.eye(128, dtype=np.float32)
    s = np.arange(128)[:, None]; t = np.arange(128)[None, :]
    same = (s // 64) == (t // 64)
    mid = (t // 64) * 64 + 31
    LT = (same & (s <= t)).astype(np.float32)
    LR = (same & (s <= mid)).astype(np.float32)
    c["LT"] = LT; c["LD"] = LT - LR; c["LU"] = (same & (s > t)).astype(np.float32)
    c["ind"] = np.stack([(np.arange(128) < 64), (np.arange(128) >= 64)], 1).astype(np.float32)
    c["cmaskh"] = LT.copy()
    c["cmask"] = (s <= t).astype(np.float32)
    sel = np.zeros((16, 16 * 128), np.float32)
    for e in range(16):
        sel[e, e*128:(e+1)*128] = 1
    c["sel"] = sel
    return c


CONST_SPECS = [("tabs0", [128, 17, 512], BF16), ("tabs1", [128, 17, 512], BF16), ("G0", [128, 256], F32), ("G1", [128, 256], F32),
               ("ovl", [128, 4, 128], BF16), ("E", [64, 8192], BF16), ("idb", [128, 128], BF16), ("idf", [128, 128], F32),
               ("LT", [128, 128], F32), ("LD", [128, 128], F32), ("LU", [128, 128], F32), ("ind", [128, 2], F32),
               ("cmaskh", [128, 128], F32), ("cmask", [128, 128], F32), ("sel", [16, 2048], F32)]
LAYER_SPECS = [("w_in", [1024, 2840]), ("w1k", [64, 4096]), ("w1v", [64, 4096]), ("w2k", [128, 64]), ("w2v", [128, 64]), ("posk", [64, 32]), ("posv", [64, 32]),
               ("gb_0", [128, 384]), ("gb_1", [128, 384]), ("lsel", [128, 64]), ("vgain", [128, 256]), ("vbias", [128, 256]), ("wsT", [128, 4, 128]),
               ("bsT", [128, 4]), ("ogain", [128, 1024]), ("w_out", [1024, 1024]), ("ln1g", [128, 1024]), ("ln1b", [128, 1024]), ("wr", [1024, 20]),
               ("br", [128, 20]), ("wg", [16, 1024, 512]), ("wu", [16, 1024, 512]), ("wd", [16, 512, 1024]), ("ln2g", [128, 1024]), ("ln2b", [128, 1024])]
HT_BLOCKS = (512, 640, 768, 896, 1024, 1152, 1280, 1536)
DEBUG = False
NCORES = 2
DEBUG_NAMES = ()


def build_fused():
    nc = bass.Bass("TRN2", target_bir_lowering=False)
    I = {}
    def din(name, shape, dt=F32):
        I[name] = nc.dram_tensor(name, list(shape), dt, kind="ExternalInput").ap()
    din("x", [SEQ, 1024])
    for nm, sh, dt in CONST_SPECS:
        din(nm, sh, dt)
    for hd in range(4):
        din("hgl0_%d" % hd, [128, 64]); din("hgl1_%d" % hd, [128, 64])
    for l in range(2):
        for nm, sh in LAYER_SPECS:
            din("%s%d" % (nm, l), sh)
    din("qind", [128, 4])
    out = nc.dram_tensor("out", [2048, 1024], F32, kind="ExternalOutput").ap()
    def scr(name, shape):
        return nc.dram_tensor(name, list(shape), F32, kind="Internal").ap()
    h = scr("s_h", [SEQ, 2840]); hT = nc.dram_tensor("s_hT", [1024, SEQ], BF16, kind="Internal").ap(); yb = scr("s_yb", [SEQ, 512]); yc = scr("s_yc", [SEQ, 256])
    y = scr("s_y", [SEQ, 1024]); mix = scr("s_mix", [SEQ, 1024]); x1 = scr("s_x1", [SEQ, 1024]); moe = scr("s_moe", [SEQ, 1024]); x2 = scr("s_x2", [SEQ, 1024])
    dbg = {}
    STAGE_COUNT[0] = 0
    with ExitStack() as st:
        fw = FW(nc, st)
        for l in range(2):
            P = lambda nm: I["%s%d" % (nm, l)]
            src = I["x"] if l == 0 else x2
            dst = x2
            for qt in range(4):
                r = slice(qt*2048, (qt+1)*2048)
                stage(fw, emit_mm, src[r, :], P("w_in"), h[r, :], 1024, 2840, 16, I["idf"], hT_ap=hT[:, r], hT_blocks=HT_BLOCKS)
            for hk in range(2):
                for p in range(2):
                    A = dict(tabs=I["tabs%d" % p], G=I["G%d" % p], ovl=I["ovl"], E=I["E"], idb=I["idb"], idf=I["idf"], gb=P("gb_%d" % hk),
                             w1k=P("w1k"), w1v=P("w1v"), w2k=P("w2k"), w2v=P("w2v"), posk=P("posk"), posv=P("posv"),
                             ksT=hT[768 + hk*64:768 + (hk+1)*64, :], kwT=hT[896 + hk*64:896 + (hk+1)*64, :],
                             kcT=hT[512 + hk*64:512 + (hk+1)*64, :], vcT=hT[640 + hk*64:640 + (hk+1)*64, :],
                             vs=h[:, 1408 + hk*64:1408 + (hk+1)*64].rearrange("(k p) d -> p k d", p=128),
                             vw=h[:, 1664 + hk*64:1664 + (hk+1)*64].rearrange("(k p) d -> p k d", p=128),
                             ng=h[:, 1792 + hk*12:1792 + (hk+1)*12].rearrange("(i t q) c -> q i t c", t=2, q=128)[:, :, p, :])
                    A["p"] = p
                    A["qT"] = (lambda c, g, hk=hk, p=p: hT[hk*256 + g*64:hk*256 + (g+1)*64, :].rearrange("d (i t q) -> d i t q", t=2, q=128)[:, c*8:(c+1)*8, p, :])
                    A["out"] = (lambda i, hk=hk, p=p: yb[(2*i+p)*128:(2*i+p+1)*128, hk*256:(hk+1)*256])
                    stage(fw, emit_nsa, nc, A)
            for hd in range(4):
                tm = lambda c0: h[:, c0 + hd*64:c0 + (hd+1)*64].rearrange("(k p) d -> p k d", p=128)
                A = dict(q=tm(1816), f=tm(2072), iv=tm(2328), h0=I["hgl0_%d" % hd], h1=I["hgl1_%d" % hd], lsel=P("lsel"), LT=I["LT"], LD=I["LD"], LU=I["LU"],
                         ind=I["ind"], cmask=I["cmaskh"], idb=I["idb"], out=yc[:, hd*64:(hd+1)*64].rearrange("(k p) d -> p k d", p=128))
                stage(fw, emit_hgrn, nc, A)
            if l == 1:
                hq = scr("s_hq", [2048, 2840]); ybq = scr("s_ybq", [2048, 512]); ycq = scr("s_ycq", [2048, 256]); xq = scr("s_xq", [2048, 1024])
                pairs = [(h[:, 0:512], hq[:, 0:512], 512), (h[:, 2584:2840], hq[:, 2584:2840], 256), (yb, ybq, 512), (yc, ycq, 256), (x2, xq, 1024)]
                stage(fw, emit_select, nc, dict(ind=I["qind"], pairs=pairs))
                r = slice(0, 2048)
                A = dict(h=hq, yb=ybq, yc=ycq, out=y[r, :], vgain=P("vgain"), vbias=P("vbias"), wsT=P("wsT"), cmask=I["cmask"], bsT=P("bsT"), ogain=P("ogain"))
                stage(fw, emit_post, nc, A, 16)
                stage(fw, emit_mm, y[r, :], P("w_out"), mix[r, :], 1024, 1024, 16, I["idf"])
                stage(fw, emit_addln, nc, dict(a=xq, b=mix[r, :], g=P("ln1g"), beta=P("ln1b"), out=x1[r, :]), 16)
                A = dict(x=x1[r, :], out=moe[r, :], sel=I["sel"], idf=I["idf"], wr=P("wr"), br=P("br"), wg=P("wg"), wu=P("wu"), wd=P("wd"))
                stage(fw, emit_moe, nc, A)
                stage(fw, emit_addln, nc, dict(a=x1[r, :], b=moe[r, :], g=P("ln2g"), beta=P("ln2b"), out=out), 16)
                continue
            for qt in range(4):
                r = slice(qt*2048, (qt+1)*2048)
                A = dict(h=h[r, :], yb=yb[r, :], yc=yc[r, :], out=y[r, :], vgain=P("vgain"), vbias=P("vbias"), wsT=P("wsT"), cmask=I["cmask"], bsT=P("bsT"), ogain=P("ogain"))
                stage(fw, emit_post, nc, A, 16)
            for qt in range(4):
                r = slice(qt*2048, (qt+1)*2048)
                stage(fw, emit_mm, y[r, :], P("w_out"), mix[r, :], 1024, 1024, 16, I["idf"])
            for qt in range(4):
                r = slice(qt*2048, (qt+1)*2048)
                stage(fw, emit_addln, nc, dict(a=src[r, :], b=mix[r, :], g=P("ln1g"), beta=P("ln1b"), out=x1[r, :]), 16)
            for qt in range(4):
                r = slice(qt*2048, (qt+1)*2048)
                A = dict(x=x1[r, :], out=moe[r, :], sel=I["sel"], idf=I["idf"], wr=P("wr"), br=P("br"), wg=P("wg"), wu=P("wu"), wd=P("wd"))
                stage(fw, emit_moe, nc, A)
            for qt in range(4):
                r = slice(qt*2048, (qt+1)*2048)
                stage(fw, emit_addln, nc, dict(a=x1[r, :], b=moe[r, :], g=P("ln2g"), beta=P("ln2b"), out=dst[r, :]), 16)
            if DEBUG and l == 0:
                for nm, ap in (("h", h), ("yb", yb), ("yc", yc), ("y", y), ("x1", x1), ("moe", moe)):
                    if nm not in DEBUG_NAMES:
                        continue
                    d = nc.dram_tensor("dbg_" + nm, list(ap.shape), F32, kind="ExternalOutput").ap()
                    tt = T("dbg" + nm)
                    nrow = ap.shape[0]
                    for c in range(8):
                        fw.dma("sp", d[c*nrow//8:(c+1)*nrow//8, :], ap[c*nrow//8:(c+1)*nrow//8, :], key=tt)
                fw.barrier()
        print("fused instr", fw.n_inst)
    return nc


def _bc(v, n=128):
    v = np.asarray(v)
    return np.ascontiguousarray(np.broadcast_to(v[None, :], (n, v.shape[0])))


_NC = {}

def kernel(x, w_in, gm_v_gain, gm_v_bias, gm_w_s, gm_b_s, cmp_pos, cmp_w1, cmp_w2, nsa_gate_b,
           hg_lower, out_gain, w_out, ln1_g, ln1_b, router_group_w, router_group_b,
           router_expert_w, router_expert_b, exp_w_gate, exp_w_up, exp_w_down, ln2_g, ln2_b):
    A = lambda a: np.ascontiguousarray(np.asarray(a, dtype=np.float32))
    x = A(x); hg_lower = A(hg_lower)
    m = dict(const_inputs())
    for hd in range(4):
        m["hgl0_%d" % hd] = _bc(hg_lower[0][hd*64:(hd+1)*64]); m["hgl1_%d" % hd] = _bc(hg_lower[1][hd*64:(hd+1)*64])
    for l in range(2):
        L = lambda nm, v: m.__setitem__("%s%d" % (nm, l), v)
        L("w_in", A(w_in[l]))
        for j, nm in enumerate(("k", "v")):
            L("w1" + nm, np.ascontiguousarray(A(cmp_w1[l][j]).reshape(32, 64, 128).transpose(1, 0, 2).reshape(64, 32*128)))
            L("w2" + nm, A(cmp_w2[l][j])); L("pos" + nm, np.ascontiguousarray(A(cmp_pos[l][j]).T))
        gb = A(nsa_gate_b[l]).reshape(8, 3)
        for hk in range(2):
            L("gb_%d" % hk, _bc(np.tile(gb[hk*4:(hk+1)*4].reshape(12), 32)))
        L("lsel", np.full((128, 64), float(l), np.float32))
        L("vgain", _bc(A(gm_v_gain[l]))); L("vbias", _bc(A(gm_v_bias[l])))
        L("wsT", np.ascontiguousarray(A(gm_w_s[l]).transpose(2, 0, 1))); L("bsT", np.ascontiguousarray(A(gm_b_s[l]).T))
        L("ogain", _bc(A(out_gain[l]))); L("w_out", A(w_out[l]))
        L("ln1g", _bc(A(ln1_g[l]))); L("ln1b", _bc(A(ln1_b[l]))); L("ln2g", _bc(A(ln2_g[l]))); L("ln2b", _bc(A(ln2_b[l])))
        L("wr", np.ascontiguousarray(np.concatenate([A(router_group_w[l]), A(router_expert_w[l])], 1)))
        L("br", _bc(np.concatenate([A(router_group_b[l]), A(router_expert_b[l])])))
        L("wg", A(exp_w_gate[l])); L("wu", A(exp_w_up[l])); L("wd", A(exp_w_down[l]))
    if "nc" not in _NC:
        _NC["nc"] = build_fused()
    in_maps = []
    for c in range(8):
        mm = dict(m); mm["x"] = x[c // 4]
        ind = np.zeros((128, 4), np.float32); ind[:, c % 4] = 1.0
        mm["qind"] = ind
        in_maps.append(mm)
    res = run_bass_kernel_spmd(_NC["nc"], in_maps, core_ids=list(range(8)))
    _NC["res"] = res.results
    out = np.zeros((2, SEQ, 1024), np.float32)
    for c in range(8):
        out[c // 4, (c % 4)*2048:(c % 4 + 1)*2048] = res.results[c]["out"]
    return out
```
